# Optimizing a Trainium2 kernel written in Bass

```python
import jax
import jax.numpy as jnp
from jax import lax
import numpy as np

D_MODEL = 1024
BATCH = 2
SEQ = 16384
DEPTH = 2

N_MIXERS = 2
PLE_DIM = 256
D_FF = 2816
NORM_EPS = 1e-6
ROPE_THETA = 10000.0

NSA_HEADS = 16
NSA_KV_GROUPS = 4
NSA_HEAD_DIM = D_MODEL // NSA_HEADS
CMP_BLOCK = 32
CMP_STRIDE = 16
SEL_BLOCK = 64
N_SELECT = 16
WINDOW = 512
Q_BLOCK = 128
NSA_Q_COLS = NSA_HEADS * NSA_HEAD_DIM
NSA_KV_COLS = NSA_KV_GROUPS * NSA_HEAD_DIM
NSA_IN_COLS = NSA_Q_COLS + 6 * NSA_KV_COLS + 3 * NSA_HEADS

HGRN_EXPAND = 128
HGRN_HEADS = D_MODEL // HGRN_EXPAND
HGRN_DK = HGRN_EXPAND
HGRN_DV = D_MODEL // HGRN_HEADS
HGRN_CHUNK = 64
HGRN_IN_COLS = 2 * HGRN_HEADS * HGRN_DK + 2 * HGRN_HEADS * HGRN_DV

kernel_name = "hybrid_nsa_hgrn2_macaron_block"


def rms_norm(x, gain):
    xf = x.astype(jnp.float32)
    y = xf * lax.rsqrt(jnp.mean(xf * xf, axis=-1, keepdims=True) + NORM_EPS)
    return (y * gain.astype(jnp.float32)).astype(x.dtype)


def rope(x, pos):
    half = x.shape[-1] // 2
    inv = ROPE_THETA ** (-jnp.arange(half, dtype=jnp.float32) / half)
    ang = pos.astype(jnp.float32)[:, None] * inv[None, :]
    cos = jnp.cos(ang)[None, :, None, :]
    sin = jnp.sin(ang)[None, :, None, :]
    x1 = x[..., :half].astype(jnp.float32)
    x2 = x[..., half:].astype(jnp.float32)
    return jnp.concatenate([x1 * cos - x2 * sin, x2 * cos + x1 * sin], axis=-1).astype(x.dtype)


def masked_softmax(s, mask):
    s = jnp.where(mask, s.astype(jnp.float32), -jnp.inf)
    m = jnp.max(s, axis=-1, keepdims=True)
    m = jnp.where(jnp.isfinite(m), m, 0.0)
    e = jnp.exp(s - m)
    return e / jnp.maximum(jnp.sum(e, axis=-1, keepdims=True), 1e-30)


def swiglu(x, w_in, w_out):
    g, u = jnp.split(x @ w_in, 2, axis=-1)
    return (jax.nn.silu(g) * u) @ w_out


def compress_blocks(kv, pos_enc, w1, w2):
    b, s, g, d = kv.shape
    ratio = CMP_BLOCK // CMP_STRIDE
    n_cmp = s // CMP_STRIDE - ratio + 1
    c = kv.reshape(b, s // CMP_STRIDE, CMP_STRIDE, g, d)
    blocks = jnp.concatenate([c[:, r:r + n_cmp] for r in range(ratio)], axis=2)
    blocks = blocks + pos_enc[:, None, :].astype(kv.dtype)
    flat = blocks.transpose(0, 1, 3, 2, 4).reshape(b, n_cmp, g, CMP_BLOCK * d)
    return jax.nn.silu(flat @ w1) @ w2


def selection_importance(p_cmp, n_sel):
    r = SEL_BLOCK // CMP_STRIDE
    lo = -(CMP_BLOCK // CMP_STRIDE - 1)
    hi = r - 1
    n_cmp = p_cmp.shape[-1]
    pad = [(0, 0)] * (p_cmp.ndim - 1) + [(-lo, r * n_sel + hi + 1 - n_cmp)]
    pp = jnp.pad(p_cmp, pad)
    return sum(pp[..., o - lo:o - lo + r * n_sel:r] for o in range(lo, hi + 1))


def nsa_mixer(h, w_in, w_out, cmp_pos, cmp_w1, cmp_w2):
    b, s, _ = h.shape
    H, G, D = NSA_HEADS, NSA_KV_GROUPS, NSA_HEAD_DIM
    HG = H // G
    splits = [NSA_Q_COLS + i * NSA_KV_COLS for i in range(7)]
    q, k_c, v_c, k_s, v_s, k_w, v_w, gates = jnp.split(h @ w_in, splits, axis=-1)
    pos = jnp.arange(s)
    q = rope(q.reshape(b, s, H, D), pos) * (D ** -0.5)
    k_c, k_s, k_w = [rope(t.reshape(b, s, G, D), pos) for t in (k_c, k_s, k_w)]
    v_c, v_s, v_w = [t.reshape(b, s, G, D) for t in (v_c, v_s, v_w)]
    gates = jax.nn.sigmoid(gates.astype(jnp.float32)).reshape(b, s, H, 3)

    k_cmp = compress_blocks(k_c, cmp_pos[0], cmp_w1[0], cmp_w2[0])
    v_cmp = compress_blocks(v_c, cmp_pos[1], cmp_w1[1], cmp_w2[1])
    n_cmp = k_cmp.shape[1]
    cmp_end = jnp.arange(n_cmp) * CMP_STRIDE + CMP_BLOCK - 1

    n_sel = s // SEL_BLOCK
    top_n = min(N_SELECT, n_sel)
    k_sel = k_s.reshape(b, n_sel, SEL_BLOCK, G, D).transpose(0, 3, 1, 2, 4)
    v_sel = v_s.reshape(b, n_sel, SEL_BLOCK, G, D).transpose(0, 3, 1, 2, 4)
    blk = jnp.arange(n_sel)
    bi = jnp.arange(b)[:, None, None, None]
    gi = jnp.arange(G)[None, :, None, None]

    k_win = jnp.pad(k_w, ((0, 0), (WINDOW, 0), (0, 0), (0, 0)))
    v_win = jnp.pad(v_w, ((0, 0), (WINDOW, 0), (0, 0), (0, 0)))

    def query_block(j):
        start = j * Q_BLOCK
        t = start + jnp.arange(Q_BLOCK)
        qb = lax.dynamic_slice_in_dim(q, start, Q_BLOCK, axis=1).reshape(b, Q_BLOCK, G, HG, D)
        gb = lax.dynamic_slice_in_dim(gates, start, Q_BLOCK, axis=1).reshape(b, Q_BLOCK, G, HG, 3)

        s_c = jnp.einsum('bqghd,bkgd->bghqk', qb, k_cmp)
        p_c = masked_softmax(s_c, cmp_end[None, :] <= t[:, None])
        o_c = jnp.einsum('bghqk,bkgd->bqghd', p_c.astype(v_cmp.dtype), v_cmp)

        imp = selection_importance(jnp.sum(p_c, axis=2), n_sel)
        cur = t // SEL_BLOCK
        forced = (blk[None, :] == 0) | (blk[None, :] == cur[:, None]) | (blk[None, :] == cur[:, None] - 1)
        valid = blk[None, :] * SEL_BLOCK <= t[:, None]
        imp = jnp.where(forced, jnp.inf, jnp.where(valid, imp, -jnp.inf))
        _, idx = lax.top_k(imp, top_n)
        k_g = k_sel[bi, gi, idx]
        v_g = v_sel[bi, gi, idx]
        s_s = jnp.einsum('bqghd,bgqnkd->bghqnk', qb, k_g).reshape(b, G, HG, Q_BLOCK, top_n * SEL_BLOCK)
        tok = (idx[..., None] * SEL_BLOCK + jnp.arange(SEL_BLOCK)).reshape(b, G, Q_BLOCK, top_n * SEL_BLOCK)
        p_s = masked_softmax(s_s, tok[:, :, None] <= t[:, None])
        p_s = p_s.reshape(b, G, HG, Q_BLOCK, top_n, SEL_BLOCK)
        o_s = jnp.einsum('bghqnk,bgqnkd->bqghd', p_s.astype(v_g.dtype), v_g)

        kw = lax.dynamic_slice_in_dim(k_win, start, Q_BLOCK + WINDOW, axis=1)
        vw = lax.dynamic_slice_in_dim(v_win, start, Q_BLOCK + WINDOW, axis=1)
        kpos = start - WINDOW + jnp.arange(Q_BLOCK + WINDOW)
        mask_w = (kpos[None, :] <= t[:, None]) & (kpos[None, :] > t[:, None] - WINDOW) & (kpos[None, :] >= 0)
        s_w = jnp.einsum('bqghd,bkgd->bghqk', qb, kw)
        p_w = masked_softmax(s_w, mask_w)
        o_w = jnp.einsum('bghqk,bkgd->bqghd', p_w.astype(vw.dtype), vw)

        o = gb[..., 0:1] * o_c + gb[..., 1:2] * o_s + gb[..., 2:3] * o_w
        return o.reshape(b, Q_BLOCK, H * D).astype(h.dtype)

    out = lax.map(query_block, jnp.arange(s // Q_BLOCK))
    out = out.transpose(1, 0, 2, 3).reshape(b, s, H * D)
    return out @ w_out


def hgrn2_mixer(h, w_in, w_out, norm_gain, lower_bound):
    b, s, _ = h.shape
    H, DK, DV, C = HGRN_HEADS, HGRN_DK, HGRN_DV, HGRN_CHUNK
    q, f, i, g = jnp.split(h @ w_in, [H * DK, 2 * H * DK, 2 * H * DK + H * DV], axis=-1)
    q = jax.nn.silu(q.astype(jnp.float32))
    lb = lower_bound.astype(jnp.float32)
    log_f = jnp.logaddexp(jnp.log(lb), jnp.log1p(-lb) + jax.nn.log_sigmoid(f.astype(jnp.float32)))
    k = -jnp.expm1(log_f)

    def to_chunks(t, d):
        return t.reshape(b, s // C, C, H, d).transpose(1, 0, 3, 2, 4)

    xs = (to_chunks(q, DK), to_chunks(k, DK), to_chunks(i.astype(jnp.float32), DV), to_chunks(log_f, DK))
    causal = jnp.tril(jnp.ones((C, C), dtype=bool))[None, None, :, :, None]

    def chunk_step(state, inp):
        q_c, k_c, v_c, lf_c = inp
        cum = jnp.cumsum(lf_c, axis=2)
        diff = cum[:, :, :, None, :] - cum[:, :, None, :, :]
        decay = jnp.exp(jnp.where(causal, diff, -jnp.inf))
        att = jnp.einsum('bhtd,bhsd,bhtsd->bhts', q_c, k_c, decay)
        o = jnp.einsum('bhts,bhse->bhte', att, v_c) + jnp.einsum('bhtd,bhde->bhte', q_c * jnp.exp(cum), state)
        last = cum[:, :, -1]
        state = jnp.exp(last)[..., None] * state + jnp.einsum(
            'bhsd,bhse->bhde', k_c * jnp.exp(last[:, :, None, :] - cum), v_c)
        return state, o

    state0 = jnp.zeros((b, H, DK, DV), jnp.float32)
    _, o = lax.scan(chunk_step, state0, xs)
    o = o.transpose(1, 0, 3, 2, 4).reshape(b, s, H, DV)
    o = rms_norm(o, norm_gain) * jax.nn.silu(g.astype(jnp.float32)).reshape(b, s, H, DV)
    return o.reshape(b, s, H * DV).astype(h.dtype) @ w_out


def setup_inputs(seed: int = 0) -> dict:
    key = jax.random.key(seed)
    ks = jax.random.split(key, 16)
    n_nsa = (DEPTH + 1) // 2
    n_hgrn = DEPTH // 2
    f32 = jnp.float32

    def w(k, shape, fan_in):
        return jax.random.normal(k, shape, f32) * (fan_in ** -0.5)

    return {
        "x": jax.random.normal(ks[0], (BATCH, SEQ, D_MODEL), f32),
        "p": jax.random.normal(ks[1], (DEPTH, BATCH, SEQ, PLE_DIM), f32),
        "norm_gains": 1.0 + 0.05 * jax.random.normal(ks[2], (DEPTH, 8, D_MODEL), f32),
        "ffn_w_in": w(ks[3], (DEPTH, 2, D_MODEL, 2 * D_FF), D_MODEL),
        "ffn_w_out": w(ks[4], (DEPTH, 2, D_FF, D_MODEL), D_FF),
        "ple_w_in": w(ks[5], (DEPTH, PLE_DIM, D_MODEL), PLE_DIM),
        "ple_w_gate": w(ks[6], (DEPTH, D_MODEL, D_MODEL), D_MODEL),
        "nsa_w_in": w(ks[7], (n_nsa, D_MODEL, NSA_IN_COLS), D_MODEL),
        "nsa_w_out": w(ks[8], (n_nsa, NSA_Q_COLS, D_MODEL), NSA_Q_COLS),
        "nsa_cmp_pos": 0.5 * jax.random.normal(ks[9], (n_nsa, 2, CMP_BLOCK, NSA_HEAD_DIM), f32),
        "nsa_cmp_w1": w(ks[10], (n_nsa, 2, CMP_BLOCK * NSA_HEAD_DIM, NSA_HEAD_DIM), CMP_BLOCK * NSA_HEAD_DIM),
        "nsa_cmp_w2": w(ks[11], (n_nsa, 2, NSA_HEAD_DIM, NSA_HEAD_DIM), NSA_HEAD_DIM),
        "hgrn_w_in": w(ks[12], (n_hgrn, D_MODEL, HGRN_IN_COLS), D_MODEL),
        "hgrn_w_out": w(ks[13], (n_hgrn, HGRN_HEADS * HGRN_DV, D_MODEL), HGRN_HEADS * HGRN_DV),
        "hgrn_norm": 1.0 + 0.05 * jax.random.normal(ks[14], (n_hgrn, HGRN_DV), f32),
        "hgrn_lb_logits": jax.random.normal(ks[15], (DEPTH, HGRN_HEADS * HGRN_DK), f32),
    }


def reference(x, p, norm_gains, ffn_w_in, ffn_w_out, ple_w_in, ple_w_gate,
              nsa_w_in, nsa_w_out, nsa_cmp_pos, nsa_cmp_w1, nsa_cmp_w2,
              hgrn_w_in, hgrn_w_out, hgrn_norm, hgrn_lb_logits):
    lb_sm = jax.nn.softmax(hgrn_lb_logits.astype(jnp.float32), axis=0)
    lower_bounds = jnp.cumsum(lb_sm, axis=0) - lb_sm[0]
    for layer in range(DEPTH):
        ng = norm_gains[layer]
        x = x + 0.5 * rms_norm(swiglu(rms_norm(x, ng[0]), ffn_w_in[layer, 0], ffn_w_out[layer, 0]), ng[1])
        hn = rms_norm(x, ng[2])
        j = layer // N_MIXERS
        if layer % N_MIXERS == 0:
            y = nsa_mixer(hn, nsa_w_in[j], nsa_w_out[j], nsa_cmp_pos[j], nsa_cmp_w1[j], nsa_cmp_w2[j])
        else:
            y = hgrn2_mixer(hn, hgrn_w_in[j], hgrn_w_out[j], hgrn_norm[j], lower_bounds[layer])
        x = x + rms_norm(y, ng[3])
        x = x + 0.5 * rms_norm(swiglu(rms_norm(x, ng[4]), ffn_w_in[layer, 1], ffn_w_out[layer, 1]), ng[5])
        gate = jax.nn.sigmoid(rms_norm(x, ng[6]) @ ple_w_gate[layer])
        x = x + rms_norm((p[layer] @ ple_w_in[layer]) * gate, ng[7])
    return x
```

```python
import numpy as np
import ml_dtypes
import concourse.bass as bass
import concourse.mybir as mybir
from concourse.bass_utils import run_bass_kernel_spmd
from contextlib import ExitStack

F32 = mybir.dt.float32
BF16 = mybir.dt.bfloat16
AF = mybir.ActivationFunctionType
ALU = mybir.AluOpType
AX = mybir.AxisListType
NPBF = ml_dtypes.bfloat16

D = 1024
DFF = 2816
NCH = 8
NFC = DFF // 128
EPS = 1e-6
SEM_MAX = 30000
NCORES = 8


class T:
    def __init__(self, h, name):
        self.h = h
        self.name = name
        self.w = None
        self.rs = {}
        self.dsem = None
        self.dcnt = 0

    def __getitem__(self, k):
        return self.h[k]


class Sched:
    ENGS = ("pe", "act", "dve", "pool", "sp")

    def __init__(self, nc, es):
        self.nc = nc
        self.es = es
        self.streams = {e: [] for e in self.ENGS}
        self.sem = {}
        self.cnt = {}
        self.nsem = 0
        for e in self.ENGS:
            self._newsem(e)
        self.waited = {}
        self.ntile = 0
        self.dsem = None
        self.dcnt = 0

    def _mksem(self, name):
        self.nsem += 1
        return self.es.enter_context(self.nc.semaphore(name))

    def _newsem(self, e):
        self.sem[e] = self._mksem(f"s_{e}_{self.nsem}")
        self.cnt[e] = 0

    def sb(self, shape, dt, name=None):
        self.ntile += 1
        name = "sb_" + (name or f"t{self.ntile}")
        h = self.es.enter_context(self.nc.sbuf_tensor(name, list(shape), dt))
        return T(h, name)

    def ps(self, shape, dt=F32, name=None):
        self.ntile += 1
        name = "ps_" + (name or f"p{self.ntile}")
        h = self.es.enter_context(self.nc.psum_tensor(name, list(shape), dt))
        return T(h, name)

    def dram(self, name, shape, dt):
        h = self.nc.dram_tensor(name, list(shape), dt)
        return T(h.ap(), name)

    def _deps(self, eng, reads, writes, same_eng_sync):
        deps = []
        for t in reads:
            if t.w is not None:
                deps.append(t.w)
        for t in writes:
            if t.w is not None:
                deps.append(t.w)
            deps.extend(t.rs.values())
        best = {}
        for (sem, val, src) in deps:
            if src == eng and not same_eng_sync:
                continue
            if id(sem) not in best or best[id(sem)][1] < val:
                best[id(sem)] = (sem, val)
        waits = []
        for (sem, val) in best.values():
            key = (eng, id(sem))
            if self.waited.get(key, 0) >= val:
                continue
            self.waited[key] = val
            waits.append((sem, val))
        return waits

    def op(self, eng, fn, reads=(), writes=(), sync_same=None):
        if sync_same is None:
            sync_same = eng != "pe"
        waits = self._deps(eng, reads, writes, sync_same)
        if self.cnt[eng] >= SEM_MAX:
            self._newsem(eng)
        self.cnt[eng] += 1
        tok = (self.sem[eng], self.cnt[eng], eng)
        self.streams[eng].append((waits, fn, (self.sem[eng], 1)))
        for t in reads:
            t.rs[id(tok[0])] = tok
        for t in writes:
            t.w = tok
            t.rs = {}
        return tok

    def dma(self, q, out, in_, reads=(), writes=(), semt=None, **kw):
        waits = self._deps(q, reads, writes, True)
        t = semt if semt is not None else (list(writes) + list(reads))[0]
        if t.dsem is None or t.dcnt >= SEM_MAX:
            t.dsem = self._mksem(f"d_{self.nsem}")
            t.dcnt = 0
        t.dcnt += 16
        tok = (t.dsem, t.dcnt, "dma")
        sem = t.dsem
        self.streams[q].append((waits, lambda e: e.dma_start(out=out, in_=in_, **kw), (sem, 16)))
        for r in reads:
            r.rs[id(tok[0])] = tok
        for w in writes:
            w.w = tok
            w.rs = {}
        return tok

    def final_wait(self, eng, toks):
        best = {}
        for (s, v, _) in toks:
            if id(s) not in best or best[id(s)][1] < v:
                best[id(s)] = (s, v)
        self.streams[eng].append((list(best.values()), None, None))

    def emit(self):
        nc = self.nc
        streams = self.streams
        with nc.Block() as block:
            def run(e, name):
                for (waits, fn, inc) in streams[name]:
                    for (sem, val) in waits:
                        e.wait_ge(sem, val)
                    if fn is not None:
                        ins = fn(e)
                        if inc is not None:
                            ins.then_inc(inc[0], inc[1])

            @block.tensor
            def _(e):
                run(e, "pe")

            @block.scalar
            def _(e):
                run(e, "act")

            @block.vector
            def _(e):
                run(e, "dve")

            @block.gpsimd
            def _(e):
                run(e, "pool")

            @block.sync
            def _(e):
                run(e, "sp")


class Consts:
    def __init__(self, S):
        self.ones = S.sb([128, 128], BF16, "c_ones")
        S.op("pool", lambda e: e.memset(self.ones[:], 1.0), writes=[self.ones])


def rms_stats(S, C, sq_tiles_fn, nchunks, pstat, rstd, nt, dim):
    for c in range(nchunks):
        t, ap = sq_tiles_fn(c)
        S.op("pe", lambda e, ap=ap, c=c: e.matmul(pstat[:, 0:nt], lhsT=C.ones[:], rhs=ap, start=(c == 0), stop=(c == nchunks - 1)),
             reads=[C.ones, t], writes=[pstat])
    S.op("act", lambda e: e.activation(out=rstd[:, 0:nt], in_=pstat[:, 0:nt], func=AF.Sqrt, scale=1.0 / dim, bias=EPS),
         reads=[pstat], writes=[rstd])
    S.op("dve", lambda e: e.reciprocal(out=rstd[:, 0:nt], in_=rstd[:, 0:nt]), reads=[rstd], writes=[rstd])


def build_ffn(T_tok, NT=256):
    nc = bass.Bass("TRN2", target_bir_lowering=False)
    xT = nc.dram_tensor("xT", [D, T_tok], F32, kind="ExternalInput").ap()
    w_in = nc.dram_tensor("w_in", [D, 2 * DFF], F32, kind="ExternalInput").ap()
    w_out = nc.dram_tensor("w_out", [DFF, D], F32, kind="ExternalInput").ap()
    ng = nc.dram_tensor("ng", [128, 16], F32, kind="ExternalInput").ap()
    yT = nc.dram_tensor("yT", [D, T_tok], F32, kind="ExternalOutput").ap()
    with ExitStack() as es:
        S = Sched(nc, es)
        C = Consts(S)
        toks = ffn_phase(S, C, xT, yT, w_in, w_out, ng, T_tok, NT)
        S.final_wait("sp", toks)
        S.emit()
    return nc


def ffn_phase(S, C, xT, yT, w_in, w_out, ng, T_tok, NT):
    nt = NT
    ntiles = T_tok // nt
    win = S.sb([128, NCH, 2 * DFF], BF16, "win")
    wout = S.sb([128, NFC, D], BF16, "wout")
    g = S.sb([128, 16], F32, "gains")
    S.dma("sp", g[:], ng, writes=[g])
    w_in_v = w_in.rearrange("(c p) f -> p c f", p=128)
    w_out_v = w_out.rearrange("(j p) f -> p j f", p=128)
    for c in range(NCH):
        S.dma("pool", win[:, c, :], w_in_v[:, c, :], writes=[win])
    for j in range(0, NFC, 2):
        S.dma("pool", wout[:, j:j + 2, :], w_out_v[:, j:j + 2, :], writes=[wout])
    xs = [S.sb([128, NCH, nt], F32, f"x{i}") for i in range(2)]
    xn = S.sb([128, NCH, nt], BF16, "xn")
    sq = [S.sb([128, nt], BF16, f"sq{i}") for i in range(2)]
    act = S.sb([128, NFC, nt], BF16, "act")
    y = S.sb([128, NCH, nt], F32, "y")
    sg = [S.sb([128, nt], F32, f"sg{i}") for i in range(2)]
    rstd = S.sb([128, nt], F32, "rstd")
    rstd2 = S.sb([128, nt], F32, "rstd2")
    tmp = [S.sb([128, nt], F32, f"tmp{i}") for i in range(2)]
    pstat = S.ps([128, 512], F32, "pstat")
    pg = [S.ps([128, 512], F32, f"pg{i}") for i in range(2)]
    pu = [S.ps([128, 512], F32, f"pu{i}") for i in range(2)]
    py = [S.ps([128, 512], F32, f"py{i}") for i in range(2)]
    xT_v = xT.rearrange("(c p) t -> p c t", p=128)
    yT_v = yT.rearrange("(c p) t -> p c t", p=128)
    out_toks = []

    def load(i):
        x = xs[i % 2]
        S.dma("sp", x[:], xT_v[:, :, i * nt:(i + 1) * nt], writes=[x])

    load(0)
    for i in range(ntiles):
        x = xs[i % 2]
        if i + 1 < ntiles:
            load(i + 1)
        def sqf(c, x=x):
            s = sq[c % 2]
            S.op("pool", lambda e, s=s, c=c: e.tensor_tensor(out=s[:], in0=x[:, c, :], in1=x[:, c, :], op=ALU.mult), reads=[x], writes=[s])
            return s, s[:]
        rms_stats(S, C, sqf, NCH, pstat, rstd, nt, D)
        for c in range(NCH):
            S.op("dve", lambda e, c=c, x=x: e.scalar_tensor_tensor(out=xn[:, c, :], in0=x[:, c, :], scalar=g[:, c:c + 1], in1=rstd[:], op0=ALU.mult, op1=ALU.mult),
                 reads=[x, g, rstd], writes=[xn])
        for j in range(NFC):
            a, b = pg[j % 2], pu[j % 2]
            for c in range(NCH):
                S.op("pe", lambda e, a=a, c=c, j=j: e.matmul(a[:, 0:nt], lhsT=win[:, c, j * 128:(j + 1) * 128], rhs=xn[:, c, :], start=(c == 0), stop=(c == NCH - 1)),
                     reads=[win, xn], writes=[a])
            for c in range(NCH):
                S.op("pe", lambda e, b=b, c=c, j=j: e.matmul(b[:, 0:nt], lhsT=win[:, c, DFF + j * 128:DFF + (j + 1) * 128], rhs=xn[:, c, :], start=(c == 0), stop=(c == NCH - 1)),
                     reads=[win, xn], writes=[b])
            s = sg[j % 2]
            S.op("act", lambda e, a=a, s=s: e.activation(out=s[:], in_=a[:, 0:nt], func=AF.Silu), reads=[a], writes=[s])
            S.op("dve", lambda e, b=b, s=s, j=j: e.tensor_tensor(out=act[:, j, :], in0=b[:, 0:nt], in1=s[:], op=ALU.mult), reads=[b, s], writes=[act])
        ysq = []
        for m in range(NCH):
            p = py[m % 2]
            for j in range(NFC):
                S.op("pe", lambda e, p=p, j=j, m=m: e.matmul(p[:, 0:nt], lhsT=wout[:, j, m * 128:(m + 1) * 128], rhs=act[:, j, :], start=(j == 0), stop=(j == NFC - 1)),
                     reads=[wout, act], writes=[p])
            S.op("act", lambda e, p=p, m=m: e.activation(out=y[:, m, :], in_=p[:, 0:nt], func=AF.Copy), reads=[p], writes=[y])
        def sqy(c):
            s = sq[c % 2]
            S.op("pool", lambda e, s=s, c=c: e.tensor_tensor(out=s[:], in0=y[:, c, :], in1=y[:, c, :], op=ALU.mult), reads=[y], writes=[s])
            return s, s[:]
        rms_stats(S, C, sqy, NCH, pstat, rstd2, nt, D)
        for m in range(NCH):
            t = tmp[m % 2]
            S.op("dve", lambda e, m=m, t=t: e.scalar_tensor_tensor(out=t[:], in0=y[:, m, :], scalar=g[:, 8 + m:9 + m], in1=rstd2[:], op0=ALU.mult, op1=ALU.mult),
                 reads=[y, g, rstd2], writes=[t])
            S.op("dve", lambda e, m=m, t=t, x=x: e.scalar_tensor_tensor(out=x[:, m, :], in0=t[:], scalar=0.5, in1=x[:, m, :], op0=ALU.mult, op1=ALU.add),
                 reads=[t, x], writes=[x])
        out_toks.append(S.dma("sp", yT_v[:, :, i * nt:(i + 1) * nt], x[:], reads=[x]))
    return out_toks


def ng_layout(norm_gains_l, idxs):
    a = np.asarray(norm_gains_l, dtype=np.float32)[list(idxs)]
    a = a.reshape(len(idxs), NCH, 128).transpose(2, 0, 1).reshape(128, len(idxs) * NCH)
    return np.ascontiguousarray(a)


NSA_NROPE = 14


def build_nsain(T_tok, NT=256):
    nc = bass.Bass("TRN2", target_bir_lowering=False)
    xT = nc.dram_tensor("xT", [D, T_tok], F32, kind="ExternalInput").ap()
    w_fm = nc.dram_tensor("w_fm", [D, 30 * 128], F32, kind="ExternalInput").ap()
    w_tm = nc.dram_tensor("w_tm", [D, 560], F32, kind="ExternalInput").ap()
    ng = nc.dram_tensor("ng", [128, 8], F32, kind="ExternalInput").ap()
    cs = nc.dram_tensor("cs", [128, 2, T_tok], F32, kind="ExternalInput").ap()
    qkT = nc.dram_tensor("qkT", [NSA_NROPE * 128, T_tok], BF16, kind="ExternalOutput").ap()
    vcT = nc.dram_tensor("vcT", [256, T_tok], BF16, kind="ExternalOutput").ap()
    vsw = nc.dram_tensor("vsw", [T_tok, 512], BF16, kind="ExternalOutput").ap()
    gates = nc.dram_tensor("gates", [T_tok, 48], F32, kind="ExternalOutput").ap()
    nt = NT
    ntiles = T_tok // nt
    nsub = nt // 128
    with ExitStack() as es:
        S = Sched(nc, es)
        C = Consts(S)
        wf = S.sb([128, NCH, 30 * 128], BF16, "wf")
        wt = S.sb([128, NCH, 560], BF16, "wt")
        g = S.sb([128, 8], F32, "gains")
        S.dma("sp", g[:], ng, writes=[g])
        w_fm_v = w_fm.rearrange("(c p) f -> p c f", p=128)
        w_tm_v = w_tm.rearrange("(c p) f -> p c f", p=128)
        for c in range(NCH):
            S.dma("pool", wf[:, c, :], w_fm_v[:, c, :], writes=[wf])
        for c in range(NCH):
            S.dma("pool", wt[:, c, :], w_tm_v[:, c, :], writes=[wt])
        xs = [S.sb([128, NCH, nt], F32, f"x{i}") for i in range(2)]
        cst = [S.sb([128, 2, nt], F32, f"cs{i}") for i in range(2)]
        hn = S.sb([128, NCH, nt], BF16, "hn")
        sq = [S.sb([128, nt], BF16, f"sq{i}") for i in range(2)]
        rstd = S.sb([128, nt], F32, "rstd")
        t1 = [S.sb([128, nt], F32, f"t1_{i}") for i in range(2)]
        t2 = [S.sb([128, nt], F32, f"t2_{i}") for i in range(2)]
        oqk = [S.sb([128, NSA_NROPE, nt], BF16, f"oqk{i}") for i in range(2)]
        ovc = [S.sb([128, 2, nt], BF16, f"ovc{i}") for i in range(2)]
        ovs = [S.sb([128, nsub, 512], BF16, f"ovs{i}") for i in range(2)]
        ogt = [S.sb([128, nsub, 48], F32, f"ogt{i}") for i in range(2)]
        pstat = S.ps([128, 512], F32, "pstat")
        pa = [S.ps([128, 512], F32, f"pa{i}") for i in range(2)]
        pb = [S.ps([128, 512], F32, f"pb{i}") for i in range(2)]
        pt = [S.ps([128, 512], F32, f"pt{i}") for i in range(2)]
        pgt = T(pstat.h[:, 256:512], "pgt_alias")
        pgt = pstat
        xT_v = xT.rearrange("(c p) t -> p c t", p=128)
        qk_v = qkT.rearrange("(c p) t -> p c t", p=128)
        vc_v = vcT.rearrange("(c p) t -> p c t", p=128)
        vsw_v = vsw.rearrange("(s p) f -> p s f", p=128)
        gt_v = gates.rearrange("(s p) f -> p s f", p=128)
        toks = []

        def load(i):
            S.dma("sp", xs[i % 2][:], xT_v[:, :, i * nt:(i + 1) * nt], writes=[xs[i % 2]])
            S.dma("sp", cst[i % 2][:], cs[:, :, i * nt:(i + 1) * nt], writes=[cst[i % 2]])

        load(0)
        for i in range(ntiles):
            x = xs[i % 2]
            cs_t = cst[i % 2]
            if i + 1 < ntiles:
                load(i + 1)

            def sqf(c, x=x):
                s = sq[c % 2]
                S.op("pool", lambda e, s=s, c=c: e.tensor_tensor(out=s[:], in0=x[:, c, :], in1=x[:, c, :], op=ALU.mult), reads=[x], writes=[s])
                return s, s[:]
            rms_stats(S, C, sqf, NCH, pstat, rstd, nt, D)
            for c in range(NCH):
                S.op("dve", lambda e, c=c, x=x: e.scalar_tensor_tensor(out=hn[:, c, :], in0=x[:, c, :], scalar=g[:, c:c + 1], in1=rstd[:], op0=ALU.mult, op1=ALU.mult),
                     reads=[x, g, rstd], writes=[hn])
            oq = oqk[i % 2]
            ov = ovc[i % 2]
            for j in range(NSA_NROPE):
                a, b = pa[j % 2], pb[j % 2]
                for c in range(NCH):
                    S.op("pe", lambda e, a=a, c=c, j=j: e.matmul(a[:, 0:nt], lhsT=wf[:, c, j * 128:(j + 1) * 128], rhs=hn[:, c, :], start=(c == 0), stop=(c == NCH - 1)),
                         reads=[wf, hn], writes=[a])
                for c in range(NCH):
                    S.op("pe", lambda e, b=b, c=c, j=j: e.matmul(b[:, 0:nt], lhsT=wf[:, c, (14 + j) * 128:(15 + j) * 128], rhs=hn[:, c, :], start=(c == 0), stop=(c == NCH - 1)),
                         reads=[wf, hn], writes=[b])
                u1, u2 = t1[j % 2], t2[j % 2]
                S.op("dve", lambda e, a=a, u1=u1, cs_t=cs_t: e.tensor_tensor(out=u1[:], in0=a[:, 0:nt], in1=cs_t[:, 0, :], op=ALU.mult), reads=[a, cs_t], writes=[u1])
                S.op("dve", lambda e, b=b, u2=u2, cs_t=cs_t: e.tensor_tensor(out=u2[:], in0=b[:, 0:nt], in1=cs_t[:, 1, :], op=ALU.mult), reads=[b, cs_t], writes=[u2])
                S.op("pool", lambda e, u1=u1, u2=u2, j=j, oq=oq: e.tensor_tensor(out=oq[:, j, :], in0=u1[:], in1=u2[:], op=ALU.add), reads=[u1, u2], writes=[oq])
            for j in range(2):
                a = pa[j % 2]
                for c in range(NCH):
                    S.op("pe", lambda e, a=a, c=c, j=j: e.matmul(a[:, 0:nt], lhsT=wf[:, c, (28 + j) * 128:(29 + j) * 128], rhs=hn[:, c, :], start=(c == 0), stop=(c == NCH - 1)),
                         reads=[wf, hn], writes=[a])
                S.op("act", lambda e, a=a, j=j, ov=ov: e.activation(out=ov[:, j, :], in_=a[:, 0:nt], func=AF.Copy), reads=[a], writes=[ov])
            osw = ovs[i % 2]
            og = ogt[i % 2]
            for s in range(nsub):
                p = pt[s % 2]
                for c in range(NCH):
                    S.op("pe", lambda e, p=p, c=c, s=s: e.matmul(p[:, 0:512], lhsT=hn[:, c, s * 128:(s + 1) * 128], rhs=wt[:, c, 0:512], start=(c == 0), stop=(c == NCH - 1)),
                         reads=[wt, hn], writes=[p])
                S.op("act", lambda e, p=p, s=s, osw=osw: e.activation(out=osw[:, s, :], in_=p[:, 0:512], func=AF.Copy), reads=[p], writes=[osw])
                for c in range(NCH):
                    S.op("pe", lambda e, c=c, s=s: e.matmul(pgt[:, 256:304], lhsT=hn[:, c, s * 128:(s + 1) * 128], rhs=wt[:, c, 512:560], start=(c == 0), stop=(c == NCH - 1)),
                         reads=[wt, hn], writes=[pgt])
                S.op("act", lambda e, s=s, og=og: e.activation(out=og[:, s, :], in_=pgt[:, 256:304], func=AF.Sigmoid), reads=[pgt], writes=[og])
            sl = slice(i * nt, (i + 1) * nt)
            toks.append(S.dma("sp", qk_v[:, 0:7, sl], oq[:, 0:7, :], reads=[oq]))
            toks.append(S.dma("sp", qk_v[:, 7:14, sl], oq[:, 7:14, :], reads=[oq]))
            toks.append(S.dma("sp", vc_v[:, :, sl], ov[:], reads=[ov]))
            toks.append(S.dma("sp", vsw_v[:, i * nsub:(i + 1) * nsub, :], osw[:], reads=[osw]))
            toks.append(S.dma("sp", gt_v[:, i * nsub:(i + 1) * nsub, :], og[:], reads=[og]))
        S.final_wait("sp", toks)
        S.emit()
    return nc


def rope_tables(pos):
    half = 32
    inv = (10000.0 ** (-np.arange(half, dtype=np.float32) / half)).astype(np.float32)
    ang = pos.astype(np.float32)[None, :] * inv[:, None]
    cos = np.cos(ang).astype(np.float32)
    sin = np.sin(ang).astype(np.float32)
    r = np.arange(128)
    f = r % 32
    sign = np.where((r % 64) < 32, -1.0, 1.0).astype(np.float32)
    out = np.empty((128, 2, len(pos)), np.float32)
    out[:, 0, :] = cos[f]
    out[:, 1, :] = sin[f] * sign[:, None]
    return out


def nsa_w_layout(w):
    w = np.asarray(w, np.float32)
    rope_cols = np.concatenate([np.arange(0, 1024), np.arange(1024, 1280), np.arange(1536, 1792), np.arange(2048, 2304)])
    sw = rope_cols.reshape(-1, 2, 32)[:, ::-1, :].reshape(-1)
    w_fm = np.concatenate([w[:, rope_cols], w[:, sw], w[:, 1280:1536]], axis=1)
    w_tm = np.concatenate([w[:, 1792:2048], w[:, 2304:2560], w[:, 2560:2608]], axis=1)
    return np.ascontiguousarray(w_fm), np.ascontiguousarray(w_tm)


BIG = 30000.0


def nsa_consts(S_len):
    nsel = S_len // 64
    ncmp = S_len // 16 - 1
    nct = (ncmp + 127) // 128
    bk = min(128, nsel)
    c = {}
    c["ident"] = np.eye(128, dtype=np.float32).astype(NPBF)
    kl = np.arange(128)[:, None]
    ql = np.arange(128)[None, :]
    c["tri_le"] = np.where(kl <= ql, 0.0, -BIG).astype(NPBF)
    c["tri_gt"] = np.where(kl > ql, 0.0, -BIG).astype(NPBF)
    m = np.arange(17)[None, :, None]
    cb = np.where(16 * kl[:, :, None] + 31 <= 128 * m + ql[None, :, :], 0.0, -BIG)
    c["cb"] = cb.astype(NPBF)
    ni = bk // 2
    eb = np.zeros((bk, ni, 128), np.float32)
    for i in range(ni):
        eb[2 * i, i, 0:64] = 1.0
        eb[2 * i + 1, i, 64:128] = 1.0
    c["ebig"] = eb.reshape(bk, ni * 128).astype(NPBF)
    cc = np.arange(nct * 128)[:, None]
    nn = np.arange(nsel)[None, :]
    mm = ((cc >= 4 * nn - 1) & (cc <= 4 * nn + 3) & (cc < ncmp)).astype(np.float32)
    c["mmat"] = np.ascontiguousarray(mm.reshape(nct, 128, nsel).transpose(1, 0, 2)).astype(NPBF)
    rel = np.arange(2 * nsel)[None, :] - nsel
    cur = (np.arange(128)[:, None] >= 64).astype(np.int64)
    fb = np.where((rel == cur) | (rel == cur - 1), 100.0, np.where(rel > cur, -100.0, 0.0))
    c["fb"] = fb.astype(np.float32)
    return c


def build_nsa_attn(S_len):
    nsel = S_len // 64
    ncmp = S_len // 16 - 1
    nct = (ncmp + 127) // 128
    ncp = nct * 128
    nq = S_len // 128
    bk = min(128, nsel)
    nbc = (nsel + 127) // 128
    ni = bk // 2
    VX = 65 + nsel
    nc = bass.Bass("TRN2", target_bir_lowering=False)
    def din(name, shape, dt):
        return nc.dram_tensor(name, list(shape), dt, kind="ExternalInput").ap()
    qT = din("qT", [64, 4, S_len], BF16)
    kc2 = din("kc2", [128, S_len], BF16)
    vc2 = din("vc2", [128, S_len], BF16)
    ksT = din("ksT", [64, S_len], BF16)
    kwT = din("kwT", [64, S_len], BF16)
    vs = din("vs", [S_len, 64], BF16)
    vw = din("vw", [S_len, 64], BF16)
    gates = din("gates", [S_len, 12], F32)
    w1s = din("w1s", [2, 128, 16, 64], F32)
    w2 = din("w2", [2, 64, 64], F32)
    pos2 = din("pos2", [2, 128, 16], F32)
    c_ident = din("ident", [128, 128], BF16)
    c_tle = din("tri_le", [128, 128], BF16)
    c_tgt = din("tri_gt", [128, 128], BF16)
    c_cb = din("cb", [128, 17, 128], BF16)
    c_eb = din("ebig", [bk, ni * 128], BF16)
    c_mm = din("mmat", [128, nct, nsel], BF16)
    c_fb = din("fb", [128, 2 * nsel], F32)
    o = nc.dram_tensor("o", [S_len, 256], BF16, kind="ExternalOutput").ap()
    with ExitStack() as es:
        S = Sched(nc, es)
        ident = S.sb([128, 128], BF16, "ident_sb")
        tle = S.sb([128, 128], BF16, "tle")
        tgt = S.sb([128, 128], BF16, "tgt")
        cb = S.sb([128, 17, 128], BF16, "cb")
        eb = S.sb([bk, ni * 128], BF16, "eb")
        fb = S.sb([128, 2 * nsel], F32, "fb")
        ks_sb = S.sb([64, S_len], BF16, "ks_sb")
        kw_sb = S.sb([64, S_len], BF16, "kw_sb")
        vsx = S.sb([128, nq, 65], BF16, "vsx")
        vwx = S.sb([128, nq, 65], BF16, "vwx")
        vcx = S.sb([128, nct, VX], BF16, "vcx")
        kcmpT = S.sb([64, ncp], BF16, "kcmpT")
        w1b = S.sb([128, 2, 16, 64], BF16, "w1b")
        w2b = S.sb([64, 2, 64], BF16, "w2b")
        p2f = S.sb([128, 2, 16], F32, "p2f")
        p2b = S.sb([128, 2, 16], BF16, "p2b")
        GS = min(S_len, 8192)
        stage = S.sb([128, GS], BF16, "stage")
        hbias = S.sb([64, 2], F32, "hbias")
        actT = S.sb([64, 512], BF16, "actT")
        for (t, src) in ((ident, c_ident), (tle, c_tle), (tgt, c_tgt), (eb, c_eb), (fb, c_fb)):
            S.dma("sp", t[:], src, writes=[t])
        for m0 in range(0, 17, 6):
            m1 = min(17, m0 + 6)
            S.dma("sp", cb[:, m0:m1, :], c_cb[:, m0:m1, :], writes=[cb])
        for (t, src) in ((ks_sb, ksT), (kw_sb, kwT)):
            st = min(4096, S_len)
            for s0 in range(0, S_len, st):
                S.dma("sp", t[:, s0:s0 + st], src[:, s0:s0 + st], writes=[t])
        for (t, src) in ((vsx, vs), (vwx, vw)):
            S.op("pool", lambda e, t=t: e.memset(t[:, :, 64:65], 1.0), writes=[t])
            srcv = src.rearrange("(n p) d -> p n d", p=128)
            for n0 in range(0, nq, 8):
                S.dma("sp", t[:, n0:n0 + 8, 0:64], srcv[:, n0:n0 + 8, :], writes=[t])
        S.op("pool", lambda e: e.memset(vcx[:, :, 64:65], 1.0), writes=[vcx])
        for ct in range(nct):
            S.dma("sp", vcx[:, ct, 65:VX], c_mm[:, ct, :], writes=[vcx])
        for kv in range(2):
            S.dma("pool", w1b[:, kv, :, :], w1s[kv], writes=[w1b])
            S.dma("pool", w2b[:, kv, :], w2[kv], writes=[w2b])
            S.dma("sp", p2f[:, kv, :], pos2[kv], writes=[p2f])
        S.op("dve", lambda e: e.tensor_copy(out=p2b[:], in_=p2f[:]), reads=[p2f], writes=[p2b])
        pss = [S.ps([128, 512], F32, f"pss{i}") for i in range(2)]
        acc = [S.ps([128, 512], F32, f"acc{i}") for i in range(4)]
        pmisc = S.ps([128, 512], F32, "pmisc")
        for kv in range(2):
            src = kc2 if kv == 0 else vc2
            for j in range(16):
                S.op("pe", lambda e, j=j, kv=kv: e.matmul(pmisc[0:64, 0:1], lhsT=w1b[:, kv, j, :], rhs=p2b[:, kv, j:j + 1], start=(j == 0), stop=(j == 15)),
                     reads=[w1b, p2b], writes=[pmisc])
            S.op("dve", lambda e, kv=kv: e.tensor_copy(out=hbias[:, kv:kv + 1], in_=pmisc[0:64, 0:1]), reads=[pmisc], writes=[hbias])
            for g0 in range(0, ncp, 512):
                gn = min(512, ncp - g0)
                t0 = g0 * 16
                if t0 % GS == 0:
                    for s0 in range(0, GS, 2048):
                        S.dma("sp", stage[:, s0:s0 + 2048], src[:, t0 + s0:t0 + s0 + 2048], writes=[stage])
                tb = t0 % GS
                ph = pss[0]
                for j in range(16):
                    S.op("pe", lambda e, j=j, kv=kv, tb=tb, gn=gn, ph=ph: e.matmul(ph[0:64, 0:gn], lhsT=w1b[:, kv, j, :], rhs=stage[:, tb + j:tb + j + 16 * (gn - 1) + 1:16], start=(j == 0), stop=(j == 15)),
                         reads=[w1b, stage], writes=[ph])
                S.op("act", lambda e, kv=kv, gn=gn, ph=ph: e.activation(out=actT[:, 0:gn], in_=ph[0:64, 0:gn], func=AF.Silu, bias=hbias[:, kv:kv + 1]), reads=[ph, hbias], writes=[actT])
                if kv == 0:
                    pk = pss[1]
                    S.op("pe", lambda e, gn=gn, pk=pk: e.matmul(pk[0:64, 0:gn], lhsT=w2b[:, 0, :], rhs=actT[:, 0:gn], start=True, stop=True), reads=[w2b, actT], writes=[pk])
                    S.op("dve", lambda e, gn=gn, g0=g0, pk=pk: e.tensor_copy(out=kcmpT[:, g0:g0 + gn], in_=pk[0:64, 0:gn]), reads=[pk], writes=[kcmpT])
                else:
                    for s in range(gn // 128):
                        S.op("pe", lambda e, s=s: e.matmul(pmisc[:, 0:64], lhsT=actT[:, s * 128:(s + 1) * 128], rhs=w2b[:, 1, :], start=True, stop=True), reads=[w2b, actT], writes=[pmisc])
                        ct = g0 // 128 + s
                        S.op("dve", lambda e, ct=ct: e.tensor_copy(out=vcx[:, ct, 0:64], in_=pmisc[:, 0:64]), reads=[pmisc], writes=[vcx])
        qt = [S.sb([64, 4, 128], BF16, f"qt{i}") for i in range(2)]
        gt = [S.sb([128, 12], F32, f"gt{i}") for i in range(2)]
        pT = [S.sb([128, 512], BF16, f"pT{i}") for i in range(4)]
        ot = [S.sb([128, 256], F32, f"ot{i}") for i in range(2)]
        otb = [S.sb([128, 256], BF16, f"otb{i}") for i in range(2)]
        imp = S.sb([128, nsel], F32, "imp")
        scr = S.sb([128, nsel], F32, "scr")
        nb = S.sb([128, nsel], BF16, "nb")
        m8 = S.sb([128, 16], F32, "m8")
        nbT = S.sb([bk, nbc, 128], BF16, "nbT")
        rz = S.sb([128, 4], F32, "rz")
        wgt = S.sb([128, 4], F32, "wgt")
        gv = gates.rearrange("(n p) f -> p n f", p=128)
        ov = o.rearrange("(n p) f -> p n f", p=128)
        out_toks = []
        pcount = [0]

        def load_q(j):
            S.dma("sp", qt[j % 2][:], qT[:, :, j * 128:(j + 1) * 128], writes=[qt[j % 2]])
            S.dma("sp", gt[j % 2][:], gv[:, j, :], writes=[gt[j % 2]])

        def branch(q, tiles, width, first_branch, gcol, g_t, o_t):
            n = len(tiles)
            sc = []

            def emit_scores(k):
                ps = pss[k % 2]
                mms = tiles[k][0]
                for idx, (lhsT_t, lhsT_ap, rhs_t, rhs_ap) in enumerate(mms):
                    S.op("pe", lambda e, ps=ps, a=lhsT_ap, b=rhs_ap, idx=idx, nm=len(mms): e.matmul(ps[:, 0:512].rearrange("p (h q) -> p h q", h=4), lhsT=a, rhs=b, start=(idx == 0), stop=(idx == nm - 1)),
                         reads=[lhsT_t, rhs_t], writes=[ps])

            def emit_exp(k):
                ps = pss[k % 2]
                p = pT[pcount[0] % 4]
                pcount[0] += 1
                S.op("act", lambda e, ps=ps, p=p: e.activation(out=p[:], in_=ps[:, 0:512], func=AF.Exp, scale=0.125), reads=[ps], writes=[p])
                return p

            def emit_pv(k, p):
                vt, vap = tiles[k][1]
                for h in range(4):
                    S.op("pe", lambda e, h=h, p=p, vap=vap, k=k: e.matmul(acc[h][:, 0:width], lhsT=p[:, h * 128:(h + 1) * 128], rhs=vap, start=(k == 0), stop=(k == n - 1)),
                         reads=[p, vt], writes=[acc[h]])

            emit_scores(0)
            for k in range(n):
                if k + 1 < n:
                    emit_scores(k + 1)
                p = emit_exp(k)
                emit_pv(k, p)
            for h in range(4):
                S.op("dve", lambda e, h=h: e.tensor_scalar(out=rz[:, h:h + 1], in0=acc[h][:, 64:65], scalar1=1e-30, scalar2=None, op0=ALU.max), reads=[acc[h]], writes=[rz])
            S.op("dve", lambda e: e.reciprocal(out=rz[:], in_=rz[:]), reads=[rz], writes=[rz])
            S.op("dve", lambda e: e.tensor_tensor(out=wgt[:], in0=rz[:], in1=g_t[:, gcol:12:3], op=ALU.mult), reads=[rz, g_t], writes=[wgt])
            for h in range(4):
                if first_branch:
                    S.op("dve", lambda e, h=h: e.tensor_scalar(out=o_t[:, h * 64:(h + 1) * 64], in0=acc[h][:, 0:64], scalar1=wgt[:, h:h + 1], scalar2=None, op0=ALU.mult),
                         reads=[acc[h], wgt], writes=[o_t])
                else:
                    S.op("dve", lambda e, h=h: e.scalar_tensor_tensor(out=o_t[:, h * 64:(h + 1) * 64], in0=acc[h][:, 0:64], scalar=wgt[:, h:h + 1], in1=o_t[:, h * 64:(h + 1) * 64], op0=ALU.mult, op1=ALU.add),
                         reads=[acc[h], wgt, o_t], writes=[o_t])

        load_q(0)
        for j in range(nq):
            q = qt[j % 2]
            g_t = gt[j % 2]
            o_t = ot[j % 2]
            if j + 1 < nq:
                load_q(j + 1)
            qap = q[:]
            nct_j = min(nct, (8 * j + 6) // 128 + 1)
            tiles = []
            for ct in range(nct_j):
                mms = [(kcmpT, kcmpT[:, ct * 128:(ct + 1) * 128], q, qap)]
                m = j - 16 * ct
                if m <= 16:
                    mms.append((ident, ident[:], cb, cb[:, max(m, 0), :].unsqueeze(1).to_broadcast([128, 4, 128])))
                tiles.append((mms, (vcx, vcx[:, ct, :])))
            branch(q, tiles, VX, True, 0, g_t, o_t)
            for h in range(4):
                if h == 0:
                    S.op("dve", lambda e: e.tensor_scalar(out=imp[:], in0=acc[0][:, 65:VX], scalar1=rz[:, 0:1], scalar2=None, op0=ALU.mult), reads=[acc[0], rz], writes=[imp])
                else:
                    S.op("dve", lambda e, h=h: e.scalar_tensor_tensor(out=imp[:], in0=acc[h][:, 65:VX], scalar=rz[:, h:h + 1], in1=imp[:], op0=ALU.mult, op1=ALU.add), reads=[acc[h], rz, imp], writes=[imp])
            S.op("dve", lambda e, j=j: e.tensor_tensor(out=imp[:], in0=imp[:], in1=fb[:, nsel - 2 * j:2 * nsel - 2 * j], op=ALU.add), reads=[imp, fb], writes=[imp])
            S.op("dve", lambda e: e.memset(imp[:, 0:1], 100.0), reads=[imp], writes=[imp])
            S.op("dve", lambda e: e.max(out=m8[:, 0:8], in_=imp[:]), reads=[imp], writes=[m8])
            S.op("dve", lambda e: e.match_replace(out=scr[:], in_to_replace=m8[:, 0:8], in_values=imp[:], imm_value=-1e30), reads=[imp, m8], writes=[scr])
            S.op("dve", lambda e: e.max(out=m8[:, 8:16], in_=scr[:]), reads=[scr], writes=[m8])
            S.op("dve", lambda e: e.tensor_scalar(out=nb[:], in0=imp[:], scalar1=m8[:, 15:16], scalar2=1.0, op0=ALU.is_ge, op1=ALU.subtract), reads=[imp, m8], writes=[nb])
            ptr = pmisc
            for cidx in range(nbc):
                S.op("pe", lambda e, cidx=cidx: e.transpose(out=ptr[0:bk, 0:64].bitcast(BF16), in_=nb[:, cidx * bk:(cidx + 1) * bk], identity=ident[:]), reads=[nb, ident], writes=[ptr])
                S.op("dve", lambda e, cidx=cidx: e.tensor_scalar(out=nbT[:, cidx, :], in0=ptr[0:bk, 0:64].bitcast(BF16), scalar1=BIG, scalar2=None, op0=ALU.mult), reads=[ptr], writes=[nbT])
            tiles = []
            for kt in range(j + 1):
                cidx = kt // ni
                mms = [(ks_sb, ks_sb[:, kt * 128:(kt + 1) * 128], q, qap),
                       (eb, eb[:, (kt % ni) * 128:(kt % ni + 1) * 128], nbT, nbT[:, cidx, :].unsqueeze(1).to_broadcast([bk, 4, 128]))]
                if kt == j:
                    mms.append((ident, ident[:], tle, tle[:].unsqueeze(1).to_broadcast([128, 4, 128])))
                tiles.append((mms, (vsx, vsx[:, kt, :])))
            branch(q, tiles, 65, False, 1, g_t, o_t)
            tiles = []
            for kt in range(max(0, j - 4), j + 1):
                mms = [(kw_sb, kw_sb[:, kt * 128:(kt + 1) * 128], q, qap)]
                if kt == j - 4:
                    mms.append((ident, ident[:], tgt, tgt[:].unsqueeze(1).to_broadcast([128, 4, 128])))
                if kt == j:
                    mms.append((ident, ident[:], tle, tle[:].unsqueeze(1).to_broadcast([128, 4, 128])))
                tiles.append((mms, (vwx, vwx[:, kt, :])))
            branch(q, tiles, 65, False, 2, g_t, o_t)
            ob_ = otb[j % 2]
            S.op("pool", lambda e, ob_=ob_, o_t=o_t: e.tensor_copy(out=ob_[:], in_=o_t[:]), reads=[o_t], writes=[ob_])
            out_toks.append(S.dma("sp", ov[:, j, :], ob_[:], reads=[ob_]))
        S.final_wait("sp", out_toks)
        S.emit()
    return nc


def nsa_attn_inputs(qkT_b, vcT_b, vsw_b, gates_b, cmp_pos, cmp_w1, cmp_w2, g, consts):
    S_len = qkT_b.shape[1]
    qT = np.ascontiguousarray(qkT_b[256 * g:256 * g + 256].reshape(4, 64, S_len).transpose(1, 0, 2))
    kc = qkT_b[1024 + 64 * g:1024 + 64 * g + 64]
    ks = qkT_b[1280 + 64 * g:1280 + 64 * g + 64]
    kw = qkT_b[1536 + 64 * g:1536 + 64 * g + 64]
    vc = vcT_b[64 * g:64 * g + 64]

    def stack2(a):
        o = np.zeros((128, S_len), a.dtype)
        o[0:64] = a
        o[64:128, :S_len - 16] = a[:, 16:]
        return o
    d = {"qT": qT, "kc2": stack2(kc), "vc2": stack2(vc), "ksT": np.ascontiguousarray(ks), "kwT": np.ascontiguousarray(kw),
         "vs": np.ascontiguousarray(vsw_b[:, 64 * g:64 * g + 64]), "vw": np.ascontiguousarray(vsw_b[:, 256 + 64 * g:256 + 64 * g + 64]),
         "gates": np.ascontiguousarray(gates_b.reshape(S_len, 16, 3)[:, 4 * g:4 * g + 4, :].reshape(S_len, 12))}
    w1 = np.asarray(cmp_w1, np.float32).reshape(2, 2, 16, 64, 64)
    d["w1s"] = np.ascontiguousarray(w1.transpose(0, 1, 3, 2, 4).reshape(2, 128, 16, 64))
    d["w2"] = np.ascontiguousarray(np.asarray(cmp_w2, np.float32))
    p = np.asarray(cmp_pos, np.float32).reshape(2, 2, 16, 64)
    d["pos2"] = np.ascontiguousarray(p.transpose(0, 1, 3, 2).reshape(2, 128, 16))
    d.update(consts)
    return d


def build_out(T_tok, NT=512):
    nc = bass.Bass("TRN2", target_bir_lowering=False)
    xT = nc.dram_tensor("xT", [D, T_tok], F32, kind="ExternalInput").ap()
    oT = nc.dram_tensor("oT", [D, T_tok], BF16, kind="ExternalInput").ap()
    w = nc.dram_tensor("w", [D, D], F32, kind="ExternalInput").ap()
    ng = nc.dram_tensor("ng", [128, 8], F32, kind="ExternalInput").ap()
    yT = nc.dram_tensor("yT", [D, T_tok], F32, kind="ExternalOutput").ap()
    nt = NT
    ntiles = T_tok // nt
    with ExitStack() as es:
        S = Sched(nc, es)
        C = Consts(S)
        wb = S.sb([128, NCH, D], BF16, "wb")
        g = S.sb([128, 8], F32, "gains")
        S.dma("sp", g[:], ng, writes=[g])
        w_v = w.rearrange("(c p) f -> p c f", p=128)
        for c in range(NCH):
            S.dma("pool", wb[:, c, :], w_v[:, c, :], writes=[wb])
        xs = [S.sb([128, NCH, nt], F32, f"x{i}") for i in range(2)]
        os_ = [S.sb([128, NCH, nt], BF16, f"o{i}") for i in range(2)]
        y = S.sb([128, NCH, nt], F32, "y")
        sq = [S.sb([128, nt], BF16, f"sq{i}") for i in range(2)]
        rstd = S.sb([128, nt], F32, "rstd")
        tmp = [S.sb([128, nt], F32, f"tmp{i}") for i in range(2)]
        pstat = S.ps([128, 512], F32, "pstat")
        py = [S.ps([128, 512], F32, f"py{i}") for i in range(2)]
        xT_v = xT.rearrange("(c p) t -> p c t", p=128)
        oT_v = oT.rearrange("(c p) t -> p c t", p=128)
        yT_v = yT.rearrange("(c p) t -> p c t", p=128)
        toks = []

        def load(i):
            for c0 in range(0, NCH, 2):
                S.dma("sp", xs[i % 2][:, c0:c0 + 2, :], xT_v[:, c0:c0 + 2, i * nt:(i + 1) * nt], writes=[xs[i % 2]])
            for c0 in range(0, NCH, 4):
                S.dma("sp", os_[i % 2][:, c0:c0 + 4, :], oT_v[:, c0:c0 + 4, i * nt:(i + 1) * nt], writes=[os_[i % 2]])
        load(0)
        for i in range(ntiles):
            x, ob = xs[i % 2], os_[i % 2]
            if i + 1 < ntiles:
                load(i + 1)
            for m in range(NCH):
                p = py[m % 2]
                for c in range(NCH):
                    S.op("pe", lambda e, p=p, c=c, m=m, ob=ob: e.matmul(p[:, 0:nt], lhsT=wb[:, c, m * 128:(m + 1) * 128], rhs=ob[:, c, :], start=(c == 0), stop=(c == NCH - 1)),
                         reads=[wb, ob], writes=[p])
                S.op("act", lambda e, p=p, m=m: e.activation(out=y[:, m, :], in_=p[:, 0:nt], func=AF.Copy), reads=[p], writes=[y])

            def sqy(c):
                s = sq[c % 2]
                S.op("pool", lambda e, s=s, c=c: e.tensor_tensor(out=s[:], in0=y[:, c, :], in1=y[:, c, :], op=ALU.mult), reads=[y], writes=[s])
                return s, s[:]
            rms_stats(S, C, sqy, NCH, pstat, rstd, nt, D)
            for m in range(NCH):
                t = tmp[m % 2]
                S.op("dve", lambda e, m=m, t=t: e.scalar_tensor_tensor(out=t[:], in0=y[:, m, :], scalar=g[:, m:m + 1], in1=rstd[:], op0=ALU.mult, op1=ALU.mult),
                     reads=[y, g, rstd], writes=[t])
                S.op("pool", lambda e, m=m, t=t, x=x: e.tensor_tensor(out=x[:, m, :], in0=t[:], in1=x[:, m, :], op=ALU.add), reads=[t, x], writes=[x])
            for c0 in range(0, NCH, 2):
                toks.append(S.dma("sp", yT_v[:, c0:c0 + 2, i * nt:(i + 1) * nt], x[:, c0:c0 + 2, :], reads=[x]))
        S.final_wait("sp", toks)
        S.emit()
    return nc


def build_ple(T_tok, NT=512):
    nc = bass.Bass("TRN2", target_bir_lowering=False)
    xT = nc.dram_tensor("xT", [D, T_tok], F32, kind="ExternalInput").ap()
    pT = nc.dram_tensor("pT", [256, T_tok], F32, kind="ExternalInput").ap()
    w_p = nc.dram_tensor("w_p", [256, D], F32, kind="ExternalInput").ap()
    w_g = nc.dram_tensor("w_g", [D, D], F32, kind="ExternalInput").ap()
    ng = nc.dram_tensor("ng", [128, 16], F32, kind="ExternalInput").ap()
    yT = nc.dram_tensor("yT", [D, T_tok], F32, kind="ExternalOutput").ap()
    nt = NT
    ntiles = T_tok // nt
    with ExitStack() as es:
        S = Sched(nc, es)
        C = Consts(S)
        wg = S.sb([128, NCH, D], BF16, "wg")
        wp = S.sb([128, 2, D], BF16, "wp")
        g = S.sb([128, 16], F32, "gains")
        S.dma("sp", g[:], ng, writes=[g])
        wg_v = w_g.rearrange("(c p) f -> p c f", p=128)
        wp_v = w_p.rearrange("(c p) f -> p c f", p=128)
        for c in range(NCH):
            S.dma("pool", wg[:, c, :], wg_v[:, c, :], writes=[wg])
        for c in range(2):
            S.dma("pool", wp[:, c, :], wp_v[:, c, :], writes=[wp])
        xs = [S.sb([128, NCH, nt], F32, f"x{i}") for i in range(2)]
        pb = [S.sb([128, 2, nt], BF16, f"p{i}") for i in range(2)]
        xn = S.sb([128, NCH, nt], BF16, "xn")
        v = S.sb([128, NCH, nt], F32, "v")
        gate = [S.sb([128, nt], F32, f"gate{i}") for i in range(2)]
        sq = [S.sb([128, nt], BF16, f"sq{i}") for i in range(2)]
        rstd = S.sb([128, nt], F32, "rstd")
        rstd2 = S.sb([128, nt], F32, "rstd2")
        tmp = [S.sb([128, nt], F32, f"tmp{i}") for i in range(2)]
        pstat = S.ps([128, 512], F32, "pstat")
        pgp = [S.ps([128, 512], F32, f"pgp{i}") for i in range(2)]
        pep = [S.ps([128, 512], F32, f"pep{i}") for i in range(2)]
        xT_v = xT.rearrange("(c p) t -> p c t", p=128)
        pT_v = pT.rearrange("(c p) t -> p c t", p=128)
        yT_v = yT.rearrange("(c p) t -> p c t", p=128)
        toks = []

        def load(i):
            for c0 in range(0, NCH, 2):
                S.dma("sp", xs[i % 2][:, c0:c0 + 2, :], xT_v[:, c0:c0 + 2, i * nt:(i + 1) * nt], writes=[xs[i % 2]])
            S.dma("pool", pb[i % 2][:], pT_v[:, :, i * nt:(i + 1) * nt], writes=[pb[i % 2]])
        load(0)
        for i in range(ntiles):
            x, pp = xs[i % 2], pb[i % 2]
            if i + 1 < ntiles:
                load(i + 1)

            def sqf(c, x=x):
                s = sq[c % 2]
                S.op("pool", lambda e, s=s, c=c: e.tensor_tensor(out=s[:], in0=x[:, c, :], in1=x[:, c, :], op=ALU.mult), reads=[x], writes=[s])
                return s, s[:]
            rms_stats(S, C, sqf, NCH, pstat, rstd, nt, D)
            for c in range(NCH):
                S.op("dve", lambda e, c=c, x=x: e.scalar_tensor_tensor(out=xn[:, c, :], in0=x[:, c, :], scalar=g[:, c:c + 1], in1=rstd[:], op0=ALU.mult, op1=ALU.mult),
                     reads=[x, g, rstd], writes=[xn])
            for m in range(NCH):
                a, b = pgp[m % 2], pep[m % 2]
                for c in range(NCH):
                    S.op("pe", lambda e, a=a, c=c, m=m: e.matmul(a[:, 0:nt], lhsT=wg[:, c, m * 128:(m + 1) * 128], rhs=xn[:, c, :], start=(c == 0), stop=(c == NCH - 1)),
                         reads=[wg, xn], writes=[a])
                for c in range(2):
                    S.op("pe", lambda e, b=b, c=c, m=m, pp=pp: e.matmul(b[:, 0:nt], lhsT=wp[:, c, m * 128:(m + 1) * 128], rhs=pp[:, c, :], start=(c == 0), stop=(c == 1)),
                         reads=[wp, pp], writes=[b])
                gt_ = gate[m % 2]
                S.op("act", lambda e, a=a, gt_=gt_: e.activation(out=gt_[:], in_=a[:, 0:nt], func=AF.Sigmoid), reads=[a], writes=[gt_])
                S.op("dve", lambda e, b=b, gt_=gt_, m=m: e.tensor_tensor(out=v[:, m, :], in0=b[:, 0:nt], in1=gt_[:], op=ALU.mult), reads=[b, gt_], writes=[v])

            def sqv(c):
                s = sq[c % 2]
                S.op("pool", lambda e, s=s, c=c: e.tensor_tensor(out=s[:], in0=v[:, c, :], in1=v[:, c, :], op=ALU.mult), reads=[v], writes=[s])
                return s, s[:]
            rms_stats(S, C, sqv, NCH, pstat, rstd2, nt, D)
            for m in range(NCH):
                t = tmp[m % 2]
                S.op("dve", lambda e, m=m, t=t: e.scalar_tensor_tensor(out=t[:], in0=v[:, m, :], scalar=g[:, 8 + m:9 + m], in1=rstd2[:], op0=ALU.mult, op1=ALU.mult),
                     reads=[v, g, rstd2], writes=[t])
                S.op("pool", lambda e, m=m, t=t, x=x: e.tensor_tensor(out=x[:, m, :], in0=t[:], in1=x[:, m, :], op=ALU.add), reads=[t, x], writes=[x])
            for c0 in range(0, NCH, 2):
                toks.append(S.dma("sp", yT_v[:, c0:c0 + 2, i * nt:(i + 1) * nt], x[:, c0:c0 + 2, :], reads=[x]))
        S.final_wait("sp", toks)
        S.emit()
    return nc


def build_hgrnin(T_tok, NT=256):
    nc = bass.Bass("TRN2", target_bir_lowering=False)
    xT = nc.dram_tensor("xT", [D, T_tok], F32, kind="ExternalInput").ap()
    w = nc.dram_tensor("w", [D, 4096], F32, kind="ExternalInput").ap()
    ng = nc.dram_tensor("ng", [128, 8], F32, kind="ExternalInput").ap()
    qfT = nc.dram_tensor("qfT", [2048, T_tok], F32, kind="ExternalOutput").ap()
    iv = nc.dram_tensor("iv", [T_tok, D], BF16, kind="ExternalOutput").ap()
    gs = nc.dram_tensor("gs", [T_tok, D], F32, kind="ExternalOutput").ap()
    nt = NT
    ntiles = T_tok // nt
    nsub = nt // 128
    with ExitStack() as es:
        S = Sched(nc, es)
        C = Consts(S)
        wb = S.sb([128, NCH, 4096], BF16, "wb")
        g = S.sb([128, 8], F32, "gains")
        S.dma("sp", g[:], ng, writes=[g])
        w_v = w.rearrange("(c p) f -> p c f", p=128)
        for c in range(NCH):
            S.dma("pool", wb[:, c, :], w_v[:, c, :], writes=[wb])
        xs = [S.sb([128, NCH, nt], F32, f"x{i}") for i in range(2)]
        hn = S.sb([128, NCH, nt], BF16, "hn")
        sq = [S.sb([128, nt], BF16, f"sq{i}") for i in range(2)]
        rstd = S.sb([128, nt], F32, "rstd")
        ofm = [S.sb([128, 16, nt], F32, f"ofm{i}") for i in range(2)]
        oiv = [S.sb([128, nsub, D], BF16, f"oiv{i}") for i in range(2)]
        ogs = [S.sb([128, nsub, D], F32, f"ogs{i}") for i in range(2)]
        pstat = S.ps([128, 512], F32, "pstat")
        pa = [S.ps([128, 512], F32, f"pa{i}") for i in range(2)]
        pt = [S.ps([128, 512], F32, f"pt{i}") for i in range(2)]
        xT_v = xT.rearrange("(c p) t -> p c t", p=128)
        qf_v = qfT.rearrange("(c p) t -> p c t", p=128)
        iv_v = iv.rearrange("(s p) f -> p s f", p=128)
        gs_v = gs.rearrange("(s p) f -> p s f", p=128)
        toks = []

        def load(i):
            S.dma("sp", xs[i % 2][:], xT_v[:, :, i * nt:(i + 1) * nt], writes=[xs[i % 2]])
        load(0)
        for i in range(ntiles):
            x = xs[i % 2]
            if i + 1 < ntiles:
                load(i + 1)

            def sqf(c, x=x):
                s = sq[c % 2]
                S.op("pool", lambda e, s=s, c=c: e.tensor_tensor(out=s[:], in0=x[:, c, :], in1=x[:, c, :], op=ALU.mult), reads=[x], writes=[s])
                return s, s[:]
            rms_stats(S, C, sqf, NCH, pstat, rstd, nt, D)
            for c in range(NCH):
                S.op("dve", lambda e, c=c, x=x: e.scalar_tensor_tensor(out=hn[:, c, :], in0=x[:, c, :], scalar=g[:, c:c + 1], in1=rstd[:], op0=ALU.mult, op1=ALU.mult),
                     reads=[x, g, rstd], writes=[hn])
            of = ofm[i % 2]
            for j in range(16):
                a = pa[j % 2]
                for c in range(NCH):
                    S.op("pe", lambda e, a=a, c=c, j=j: e.matmul(a[:, 0:nt], lhsT=wb[:, c, j * 128:(j + 1) * 128], rhs=hn[:, c, :], start=(c == 0), stop=(c == NCH - 1)),
                         reads=[wb, hn], writes=[a])
                fn = AF.Silu if j < 8 else AF.Copy
                S.op("act", lambda e, a=a, j=j, of=of, fn=fn: e.activation(out=of[:, j, :], in_=a[:, 0:nt], func=fn), reads=[a], writes=[of])
            oi, og = oiv[i % 2], ogs[i % 2]
            for s in range(nsub):
                for hf in range(4):
                    p = pt[hf % 2]
                    for c in range(NCH):
                        S.op("pe", lambda e, p=p, c=c, s=s, hf=hf: e.matmul(p[:, 0:512], lhsT=hn[:, c, s * 128:(s + 1) * 128], rhs=wb[:, c, 2048 + hf * 512:2048 + (hf + 1) * 512], start=(c == 0), stop=(c == NCH - 1)),
                             reads=[wb, hn], writes=[p])
                    if hf < 2:
                        S.op("dve", lambda e, p=p, s=s, hf=hf, oi=oi: e.tensor_copy(out=oi[:, s, hf * 512:(hf + 1) * 512], in_=p[:, 0:512]), reads=[p], writes=[oi])
                    else:
                        S.op("act", lambda e, p=p, s=s, hf=hf, og=og: e.activation(out=og[:, s, (hf - 2) * 512:(hf - 1) * 512], in_=p[:, 0:512], func=AF.Silu), reads=[p], writes=[og])
            sl = slice(i * nt, (i + 1) * nt)
            toks.append(S.dma("sp", qf_v[:, 0:8, sl], of[:, 0:8, :], reads=[of]))
            toks.append(S.dma("sp", qf_v[:, 8:16, sl], of[:, 8:16, :], reads=[of]))
            toks.append(S.dma("sp", iv_v[:, i * nsub:(i + 1) * nsub, :], oi[:], reads=[oi]))
            toks.append(S.dma("sp", gs_v[:, i * nsub:(i + 1) * nsub, :], og[:], reads=[og]))
        S.final_wait("sp", toks)
        S.emit()
    return nc


def build_hgrn(S_len, TB=1024):
    CH = 64
    nch = TB // CH
    nblk = S_len // TB
    nc = bass.Bass("TRN2", target_bir_lowering=False)
    def din(name, shape, dt):
        return nc.dram_tensor(name, list(shape), dt, kind="ExternalInput").ap()
    qT = din("qT", [256, S_len], F32)
    fT = din("fT", [256, S_len], F32)
    v = din("v", [S_len, 256], BF16)
    gs = din("gs", [S_len, 256], F32)
    lg = din("lg", [128, 2, 2], F32)
    gn = din("gn", [64, 128], F32)
    smask = din("smask", [128, TB], F32)
    cmask = din("cmask", [64, 64], F32)
    c_ident = din("ident", [128, 128], BF16)
    o = nc.dram_tensor("o", [S_len, 256], BF16, kind="ExternalOutput").ap()
    with ExitStack() as es:
        S = Sched(nc, es)
        ident = S.sb([128, 128], BF16, "ident_sb")
        sm = S.sb([128, TB], F32, "smask_sb")
        cm = S.sb([64, 64], F32, "cmask_sb")
        gnt = S.sb([64, 128], F32, "gn_sb")
        lgt = S.sb([128, 2, 2], F32, "lg_sb")
        lb = S.sb([128, 2], F32, "lb")
        oml = S.sb([128, 2], F32, "oml")
        noml = S.sb([128, 2], F32, "noml")
        for (t, src) in ((ident, c_ident), (sm, smask), (cm, cmask), (gnt, gn), (lgt, lg)):
            S.dma("sp", t[:], src, writes=[t])
        S.op("dve", lambda e: e.tensor_tensor(out=lb[:], in0=lgt[:, 1, :], in1=lgt[:, 0, :], op=ALU.subtract), reads=[lgt], writes=[lb])
        S.op("act", lambda e: e.activation(out=lb[:], in_=lb[:], func=AF.Sigmoid), reads=[lb], writes=[lb])
        S.op("dve", lambda e: e.tensor_scalar(out=oml[:], in0=lb[:], scalar1=-1.0, scalar2=1.0, op0=ALU.mult, op1=ALU.add), reads=[lb], writes=[oml])
        S.op("dve", lambda e: e.tensor_scalar(out=noml[:], in0=oml[:], scalar1=-1.0, scalar2=None, op0=ALU.mult), reads=[oml], writes=[noml])
        def mk(name, shape, dt):
            return [S.sb(shape, dt, f"{name}{h}") for h in range(2)]
        fr = mk("fr", [128, TB], F32)
        qf = mk("qf", [128, TB], F32)
        t1 = mk("t1", [128, TB], F32)
        t2 = mk("t2", [128, TB], F32)
        t3 = mk("t3", [128, TB], F32)
        cum = mk("cum", [128, TB], F32)
        Qh = mk("Qh", [128, TB], BF16)
        Qt = mk("Qt", [128, TB], BF16)
        Kh = mk("Kh", [128, TB], BF16)
        Kt = [[S.sb([128, TB], BF16, f"Kt{h}_{i}") for i in range(4)] for h in range(2)]
        aa = mk("aa", [128, nch], F32)
        vb = mk("vb", [64, nch, 128], BF16)
        gsb = mk("gsb", [64, nch, 128], F32)
        ob = mk("ob", [64, nch, 128], F32)
        osq = mk("osq", [64, nch, 128], F32)
        ofin = mk("ofin", [64, nch, 128], BF16)
        ssum = mk("ssum", [64, nch], F32)
        state = mk("state", [128, 128], F32)
        sref = [[S.sb([128, 128], BF16, f"sref{h}_{i}") for i in range(2)] for h in range(2)]
        ATm = [[S.sb([64, 64], BF16, f"ATm{h}_{i}") for i in range(2)] for h in range(2)]
        Ktok = [[S.sb([64, 128], BF16, f"Ktok{h}_{i}") for i in range(2)] for h in range(2)]
        pA = [S.ps([128, 512], F32, f"pA{h}") for h in range(2)]
        pP = [S.ps([128, 512], F32, f"pP{h}") for h in range(2)]
        pO = [S.ps([128, 512], F32, f"pO{h}") for h in range(2)]
        for h in range(2):
            S.op("pool", lambda e, h=h: e.memset(state[h][:], 0.0), writes=[state[h]])
        v_v = v.rearrange("(c p) e -> p c e", p=CH)
        gs_v = gs.rearrange("(c p) e -> p c e", p=CH)
        o_v = o.rearrange("(c p) e -> p c e", p=CH)
        out_toks = []
        CLAMP = 1e30
        for b in range(nblk):
            tsl = slice(b * TB, (b + 1) * TB)
            csl = slice(b * nch, (b + 1) * nch)
            for h in range(2):
                hs = slice(h * 128, (h + 1) * 128)
                S.dma("sp", fr[h][:], fT[hs, tsl], writes=[fr[h]])
                S.dma("sp", qf[h][:], qT[hs, tsl], writes=[qf[h]])
                S.dma("sp", vb[h][:], v_v[:, csl, hs], writes=[vb[h]])
                S.dma("sp", gsb[h][:], gs_v[:, csl, hs], writes=[gsb[h]])
            for h in range(2):
                f_, q_, a1, a2, a3, cu = fr[h], qf[h], t1[h], t2[h], t3[h], cum[h]
                S.op("act", lambda e, f_=f_: e.activation(out=f_[:], in_=f_[:], func=AF.Sigmoid), reads=[f_], writes=[f_])
                S.op("dve", lambda e, f_=f_, a1=a1, h=h: e.tensor_scalar(out=a1[:], in0=f_[:], scalar1=oml[:, h:h + 1], scalar2=lb[:, h:h + 1], op0=ALU.mult, op1=ALU.add), reads=[f_, oml, lb], writes=[a1])
                S.op("pool", lambda e, f_=f_, a2=a2, h=h: e.tensor_scalar(out=a2[:], in0=f_[:], scalar1=noml[:, h:h + 1], scalar2=oml[:, h:h + 1], op0=ALU.mult, op1=ALU.add), reads=[f_, oml, noml], writes=[a2])
                S.op("act", lambda e, a1=a1: e.activation(out=a1[:], in_=a1[:], func=AF.Ln), reads=[a1], writes=[a1])
                S.op("dve", lambda e, a1=a1, cu=cu: e.tensor_tensor_scan(out=cu[:], data0=sm[:], data1=a1[:], initial=0.0, op0=ALU.mult, op1=ALU.add), reads=[sm, a1], writes=[cu])
                cu_v = cu[:].rearrange("p (c s) -> p c s", s=CH)
                cu_v16 = cu[:].rearrange("p (c s) -> p c s", s=16)
                S.op("act", lambda e, a1=a1, cu=cu: e.activation(out=a1[:], in_=cu[:], func=AF.Exp), reads=[cu], writes=[a1])
                S.op("pool", lambda e, q_=q_, a1=a1, h=h: e.tensor_tensor(out=Qh[h][:], in0=q_[:], in1=a1[:], op=ALU.mult), reads=[q_, a1], writes=[Qh[h]])
                S.op("dve", lambda e, a3=a3, cu_v16=cu_v16: e.tensor_tensor(out=a3[:].rearrange("p (c s) -> p c s", s=16), in0=cu_v16, in1=cu_v16[:, :, 0:1].to_broadcast([128, TB // 16, 16]), op=ALU.subtract), reads=[cu], writes=[a3])
                S.op("act", lambda e, a3=a3: e.activation(out=a3[:], in_=a3[:], func=AF.Exp), reads=[a3], writes=[a3])
                S.op("pool", lambda e, q_=q_, a3=a3, h=h: e.tensor_tensor(out=Qt[h][:], in0=q_[:], in1=a3[:], op=ALU.mult), reads=[q_, a3], writes=[Qt[h]])
                S.op("dve", lambda e, a1=a1, cu_v=cu_v: e.tensor_tensor(out=a1[:].rearrange("p (c s) -> p c s", s=CH), in0=cu_v, in1=cu_v[:, :, CH - 1:CH].to_broadcast([128, nch, CH]), op=ALU.subtract), reads=[cu], writes=[a1])
                S.op("act", lambda e, a1=a1: e.activation(out=a1[:], in_=a1[:], func=AF.Exp, scale=-1.0), reads=[a1], writes=[a1])
                S.op("pool", lambda e, a1=a1, a2=a2, h=h: e.tensor_tensor(out=Kh[h][:], in0=a2[:], in1=a1[:], op=ALU.mult), reads=[a1, a2], writes=[Kh[h]])
                for i in range(4):
                    w_ = a3 if i % 2 == 0 else a1
                    S.op("dve", lambda e, w_=w_, cu_v=cu_v, i=i: e.tensor_tensor(out=w_[:].rearrange("p (c s) -> p c s", s=CH), in0=cu_v, in1=cu_v[:, :, 16 * i:16 * i + 1].to_broadcast([128, nch, CH]), op=ALU.subtract), reads=[cu], writes=[w_])
                    S.op("pool", lambda e, w_=w_: e.tensor_scalar(out=w_[:], in0=w_[:], scalar1=-80.0, scalar2=None, op0=ALU.max), reads=[w_], writes=[w_])
                    S.op("act", lambda e, w_=w_: e.activation(out=w_[:], in_=w_[:], func=AF.Exp, scale=-1.0), reads=[w_], writes=[w_])
                    S.op("dve", lambda e, w_=w_, a2=a2, h=h, i=i: e.tensor_tensor(out=Kt[h][i][:], in0=w_[:], in1=a2[:], op=ALU.mult), reads=[w_, a2], writes=[Kt[h][i]])
                S.op("act", lambda e, cu_v=cu_v, h=h: e.activation(out=aa[h][:], in_=cu_v[:, :, CH - 1], func=AF.Exp), reads=[cu], writes=[aa[h]])
            for c in range(nch):
                for h in range(2):
                    cs_ = slice(c * CH, (c + 1) * CH)
                    i2 = c % 2
                    for i in range(4):
                        S.op("pe", lambda e, h=h, cs_=cs_, i=i, c=c: e.matmul(pA[h][0:64, 16 * i:16 * i + 16], lhsT=Kt[h][i][:, cs_], rhs=Qt[h][:, c * CH + 16 * i:c * CH + 16 * i + 16], start=True, stop=True),
                             reads=[Kt[h][i], Qt[h]], writes=[pA[h]])
                    atm = ATm[h][i2]
                    S.op("dve", lambda e, h=h, atm=atm: e.tensor_tensor(out=atm[:], in0=pA[h][0:64, 0:64], in1=cm[:], op=ALU.mult), reads=[pA[h], cm], writes=[atm])
                    ktv = pA[h][0:64, 256:320].bitcast(BF16)
                    S.op("pe", lambda e, h=h, cs_=cs_, ktv=ktv: e.transpose(out=ktv, in_=Kh[h][:, cs_], identity=ident[:]), reads=[Kh[h], ident], writes=[pA[h]])
                    kt_ = Ktok[h][i2]
                    S.op("act", lambda e, ktv=ktv, kt_=kt_, h=h: e.activation(out=kt_[:], in_=ktv, func=AF.Copy), reads=[pA[h]], writes=[kt_])
                    sr = sref[h][i2]
                    S.op("act", lambda e, h=h, sr=sr: e.activation(out=sr[:], in_=state[h][:], func=AF.Copy), reads=[state[h]], writes=[sr])
                    S.op("pe", lambda e, h=h, atm=atm, c=c: e.matmul(pO[h][0:64, 0:128], lhsT=atm[:], rhs=vb[h][:, c, :], start=True, stop=False), reads=[atm, vb[h]], writes=[pO[h]])
                    S.op("pe", lambda e, h=h, sr=sr, cs_=cs_: e.matmul(pO[h][0:64, 0:128], lhsT=Qh[h][:, cs_], rhs=sr[:], start=False, stop=True), reads=[Qh[h], sr], writes=[pO[h]])
                    S.op("act", lambda e, h=h, c=c: e.activation(out=ob[h][:, c, :], in_=pO[h][0:64, 0:128], func=AF.Copy), reads=[pO[h]], writes=[ob[h]])
                    S.op("pe", lambda e, h=h, kt_=kt_, c=c: e.matmul(pP[h][:, 0:128], lhsT=kt_[:], rhs=vb[h][:, c, :], start=True, stop=True), reads=[kt_, vb[h]], writes=[pP[h]])
                    S.op("dve", lambda e, h=h, c=c: e.scalar_tensor_tensor(out=state[h][:], in0=state[h][:], scalar=aa[h][:, c:c + 1], in1=pP[h][:, 0:128], op0=ALU.mult, op1=ALU.add), reads=[state[h], aa[h], pP[h]], writes=[state[h]])
            for h in range(2):
                hs = slice(h * 128, (h + 1) * 128)
                S.op("pool", lambda e, h=h: e.tensor_tensor(out=osq[h][:], in0=ob[h][:], in1=ob[h][:], op=ALU.mult), reads=[ob[h]], writes=[osq[h]])
                S.op("dve", lambda e, h=h: e.tensor_reduce(out=ssum[h][:], in_=osq[h][:], axis=AX.X, op=ALU.add), reads=[osq[h]], writes=[ssum[h]])
                S.op("act", lambda e, h=h: e.activation(out=ssum[h][:], in_=ssum[h][:], func=AF.Sqrt, scale=1.0 / 128, bias=EPS), reads=[ssum[h]], writes=[ssum[h]])
                S.op("dve", lambda e, h=h: e.reciprocal(out=ssum[h][:], in_=ssum[h][:]), reads=[ssum[h]], writes=[ssum[h]])
                S.op("dve", lambda e, h=h: e.tensor_tensor(out=ob[h][:], in0=ob[h][:], in1=ssum[h][:].unsqueeze(2).to_broadcast([64, nch, 128]), op=ALU.mult), reads=[ob[h], ssum[h]], writes=[ob[h]])
                S.op("pool", lambda e, h=h: e.tensor_tensor(out=osq[h][:], in0=gsb[h][:], in1=gnt[:].unsqueeze(1).to_broadcast([64, nch, 128]), op=ALU.mult), reads=[gsb[h], gnt], writes=[osq[h]])
                S.op("dve", lambda e, h=h: e.tensor_tensor(out=ofin[h][:], in0=ob[h][:], in1=osq[h][:], op=ALU.mult), reads=[ob[h], osq[h]], writes=[ofin[h]])
                out_toks.append(S.dma("sp", o_v[:, csl, hs], ofin[h][:], reads=[ofin[h]]))
        S.final_wait("sp", out_toks)
        S.emit()
    return nc


def hgrn_consts(TB=1024):
    sm = np.ones((128, TB), np.float32)
    sm[:, ::64] = 0.0
    s = np.arange(64)[:, None]
    t = np.arange(64)[None, :]
    return {"smask": sm, "cmask": (s <= t).astype(np.float32), "ident": np.eye(128, dtype=np.float32).astype(NPBF)}


B_, S_, TPC = 2, 16384, 4096
_NC_CACHE = {}


def _prog(key, fn):
    if key not in _NC_CACHE:
        _NC_CACHE[key] = fn()
    return _NC_CACHE[key]


def _run(nc, in_maps):
    res = run_bass_kernel_spmd(nc, in_maps, core_ids=list(range(NCORES)))
    return res.results


def kernel(x, p, norm_gains, ffn_w_in, ffn_w_out, ple_w_in, ple_w_gate, nsa_w_in, nsa_w_out,
           nsa_cmp_pos, nsa_cmp_w1, nsa_cmp_w2, hgrn_w_in, hgrn_w_out, hgrn_norm, hgrn_lb_logits):
    f32 = np.float32
    x = np.asarray(x, f32)
    p = np.asarray(p, f32)
    norm_gains = np.asarray(norm_gains, f32)
    ffn_w_in = np.asarray(ffn_w_in, f32)
    ffn_w_out = np.asarray(ffn_w_out, f32)
    ple_w_in = np.asarray(ple_w_in, f32)
    ple_w_gate = np.asarray(ple_w_gate, f32)
    cores = [(r // 4, (r % 4) * TPC) for r in range(NCORES)]
    xT = [np.ascontiguousarray(x[b, t0:t0 + TPC].T) for (b, t0) in cores]

    def ffn(xT, layer, k):
        nc = _prog(("ffn",), lambda: build_ffn(TPC, 256))
        w_in = np.ascontiguousarray(ffn_w_in[layer, k])
        w_out = np.ascontiguousarray(ffn_w_out[layer, k])
        ng = ng_layout(norm_gains[layer], [4 * k, 4 * k + 1])
        r = _run(nc, [{"xT": xT[i], "w_in": w_in, "w_out": w_out, "ng": ng} for i in range(NCORES)])
        return [q["yT"] for q in r]

    def outp(xT, oT, w, layer):
        nc = _prog(("out",), lambda: build_out(TPC, 256))
        ng = ng_layout(norm_gains[layer], [3])
        w = np.ascontiguousarray(np.asarray(w, f32))
        r = _run(nc, [{"xT": xT[i], "oT": oT[i], "w": w, "ng": ng} for i in range(NCORES)])
        return [q["yT"] for q in r]

    def ple(xT, layer):
        nc = _prog(("ple",), lambda: build_ple(TPC, 256))
        ng = ng_layout(norm_gains[layer], [6, 7])
        wp = np.ascontiguousarray(ple_w_in[layer])
        wg = np.ascontiguousarray(ple_w_gate[layer])
        r = _run(nc, [{"xT": xT[i], "pT": np.ascontiguousarray(p[layer, b, t0:t0 + TPC].T), "w_p": wp, "w_g": wg, "ng": ng}
                      for i, (b, t0) in enumerate(cores)])
        return [q["yT"] for q in r]

    def to_rows(o_cores):
        res = []
        for (b, t0) in cores:
            blk = np.concatenate([o_cores[b * 4 + g][t0:t0 + TPC] for g in range(4)], axis=1)
            res.append(np.ascontiguousarray(blk.T))
        return res

    xT = ffn(xT, 0, 0)
    nc = _prog(("nsain",), lambda: build_nsain(TPC, 256))
    w_fm, w_tm = nsa_w_layout(np.asarray(nsa_w_in, f32)[0])
    ng = ng_layout(norm_gains[0], [2])
    r = _run(nc, [{"xT": xT[i], "w_fm": w_fm, "w_tm": w_tm, "ng": ng, "cs": rope_tables(np.arange(t0, t0 + TPC))}
                  for i, (b, t0) in enumerate(cores)])
    consts = nsa_consts(S_)
    in_maps = []
    for b in range(B_):
        qk = np.concatenate([r[b * 4 + k]["qkT"] for k in range(4)], axis=1)
        vc = np.concatenate([r[b * 4 + k]["vcT"] for k in range(4)], axis=1)
        vsw = np.concatenate([r[b * 4 + k]["vsw"] for k in range(4)], axis=0)
        gt = np.concatenate([r[b * 4 + k]["gates"] for k in range(4)], axis=0)
        for g in range(4):
            in_maps.append(nsa_attn_inputs(qk, vc, vsw, gt, np.asarray(nsa_cmp_pos, f32)[0], np.asarray(nsa_cmp_w1, f32)[0],
                                           np.asarray(nsa_cmp_w2, f32)[0], g, consts))
    nc = _prog(("attn",), lambda: build_nsa_attn(S_))
    r = _run(nc, in_maps)
    oT = to_rows([q["o"] for q in r])
    xT = outp(xT, oT, np.asarray(nsa_w_out, f32)[0], 0)
    xT = ffn(xT, 0, 1)
    xT = ple(xT, 0)
    xT = ffn(xT, 1, 0)
    nc = _prog(("hgrnin",), lambda: build_hgrnin(TPC, 256))
    ng = ng_layout(norm_gains[1], [2])
    w = np.ascontiguousarray(np.asarray(hgrn_w_in, f32)[0])
    r = _run(nc, [{"xT": xT[i], "w": w, "ng": ng} for i in range(NCORES)])
    hc = hgrn_consts(1024)
    lgn = np.asarray(hgrn_lb_logits, f32)
    gn = np.ascontiguousarray(np.tile(np.asarray(hgrn_norm, f32)[0][None, :], (64, 1)))
    in_maps = []
    for b in range(B_):
        qf = np.concatenate([r[b * 4 + k]["qfT"] for k in range(4)], axis=1)
        iv = np.concatenate([r[b * 4 + k]["iv"] for k in range(4)], axis=0)
        gs = np.concatenate([r[b * 4 + k]["gs"] for k in range(4)], axis=0)
        for r2 in range(4):
            hs = slice(256 * r2, 256 * r2 + 256)
            d = {"qT": np.ascontiguousarray(qf[hs]), "fT": np.ascontiguousarray(qf[1024 + 256 * r2:1024 + 256 * r2 + 256]),
                 "v": np.ascontiguousarray(iv[:, hs]), "gs": np.ascontiguousarray(gs[:, hs]),
                 "lg": np.ascontiguousarray(lgn[:, hs].reshape(2, 2, 128).transpose(2, 0, 1)), "gn": gn}
            d.update(hc)
            in_maps.append(d)
    nc = _prog(("hgrn",), lambda: build_hgrn(S_, 1024))
    r = _run(nc, in_maps)
    oT = to_rows([q["o"] for q in r])
    xT = outp(xT, oT, np.asarray(hgrn_w_out, f32)[0], 1)
    xT = ffn(xT, 1, 1)
    xT = ple(xT, 1)
    out = np.empty((B_, S_, D), f32)
    for i, (b, t0) in enumerate(cores):
        out[b, t0:t0 + TPC] = xT[i].T
    return out
```

```python
import numpy as np
import ml_dtypes
import concourse.bass as bass
import concourse.mybir as mybir
from concourse.bass_utils import run_bass_kernel_spmd
from contextlib import ExitStack

F32 = mybir.dt.float32
BF16 = mybir.dt.bfloat16
AF = mybir.ActivationFunctionType
ALU = mybir.AluOpType
AX = mybir.AxisListType
NPBF = ml_dtypes.bfloat16

D = 1024
DFF = 2816
NCH = 8
NFC = DFF // 128
EPS = 1e-6
SEM_MAX = 30000
CC_INC = 1
NCORES = 8


class T:
    def __init__(self, h, name):
        self.h = h
        self.name = name
        self.w = None
        self.rs = {}
        self.dsem = None
        self.dcnt = 0

    def __getitem__(self, k):
        return self.h[k]


class Sched:
    ENGS = ("pe", "act", "dve", "pool", "sp")

    def __init__(self, nc, es):
        self.nc = nc
        self.es = es
        self.streams = {e: [] for e in self.ENGS}
        self.sem = {}
        self.cnt = {}
        self.nsem = 0
        for e in self.ENGS:
            self._newsem(e)
        self.waited = {}
        self.ntile = 0
        self.dsem = None
        self.dcnt = 0
        self.pes = es
        self.pool = {}
        self.phase_tiles = []
        self.phase_dtoks = {}

    def _mksem(self, name):
        self.nsem += 1
        return self.es.enter_context(self.nc.semaphore(name))

    def _newsem(self, e):
        self.sem[e] = self._mksem(f"s_{e}_{self.nsem}")
        self.cnt[e] = 0

    def sb(self, shape, dt, name=None):
        self.ntile += 1
        name = f"sb{self.ntile}_" + (name or "t")
        h = self.pes.enter_context(self.nc.sbuf_tensor(name, list(shape), dt))
        t = T(h, name)
        t.scoped = True
        self.phase_tiles.append(t)
        return t

    def ps(self, shape, dt=F32, name=None):
        self.ntile += 1
        name = f"ps{self.ntile}_" + (name or "p")
        h = self.pes.enter_context(self.nc.psum_tensor(name, list(shape), dt))
        t = T(h, name)
        t.scoped = True
        self.phase_tiles.append(t)
        return t

    def dram(self, name, shape, dt):
        h = self.nc.dram_tensor(name, list(shape), dt)
        return T(h.ap(), name)

    def _deps(self, eng, reads, writes, same_eng_sync):
        deps = []
        for t in reads:
            if t.w is not None:
                deps.append(t.w)
        for t in writes:
            if t.w is not None:
                deps.append(t.w)
            deps.extend(t.rs.values())
        best = {}
        for (sem, val, src) in deps:
            if src == eng and not same_eng_sync:
                continue
            if id(sem) not in best or best[id(sem)][1] < val:
                best[id(sem)] = (sem, val)
        waits = []
        for (sem, val) in best.values():
            key = (eng, id(sem))
            if self.waited.get(key, 0) >= val:
                continue
            self.waited[key] = val
            waits.append((sem, val))
        return waits

    def op(self, eng, fn, reads=(), writes=(), sync_same=None):
        if sync_same is None:
            sync_same = eng != "pe"
        waits = self._deps(eng, reads, writes, sync_same)
        if self.cnt[eng] >= SEM_MAX:
            self._newsem(eng)
        self.cnt[eng] += 1
        tok = (self.sem[eng], self.cnt[eng], eng)
        self.streams[eng].append((waits, fn, (self.sem[eng], 1)))
        for t in reads:
            t.rs[id(tok[0])] = tok
        for t in writes:
            t.w = tok
            t.rs = {}
        return tok

    def dma(self, q, out, in_, reads=(), writes=(), semt=None, **kw):
        waits = self._deps(q, reads, writes, True)
        t = semt if semt is not None else (list(writes) + list(reads))[0]
        kind = "sw" if q == "pool" else "hw"
        if t.dsem is None:
            t.dsem = {}
        ds = t.dsem.get(kind)
        if ds is None or ds[1] >= SEM_MAX:
            pl = self.pool.setdefault(kind, [])
            if getattr(t, "scoped", False) and pl and pl[-1][1] < SEM_MAX:
                ds = pl.pop()
            else:
                ds = [self._mksem(f"d{kind}_{self.nsem}"), 0]
            t.dsem[kind] = ds
        ds[1] += 16
        tok = (ds[0], ds[1], "dma")
        sem = ds[0]
        self.phase_dtoks[id(sem)] = tok
        self.streams[q].append((waits, lambda e: e.dma_start(out=out, in_=in_, **kw), (sem, 16)))
        for r in reads:
            r.rs[id(tok[0])] = tok
        for w in writes:
            w.w = tok
            w.rs = {}
        return tok

    def collective(self, kind, groups, in_ap, out_ap, reads=(), writes=()):
        waits = self._deps("pool", reads, writes, True)
        t = list(writes)[0]
        if t.dsem is None:
            t.dsem = {}
        ds = t.dsem.get("cc")
        if ds is None:
            ds = [self._mksem(f"c_{self.nsem}"), 0]
            t.dsem["cc"] = ds
        ds[1] += CC_INC
        tok = (ds[0], ds[1], "dma")
        sem = ds[0]
        self.phase_dtoks[id(sem)] = tok
        self.streams["pool"].append((waits, lambda e: e.collective_compute(kind, ALU.bypass, replica_groups=groups, ins=[in_ap.opt()], outs=[out_ap.opt()]), (sem, CC_INC)))
        for r in reads:
            r.rs[id(tok[0])] = tok
        for w in writes:
            w.w = tok
            w.rs = {}
        return tok

    def begin_phase(self):
        self.pes = ExitStack()
        self.phase_tiles = []
        self.phase_dtoks = {}

    def end_phase(self):
        toks = [(self.sem[e], self.cnt[e], e) for e in self.ENGS if self.cnt[e] > 0]
        toks += list(self.phase_dtoks.values())
        for e in self.ENGS:
            ws = []
            for (sm, v, src) in toks:
                key = (e, id(sm))
                if src == e or self.waited.get(key, 0) >= v:
                    continue
                self.waited[key] = v
                ws.append((sm, v))
            self.streams[e].append((ws, None, None))
        self.emit()
        for t in self.phase_tiles:
            if t.dsem:
                for kind, ds in t.dsem.items():
                    self.pool.setdefault(kind, []).append(ds)
                t.dsem = None
        self.pes.close()
        self.pes = self.es
        self.phase_tiles = []
        self.phase_dtoks = {}

    def final_wait(self, eng, toks):
        best = {}
        for (s, v, _) in toks:
            if id(s) not in best or best[id(s)][1] < v:
                best[id(s)] = (s, v)
        self.streams[eng].append((list(best.values()), None, None))

    def emit(self):
        nc = self.nc
        streams = self.streams
        with nc.Block() as block:
            def run(e, name):
                for (waits, fn, inc) in streams[name]:
                    for (sem, val) in waits:
                        e.wait_ge(sem, val)
                    if fn is not None:
                        ins = fn(e)
                        if inc is not None:
                            ins.then_inc(inc[0], inc[1])

            @block.tensor
            def _(e):
                run(e, "pe")

            @block.scalar
            def _(e):
                run(e, "act")

            @block.vector
            def _(e):
                run(e, "dve")

            @block.gpsimd
            def _(e):
                run(e, "pool")

            @block.sync
            def _(e):
                run(e, "sp")
        self.streams = {e: [] for e in self.ENGS}


def _io(nc, ctx, name, shape, dt, kind):
    if ctx is not None and name in ctx[2]:
        return ctx[2][name]
    return nc.dram_tensor(name, list(shape), dt, kind=kind).ap()


class _SchedCtx:
    def __init__(self, nc, ctx):
        self.nc, self.ctx = nc, ctx

    def __enter__(self):
        if self.ctx is None:
            self.es = ExitStack()
            self.es.__enter__()
            S = Sched(self.nc, self.es)
            return S, Consts(S), True
        S = self.ctx[0]
        S.begin_phase()
        return S, Consts(S), False

    def __exit__(self, *a):
        if self.ctx is None:
            return self.es.__exit__(*a)
        if a[0] is None:
            self.ctx[0].end_phase()
        return False


class Consts:
    def __init__(self, S):
        self.ones = S.sb([128, 128], BF16, "c_ones")
        S.op("pool", lambda e: e.memset(self.ones[:], 1.0), writes=[self.ones])


def rms_stats(S, C, sq_tiles_fn, nchunks, pstat, rstd, nt, dim):
    for c in range(nchunks):
        t, ap = sq_tiles_fn(c)
        S.op("pe", lambda e, ap=ap, c=c: e.matmul(pstat[:, 0:nt], lhsT=C.ones[:], rhs=ap, start=(c == 0), stop=(c == nchunks - 1)),
             reads=[C.ones, t], writes=[pstat])
    S.op("act", lambda e: e.activation(out=rstd[:, 0:nt], in_=pstat[:, 0:nt], func=AF.Sqrt, scale=1.0 / dim, bias=EPS),
         reads=[pstat], writes=[rstd])
    S.op("dve", lambda e: e.reciprocal(out=rstd[:, 0:nt], in_=rstd[:, 0:nt]), reads=[rstd], writes=[rstd])


def build_ffn(T_tok, NT=256, ctx=None):
    nc = bass.Bass("TRN2", target_bir_lowering=False) if ctx is None else ctx[0].nc
    xT = _io(nc, ctx, "xT", [D, T_tok], F32, "ExternalInput")
    w_in = _io(nc, ctx, "w_in", [D, 2 * DFF], F32, "ExternalInput")
    w_out = _io(nc, ctx, "w_out", [DFF, D], F32, "ExternalInput")
    ng = _io(nc, ctx, "ng", [128, 16], F32, "ExternalInput")
    yT = _io(nc, ctx, "yT", [D, T_tok], F32, "ExternalOutput")
    with _SchedCtx(nc, ctx) as (S, C, own):
        toks = ffn_phase(S, C, xT, yT, w_in, w_out, ng, T_tok, NT)
        S.final_wait("sp", toks)
        if own:
            S.emit()
    return nc


def ffn_phase(S, C, xT, yT, w_in, w_out, ng, T_tok, NT):
    nt = NT
    ntiles = T_tok // nt
    win = S.sb([128, NCH, 2 * DFF], BF16, "win")
    wout = S.sb([128, NFC, D], BF16, "wout")
    g = S.sb([128, 16], F32, "gains")
    S.dma("sp", g[:], ng, writes=[g])
    w_in_v = w_in.rearrange("(c p) f -> p c f", p=128)
    w_out_v = w_out.rearrange("(j p) f -> p j f", p=128)
    for c in range(NCH):
        S.dma("pool", win[:, c, :], w_in_v[:, c, :], writes=[win])
    for j in range(0, NFC, 2):
        S.dma("pool", wout[:, j:j + 2, :], w_out_v[:, j:j + 2, :], writes=[wout])
    xs = [S.sb([128, NCH, nt], F32, f"x{i}") for i in range(2)]
    xn = S.sb([128, NCH, nt], BF16, "xn")
    sq = [S.sb([128, nt], BF16, f"sq{i}") for i in range(2)]
    act = S.sb([128, NFC, nt], BF16, "act")
    y = S.sb([128, NCH, nt], F32, "y")
    sg = [S.sb([128, nt], F32, f"sg{i}") for i in range(2)]
    rstd = S.sb([128, nt], F32, "rstd")
    rstd2 = S.sb([128, nt], F32, "rstd2")
    tmp = [S.sb([128, nt], F32, f"tmp{i}") for i in range(2)]
    pstat = S.ps([128, 512], F32, "pstat")
    pg = [S.ps([128, 512], F32, f"pg{i}") for i in range(2)]
    pu = [S.ps([128, 512], F32, f"pu{i}") for i in range(2)]
    py = [S.ps([128, 512], F32, f"py{i}") for i in range(2)]
    xT_v = xT.rearrange("(c p) t -> p c t", p=128)
    yT_v = yT.rearrange("(c p) t -> p c t", p=128)
    out_toks = []

    def load(i):
        x = xs[i % 2]
        S.dma("sp", x[:], xT_v[:, :, i * nt:(i + 1) * nt], writes=[x])

    load(0)
    for i in range(ntiles):
        x = xs[i % 2]
        if i + 1 < ntiles:
            load(i + 1)
        def sqf(c, x=x):
            s = sq[c % 2]
            S.op("pool", lambda e, s=s, c=c: e.tensor_tensor(out=s[:], in0=x[:, c, :], in1=x[:, c, :], op=ALU.mult), reads=[x], writes=[s])
            return s, s[:]
        rms_stats(S, C, sqf, NCH, pstat, rstd, nt, D)
        for c in range(NCH):
            S.op("dve", lambda e, c=c, x=x: e.scalar_tensor_tensor(out=xn[:, c, :], in0=x[:, c, :], scalar=g[:, c:c + 1], in1=rstd[:], op0=ALU.mult, op1=ALU.mult),
                 reads=[x, g, rstd], writes=[xn])
        for j in range(NFC):
            a, b = pg[j % 2], pu[j % 2]
            for c in range(NCH):
                S.op("pe", lambda e, a=a, c=c, j=j: e.matmul(a[:, 0:nt], lhsT=win[:, c, j * 128:(j + 1) * 128], rhs=xn[:, c, :], start=(c == 0), stop=(c == NCH - 1)),
                     reads=[win, xn], writes=[a])
            for c in range(NCH):
                S.op("pe", lambda e, b=b, c=c, j=j: e.matmul(b[:, 0:nt], lhsT=win[:, c, DFF + j * 128:DFF + (j + 1) * 128], rhs=xn[:, c, :], start=(c == 0), stop=(c == NCH - 1)),
                     reads=[win, xn], writes=[b])
            s = sg[j % 2]
            S.op("act", lambda e, a=a, s=s: e.activation(out=s[:], in_=a[:, 0:nt], func=AF.Silu), reads=[a], writes=[s])
            S.op("dve", lambda e, b=b, s=s, j=j: e.tensor_tensor(out=act[:, j, :], in0=b[:, 0:nt], in1=s[:], op=ALU.mult), reads=[b, s], writes=[act])
        ysq = []
        for m in range(NCH):
            p = py[m % 2]
            for j in range(NFC):
                S.op("pe", lambda e, p=p, j=j, m=m: e.matmul(p[:, 0:nt], lhsT=wout[:, j, m * 128:(m + 1) * 128], rhs=act[:, j, :], start=(j == 0), stop=(j == NFC - 1)),
                     reads=[wout, act], writes=[p])
            S.op("act", lambda e, p=p, m=m: e.activation(out=y[:, m, :], in_=p[:, 0:nt], func=AF.Copy), reads=[p], writes=[y])
        def sqy(c):
            s = sq[c % 2]
            S.op("pool", lambda e, s=s, c=c: e.tensor_tensor(out=s[:], in0=y[:, c, :], in1=y[:, c, :], op=ALU.mult), reads=[y], writes=[s])
            return s, s[:]
        rms_stats(S, C, sqy, NCH, pstat, rstd2, nt, D)
        for m in range(NCH):
            t = tmp[m % 2]
            S.op("dve", lambda e, m=m, t=t: e.scalar_tensor_tensor(out=t[:], in0=y[:, m, :], scalar=g[:, 8 + m:9 + m], in1=rstd2[:], op0=ALU.mult, op1=ALU.mult),
                 reads=[y, g, rstd2], writes=[t])
            S.op("dve", lambda e, m=m, t=t, x=x: e.scalar_tensor_tensor(out=x[:, m, :], in0=t[:], scalar=0.5, in1=x[:, m, :], op0=ALU.mult, op1=ALU.add),
                 reads=[t, x], writes=[x])
        out_toks.append(S.dma("sp", yT_v[:, :, i * nt:(i + 1) * nt], x[:], reads=[x]))
    return out_toks


def ng_layout(norm_gains_l, idxs):
    a = np.asarray(norm_gains_l, dtype=np.float32)[list(idxs)]
    a = a.reshape(len(idxs), NCH, 128).transpose(2, 0, 1).reshape(128, len(idxs) * NCH)
    return np.ascontiguousarray(a)


NSA_NROPE = 14


def build_nsain(T_tok, NT=256, ctx=None):
    nc = bass.Bass("TRN2", target_bir_lowering=False) if ctx is None else ctx[0].nc
    xT = _io(nc, ctx, "xT", [D, T_tok], F32, "ExternalInput")
    w_fm = _io(nc, ctx, "w_fm", [D, 30 * 128], F32, "ExternalInput")
    w_tm = _io(nc, ctx, "w_tm", [D, 560], F32, "ExternalInput")
    ng = _io(nc, ctx, "ng", [128, 8], F32, "ExternalInput")
    cs = _io(nc, ctx, "cs", [128, 2, T_tok], F32, "ExternalInput")
    qkT = _io(nc, ctx, "qkT", [NSA_NROPE * 128, T_tok], BF16, "ExternalOutput")
    vcT = _io(nc, ctx, "vcT", [256, T_tok], BF16, "ExternalOutput")
    vsw = _io(nc, ctx, "vsw", [T_tok, 512], BF16, "ExternalOutput")
    gates = _io(nc, ctx, "gates", [T_tok, 48], F32, "ExternalOutput")
    nt = NT
    ntiles = T_tok // nt
    nsub = nt // 128
    with _SchedCtx(nc, ctx) as (S, C, own):
        wf = S.sb([128, NCH, 30 * 128], BF16, "wf")
        wt = S.sb([128, NCH, 560], BF16, "wt")
        g = S.sb([128, 8], F32, "gains")
        S.dma("sp", g[:], ng, writes=[g])
        w_fm_v = w_fm.rearrange("(c p) f -> p c f", p=128)
        w_tm_v = w_tm.rearrange("(c p) f -> p c f", p=128)
        for c in range(NCH):
            S.dma("pool", wf[:, c, :], w_fm_v[:, c, :], writes=[wf])
        for c in range(NCH):
            S.dma("pool", wt[:, c, :], w_tm_v[:, c, :], writes=[wt])
        xs = [S.sb([128, NCH, nt], F32, f"x{i}") for i in range(2)]
        cst = [S.sb([128, 2, nt], F32, f"cs{i}") for i in range(2)]
        hn = S.sb([128, NCH, nt], BF16, "hn")
        sq = [S.sb([128, nt], BF16, f"sq{i}") for i in range(2)]
        rstd = S.sb([128, nt], F32, "rstd")
        t1 = [S.sb([128, nt], F32, f"t1_{i}") for i in range(2)]
        t2 = [S.sb([128, nt], F32, f"t2_{i}") for i in range(2)]
        oqk = [S.sb([128, NSA_NROPE, nt], BF16, f"oqk{i}") for i in range(2)]
        ovc = [S.sb([128, 2, nt], BF16, f"ovc{i}") for i in range(2)]
        ovs = [S.sb([128, nsub, 512], BF16, f"ovs{i}") for i in range(2)]
        ogt = [S.sb([128, nsub, 48], F32, f"ogt{i}") for i in range(2)]
        pstat = S.ps([128, 512], F32, "pstat")
        pa = [S.ps([128, 512], F32, f"pa{i}") for i in range(2)]
        pb = [S.ps([128, 512], F32, f"pb{i}") for i in range(2)]
        pt = [S.ps([128, 512], F32, f"pt{i}") for i in range(2)]
        pgt = T(pstat.h[:, 256:512], "pgt_alias")
        pgt = pstat
        xT_v = xT.rearrange("(c p) t -> p c t", p=128)
        qk_v = qkT.rearrange("(c p) t -> p c t", p=128)
        vc_v = vcT.rearrange("(c p) t -> p c t", p=128)
        vsw_v = vsw.rearrange("(s p) f -> p s f", p=128)
        gt_v = gates.rearrange("(s p) f -> p s f", p=128)
        toks = []

        def load(i):
            S.dma("sp", xs[i % 2][:], xT_v[:, :, i * nt:(i + 1) * nt], writes=[xs[i % 2]])
            S.dma("sp", cst[i % 2][:], cs[:, :, i * nt:(i + 1) * nt], writes=[cst[i % 2]])

        load(0)
        for i in range(ntiles):
            x = xs[i % 2]
            cs_t = cst[i % 2]
            if i + 1 < ntiles:
                load(i + 1)

            def sqf(c, x=x):
                s = sq[c % 2]
                S.op("pool", lambda e, s=s, c=c: e.tensor_tensor(out=s[:], in0=x[:, c, :], in1=x[:, c, :], op=ALU.mult), reads=[x], writes=[s])
                return s, s[:]
            rms_stats(S, C, sqf, NCH, pstat, rstd, nt, D)
            for c in range(NCH):
                S.op("dve", lambda e, c=c, x=x: e.scalar_tensor_tensor(out=hn[:, c, :], in0=x[:, c, :], scalar=g[:, c:c + 1], in1=rstd[:], op0=ALU.mult, op1=ALU.mult),
                     reads=[x, g, rstd], writes=[hn])
            oq = oqk[i % 2]
            ov = ovc[i % 2]
            for j in range(NSA_NROPE):
                a, b = pa[j % 2], pb[j % 2]
                for c in range(NCH):
                    S.op("pe", lambda e, a=a, c=c, j=j: e.matmul(a[:, 0:nt], lhsT=wf[:, c, j * 128:(j + 1) * 128], rhs=hn[:, c, :], start=(c == 0), stop=(c == NCH - 1)),
                         reads=[wf, hn], writes=[a])
                for c in range(NCH):
                    S.op("pe", lambda e, b=b, c=c, j=j: e.matmul(b[:, 0:nt], lhsT=wf[:, c, (14 + j) * 128:(15 + j) * 128], rhs=hn[:, c, :], start=(c == 0), stop=(c == NCH - 1)),
                         reads=[wf, hn], writes=[b])
                u1, u2 = t1[j % 2], t2[j % 2]
                S.op("dve", lambda e, a=a, u1=u1, cs_t=cs_t: e.tensor_tensor(out=u1[:], in0=a[:, 0:nt], in1=cs_t[:, 0, :], op=ALU.mult), reads=[a, cs_t], writes=[u1])
                S.op("dve", lambda e, b=b, u2=u2, cs_t=cs_t: e.tensor_tensor(out=u2[:], in0=b[:, 0:nt], in1=cs_t[:, 1, :], op=ALU.mult), reads=[b, cs_t], writes=[u2])
                S.op("pool", lambda e, u1=u1, u2=u2, j=j, oq=oq: e.tensor_tensor(out=oq[:, j, :], in0=u1[:], in1=u2[:], op=ALU.add), reads=[u1, u2], writes=[oq])
            for j in range(2):
                a = pa[j % 2]
                for c in range(NCH):
                    S.op("pe", lambda e, a=a, c=c, j=j: e.matmul(a[:, 0:nt], lhsT=wf[:, c, (28 + j) * 128:(29 + j) * 128], rhs=hn[:, c, :], start=(c == 0), stop=(c == NCH - 1)),
                         reads=[wf, hn], writes=[a])
                S.op("act", lambda e, a=a, j=j, ov=ov: e.activation(out=ov[:, j, :], in_=a[:, 0:nt], func=AF.Copy), reads=[a], writes=[ov])
            osw = ovs[i % 2]
            og = ogt[i % 2]
            for s in range(nsub):
                p = pt[s % 2]
                for c in range(NCH):
                    S.op("pe", lambda e, p=p, c=c, s=s: e.matmul(p[:, 0:512], lhsT=hn[:, c, s * 128:(s + 1) * 128], rhs=wt[:, c, 0:512], start=(c == 0), stop=(c == NCH - 1)),
                         reads=[wt, hn], writes=[p])
                S.op("act", lambda e, p=p, s=s, osw=osw: e.activation(out=osw[:, s, :], in_=p[:, 0:512], func=AF.Copy), reads=[p], writes=[osw])
                for c in range(NCH):
                    S.op("pe", lambda e, c=c, s=s: e.matmul(pgt[:, 256:304], lhsT=hn[:, c, s * 128:(s + 1) * 128], rhs=wt[:, c, 512:560], start=(c == 0), stop=(c == NCH - 1)),
                         reads=[wt, hn], writes=[pgt])
                S.op("act", lambda e, s=s, og=og: e.activation(out=og[:, s, :], in_=pgt[:, 256:304], func=AF.Sigmoid), reads=[pgt], writes=[og])
            sl = slice(i * nt, (i + 1) * nt)
            toks.append(S.dma("sp", qk_v[:, 0:7, sl], oq[:, 0:7, :], reads=[oq]))
            toks.append(S.dma("sp", qk_v[:, 7:14, sl], oq[:, 7:14, :], reads=[oq]))
            toks.append(S.dma("sp", vc_v[:, :, sl], ov[:], reads=[ov]))
            toks.append(S.dma("sp", vsw_v[:, i * nsub:(i + 1) * nsub, :], osw[:], reads=[osw]))
            toks.append(S.dma("sp", gt_v[:, i * nsub:(i + 1) * nsub, :], og[:], reads=[og]))
        S.final_wait("sp", toks)
        if own:
            S.emit()
    return nc


def rope_tables(pos):
    half = 32
    inv = (10000.0 ** (-np.arange(half, dtype=np.float32) / half)).astype(np.float32)
    ang = pos.astype(np.float32)[None, :] * inv[:, None]
    cos = np.cos(ang).astype(np.float32)
    sin = np.sin(ang).astype(np.float32)
    r = np.arange(128)
    f = r % 32
    sign = np.where((r % 64) < 32, -1.0, 1.0).astype(np.float32)
    out = np.empty((128, 2, len(pos)), np.float32)
    out[:, 0, :] = cos[f]
    out[:, 1, :] = sin[f] * sign[:, None]
    return out


def nsa_w_layout(w):
    w = np.asarray(w, np.float32)
    rope_cols = np.concatenate([np.arange(0, 1024), np.arange(1024, 1280), np.arange(1536, 1792), np.arange(2048, 2304)])
    sw = rope_cols.reshape(-1, 2, 32)[:, ::-1, :].reshape(-1)
    w_fm = np.concatenate([w[:, rope_cols], w[:, sw], w[:, 1280:1536]], axis=1)
    w_tm = np.concatenate([w[:, 1792:2048], w[:, 2304:2560], w[:, 2560:2608]], axis=1)
    return np.ascontiguousarray(w_fm), np.ascontiguousarray(w_tm)


BIG = 30000.0


def nsa_consts(S_len):
    nsel = S_len // 64
    ncmp = S_len // 16 - 1
    nct = (ncmp + 127) // 128
    bk = min(128, nsel)
    c = {}
    c["ident"] = np.eye(128, dtype=np.float32).astype(NPBF)
    kl = np.arange(128)[:, None]
    ql = np.arange(128)[None, :]
    c["tri_le"] = np.where(kl <= ql, 0.0, -BIG).astype(NPBF)
    c["tri_gt"] = np.where(kl > ql, 0.0, -BIG).astype(NPBF)
    m = np.arange(17)[None, :, None]
    cb = np.where(16 * kl[:, :, None] + 31 <= 128 * m + ql[None, :, :], 0.0, -BIG)
    c["cb"] = cb.astype(NPBF)
    ni = bk // 2
    eb = np.zeros((bk, ni, 128), np.float32)
    for i in range(ni):
        eb[2 * i, i, 0:64] = 1.0
        eb[2 * i + 1, i, 64:128] = 1.0
    c["ebig"] = eb.reshape(bk, ni * 128).astype(NPBF)
    cc = np.arange(nct * 128)[:, None]
    nn = np.arange(nsel)[None, :]
    mm = ((cc >= 4 * nn - 1) & (cc <= 4 * nn + 3) & (cc < ncmp)).astype(np.float32)
    c["mmat"] = np.ascontiguousarray(mm.reshape(nct, 128, nsel).transpose(1, 0, 2)).astype(NPBF)
    rel = np.arange(2 * nsel)[None, :] - nsel
    cur = (np.arange(128)[:, None] >= 64).astype(np.int64)
    fb = np.where((rel == cur) | (rel == cur - 1), 100.0, np.where(rel > cur, -100.0, 0.0))
    c["fb"] = fb.astype(np.float32)
    return c


def build_nsa_attn(S_len, ctx=None):
    nsel = S_len // 64
    ncmp = S_len // 16 - 1
    nct = (ncmp + 127) // 128
    ncp = nct * 128
    nq = S_len // 128
    bk = min(128, nsel)
    nbc = (nsel + 127) // 128
    ni = bk // 2
    VX = 65 + nsel
    nc = bass.Bass("TRN2", target_bir_lowering=False) if ctx is None else ctx[0].nc
    def din(name, shape, dt):
        return _io(nc, ctx, name, list(shape), dt, "ExternalInput")
    qT = din("qT", [64, 4, S_len], BF16)
    kc2 = din("kc2", [128, S_len], BF16)
    vc2 = din("vc2", [128, S_len], BF16)
    ksT = din("ksT", [64, S_len], BF16)
    kwT = din("kwT", [64, S_len], BF16)
    vs = din("vs", [S_len, 64], BF16)
    vw = din("vw", [S_len, 64], BF16)
    gates = din("gates", [S_len, 12], F32)
    w1s = din("w1s", [2, 128, 16, 64], F32)
    w2 = din("w2", [2, 64, 64], F32)
    pos2 = din("pos2", [2, 128, 16], F32)
    c_ident = din("ident", [128, 128], BF16)
    c_tle = din("tri_le", [128, 128], BF16)
    c_tgt = din("tri_gt", [128, 128], BF16)
    c_cb = din("cb", [128, 17, 128], BF16)
    c_eb = din("ebig", [bk, ni * 128], BF16)
    c_mm = din("mmat", [128, nct, nsel], BF16)
    c_fb = din("fb", [128, 2 * nsel], F32)
    o = _io(nc, ctx, "o", [S_len, 256], BF16, "ExternalOutput")
    with _SchedCtx(nc, ctx) as (S, C, own):
        ident = S.sb([128, 128], BF16, "ident_sb")
        tle = S.sb([128, 128], BF16, "tle")
        tgt = S.sb([128, 128], BF16, "tgt")
        cb = S.sb([128, 17, 128], BF16, "cb")
        eb = S.sb([bk, ni * 128], BF16, "eb")
        fb = S.sb([128, 2 * nsel], F32, "fb")
        ks_sb = S.sb([64, S_len], BF16, "ks_sb")
        kw_sb = S.sb([64, S_len], BF16, "kw_sb")
        vsx = S.sb([128, nq, 65], BF16, "vsx")
        vwx = S.sb([128, nq, 65], BF16, "vwx")
        vcx = S.sb([128, nct, VX], BF16, "vcx")
        kcmpT = S.sb([64, ncp], BF16, "kcmpT")
        w1b = S.sb([128, 2, 16, 64], BF16, "w1b")
        w2b = S.sb([64, 2, 64], BF16, "w2b")
        p2f = S.sb([128, 2, 16], F32, "p2f")
        p2b = S.sb([128, 2, 16], BF16, "p2b")
        GS = min(S_len, 8192)
        stage = S.sb([128, GS], BF16, "stage")
        hbias = S.sb([64, 2], F32, "hbias")
        actT = S.sb([64, 512], BF16, "actT")
        for (t, src) in ((ident, c_ident), (tle, c_tle), (tgt, c_tgt), (eb, c_eb), (fb, c_fb)):
            S.dma("sp", t[:], src, writes=[t])
        for m0 in range(0, 17, 6):
            m1 = min(17, m0 + 6)
            S.dma("sp", cb[:, m0:m1, :], c_cb[:, m0:m1, :], writes=[cb])
        for (t, src) in ((ks_sb, ksT), (kw_sb, kwT)):
            st = min(4096, S_len)
            for s0 in range(0, S_len, st):
                S.dma("sp", t[:, s0:s0 + st], src[:, s0:s0 + st], writes=[t])
        for (t, src) in ((vsx, vs), (vwx, vw)):
            S.op("pool", lambda e, t=t: e.memset(t[:, :, 64:65], 1.0), writes=[t])
            srcv = src.rearrange("(n p) d -> p n d", p=128)
            for n0 in range(0, nq, 8):
                S.dma("sp", t[:, n0:n0 + 8, 0:64], srcv[:, n0:n0 + 8, :], writes=[t])
        S.op("pool", lambda e: e.memset(vcx[:, :, 64:65], 1.0), writes=[vcx])
        for ct in range(nct):
            S.dma("sp", vcx[:, ct, 65:VX], c_mm[:, ct, :], writes=[vcx])
        for kv in range(2):
            S.dma("pool", w1b[:, kv, :, :], w1s[kv], writes=[w1b])
            S.dma("pool", w2b[:, kv, :], w2[kv], writes=[w2b])
            S.dma("sp", p2f[:, kv, :], pos2[kv], writes=[p2f])
        S.op("dve", lambda e: e.tensor_copy(out=p2b[:], in_=p2f[:]), reads=[p2f], writes=[p2b])
        pss = [S.ps([128, 512], F32, f"pss{i}") for i in range(2)]
        acc = [S.ps([128, 512], F32, f"acc{i}") for i in range(4)]
        pmisc = S.ps([128, 512], F32, "pmisc")
        for kv in range(2):
            src = kc2 if kv == 0 else vc2
            for j in range(16):
                S.op("pe", lambda e, j=j, kv=kv: e.matmul(pmisc[0:64, 0:1], lhsT=w1b[:, kv, j, :], rhs=p2b[:, kv, j:j + 1], start=(j == 0), stop=(j == 15)),
                     reads=[w1b, p2b], writes=[pmisc])
            S.op("dve", lambda e, kv=kv: e.tensor_copy(out=hbias[:, kv:kv + 1], in_=pmisc[0:64, 0:1]), reads=[pmisc], writes=[hbias])
            for g0 in range(0, ncp, 512):
                gn = min(512, ncp - g0)
                t0 = g0 * 16
                if t0 % GS == 0:
                    for s0 in range(0, GS, 2048):
                        S.dma("sp", stage[:, s0:s0 + 2048], src[:, t0 + s0:t0 + s0 + 2048], writes=[stage])
                tb = t0 % GS
                ph = pss[0]
                for j in range(16):
                    S.op("pe", lambda e, j=j, kv=kv, tb=tb, gn=gn, ph=ph: e.matmul(ph[0:64, 0:gn], lhsT=w1b[:, kv, j, :], rhs=stage[:, tb + j:tb + j + 16 * (gn - 1) + 1:16], start=(j == 0), stop=(j == 15)),
                         reads=[w1b, stage], writes=[ph])
                S.op("act", lambda e, kv=kv, gn=gn, ph=ph: e.activation(out=actT[:, 0:gn], in_=ph[0:64, 0:gn], func=AF.Silu, bias=hbias[:, kv:kv + 1]), reads=[ph, hbias], writes=[actT])
                if kv == 0:
                    pk = pss[1]
                    S.op("pe", lambda e, gn=gn, pk=pk: e.matmul(pk[0:64, 0:gn], lhsT=w2b[:, 0, :], rhs=actT[:, 0:gn], start=True, stop=True), reads=[w2b, actT], writes=[pk])
                    S.op("dve", lambda e, gn=gn, g0=g0, pk=pk: e.tensor_copy(out=kcmpT[:, g0:g0 + gn], in_=pk[0:64, 0:gn]), reads=[pk], writes=[kcmpT])
                else:
                    for s in range(gn // 128):
                        S.op("pe", lambda e, s=s: e.matmul(pmisc[:, 0:64], lhsT=actT[:, s * 128:(s + 1) * 128], rhs=w2b[:, 1, :], start=True, stop=True), reads=[w2b, actT], writes=[pmisc])
                        ct = g0 // 128 + s
                        S.op("dve", lambda e, ct=ct: e.tensor_copy(out=vcx[:, ct, 0:64], in_=pmisc[:, 0:64]), reads=[pmisc], writes=[vcx])
        qt = [S.sb([64, 4, 128], BF16, f"qt{i}") for i in range(2)]
        gt = [S.sb([128, 12], F32, f"gt{i}") for i in range(2)]
        pT = [S.sb([128, 512], BF16, f"pT{i}") for i in range(4)]
        ot = [S.sb([128, 256], F32, f"ot{i}") for i in range(2)]
        otb = [S.sb([128, 256], BF16, f"otb{i}") for i in range(2)]
        imp = S.sb([128, nsel], F32, "imp")
        scr = S.sb([128, nsel], F32, "scr")
        nb = S.sb([128, nsel], BF16, "nb")
        m8 = S.sb([128, 16], F32, "m8")
        nbT = S.sb([bk, nbc, 128], BF16, "nbT")
        rz = S.sb([128, 4], F32, "rz")
        wgt = S.sb([128, 4], F32, "wgt")
        gv = gates.rearrange("(n p) f -> p n f", p=128)
        ov = o.rearrange("(n p) f -> p n f", p=128)
        out_toks = []
        pcount = [0]

        def load_q(j):
            S.dma("sp", qt[j % 2][:], qT[:, :, j * 128:(j + 1) * 128], writes=[qt[j % 2]])
            S.dma("sp", gt[j % 2][:], gv[:, j, :], writes=[gt[j % 2]])

        def branch(q, tiles, width, first_branch, gcol, g_t, o_t):
            n = len(tiles)
            sc = []

            def emit_scores(k):
                ps = pss[k % 2]
                mms = tiles[k][0]
                for idx, (lhsT_t, lhsT_ap, rhs_t, rhs_ap) in enumerate(mms):
                    S.op("pe", lambda e, ps=ps, a=lhsT_ap, b=rhs_ap, idx=idx, nm=len(mms): e.matmul(ps[:, 0:512].rearrange("p (h q) -> p h q", h=4), lhsT=a, rhs=b, start=(idx == 0), stop=(idx == nm - 1)),
                         reads=[lhsT_t, rhs_t], writes=[ps])

            def emit_exp(k):
                ps = pss[k % 2]
                p = pT[pcount[0] % 4]
                pcount[0] += 1
                S.op("act", lambda e, ps=ps, p=p: e.activation(out=p[:], in_=ps[:, 0:512], func=AF.Exp, scale=0.125), reads=[ps], writes=[p])
                return p

            def emit_pv(k, p):
                vt, vap = tiles[k][1]
                for h in range(4):
                    S.op("pe", lambda e, h=h, p=p, vap=vap, k=k: e.matmul(acc[h][:, 0:width], lhsT=p[:, h * 128:(h + 1) * 128], rhs=vap, start=(k == 0), stop=(k == n - 1)),
                         reads=[p, vt], writes=[acc[h]])

            emit_scores(0)
            for k in range(n):
                if k + 1 < n:
                    emit_scores(k + 1)
                p = emit_exp(k)
                emit_pv(k, p)
            for h in range(4):
                S.op("dve", lambda e, h=h: e.tensor_scalar(out=rz[:, h:h + 1], in0=acc[h][:, 64:65], scalar1=1e-30, scalar2=None, op0=ALU.max), reads=[acc[h]], writes=[rz])
            S.op("dve", lambda e: e.reciprocal(out=rz[:], in_=rz[:]), reads=[rz], writes=[rz])
            S.op("dve", lambda e: e.tensor_tensor(out=wgt[:], in0=rz[:], in1=g_t[:, gcol:12:3], op=ALU.mult), reads=[rz, g_t], writes=[wgt])
            for h in range(4):
                if first_branch:
                    S.op("dve", lambda e, h=h: e.tensor_scalar(out=o_t[:, h * 64:(h + 1) * 64], in0=acc[h][:, 0:64], scalar1=wgt[:, h:h + 1], scalar2=None, op0=ALU.mult),
                         reads=[acc[h], wgt], writes=[o_t])
                else:
                    S.op("dve", lambda e, h=h: e.scalar_tensor_tensor(out=o_t[:, h * 64:(h + 1) * 64], in0=acc[h][:, 0:64], scalar=wgt[:, h:h + 1], in1=o_t[:, h * 64:(h + 1) * 64], op0=ALU.mult, op1=ALU.add),
                         reads=[acc[h], wgt, o_t], writes=[o_t])

        load_q(0)
        for j in range(nq):
            q = qt[j % 2]
            g_t = gt[j % 2]
            o_t = ot[j % 2]
            if j + 1 < nq:
                load_q(j + 1)
            qap = q[:]
            nct_j = min(nct, (8 * j + 6) // 128 + 1)
            tiles = []
            for ct in range(nct_j):
                mms = [(kcmpT, kcmpT[:, ct * 128:(ct + 1) * 128], q, qap)]
                m = j - 16 * ct
                if m <= 16:
                    mms.append((ident, ident[:], cb, cb[:, max(m, 0), :].unsqueeze(1).to_broadcast([128, 4, 128])))
                tiles.append((mms, (vcx, vcx[:, ct, :])))
            branch(q, tiles, VX, True, 0, g_t, o_t)
            for h in range(4):
                if h == 0:
                    S.op("dve", lambda e: e.tensor_scalar(out=imp[:], in0=acc[0][:, 65:VX], scalar1=rz[:, 0:1], scalar2=None, op0=ALU.mult), reads=[acc[0], rz], writes=[imp])
                else:
                    S.op("dve", lambda e, h=h: e.scalar_tensor_tensor(out=imp[:], in0=acc[h][:, 65:VX], scalar=rz[:, h:h + 1], in1=imp[:], op0=ALU.mult, op1=ALU.add), reads=[acc[h], rz, imp], writes=[imp])
            S.op("dve", lambda e, j=j: e.tensor_tensor(out=imp[:], in0=imp[:], in1=fb[:, nsel - 2 * j:2 * nsel - 2 * j], op=ALU.add), reads=[imp, fb], writes=[imp])
            S.op("dve", lambda e: e.memset(imp[:, 0:1], 100.0), reads=[imp], writes=[imp])
            S.op("dve", lambda e: e.max(out=m8[:, 0:8], in_=imp[:]), reads=[imp], writes=[m8])
            S.op("dve", lambda e: e.match_replace(out=scr[:], in_to_replace=m8[:, 0:8], in_values=imp[:], imm_value=-1e30), reads=[imp, m8], writes=[scr])
            S.op("dve", lambda e: e.max(out=m8[:, 8:16], in_=scr[:]), reads=[scr], writes=[m8])
            S.op("dve", lambda e: e.tensor_scalar(out=nb[:], in0=imp[:], scalar1=m8[:, 15:16], scalar2=1.0, op0=ALU.is_ge, op1=ALU.subtract), reads=[imp, m8], writes=[nb])
            ptr = pmisc
            for cidx in range(nbc):
                S.op("pe", lambda e, cidx=cidx: e.transpose(out=ptr[0:bk, 0:64].bitcast(BF16), in_=nb[:, cidx * bk:(cidx + 1) * bk], identity=ident[:]), reads=[nb, ident], writes=[ptr])
                S.op("dve", lambda e, cidx=cidx: e.tensor_scalar(out=nbT[:, cidx, :], in0=ptr[0:bk, 0:64].bitcast(BF16), scalar1=BIG, scalar2=None, op0=ALU.mult), reads=[ptr], writes=[nbT])
            tiles = []
            for kt in range(j + 1):
                cidx = kt // ni
                mms = [(ks_sb, ks_sb[:, kt * 128:(kt + 1) * 128], q, qap),
                       (eb, eb[:, (kt % ni) * 128:(kt % ni + 1) * 128], nbT, nbT[:, cidx, :].unsqueeze(1).to_broadcast([bk, 4, 128]))]
                if kt == j:
                    mms.append((ident, ident[:], tle, tle[:].unsqueeze(1).to_broadcast([128, 4, 128])))
                tiles.append((mms, (vsx, vsx[:, kt, :])))
            branch(q, tiles, 65, False, 1, g_t, o_t)
            tiles = []
            for kt in range(max(0, j - 4), j + 1):
                mms = [(kw_sb, kw_sb[:, kt * 128:(kt + 1) * 128], q, qap)]
                if kt == j - 4:
                    mms.append((ident, ident[:], tgt, tgt[:].unsqueeze(1).to_broadcast([128, 4, 128])))
                if kt == j:
                    mms.append((ident, ident[:], tle, tle[:].unsqueeze(1).to_broadcast([128, 4, 128])))
                tiles.append((mms, (vwx, vwx[:, kt, :])))
            branch(q, tiles, 65, False, 2, g_t, o_t)
            ob_ = otb[j % 2]
            S.op("pool", lambda e, ob_=ob_, o_t=o_t: e.tensor_copy(out=ob_[:], in_=o_t[:]), reads=[o_t], writes=[ob_])
            out_toks.append(S.dma("sp", ov[:, j, :], ob_[:], reads=[ob_]))
        S.final_wait("sp", out_toks)
        if own:
            S.emit()
    return nc


def nsa_attn_inputs(qkT_b, vcT_b, vsw_b, gates_b, cmp_pos, cmp_w1, cmp_w2, g, consts):
    S_len = qkT_b.shape[1]
    qT = np.ascontiguousarray(qkT_b[256 * g:256 * g + 256].reshape(4, 64, S_len).transpose(1, 0, 2))
    kc = qkT_b[1024 + 64 * g:1024 + 64 * g + 64]
    ks = qkT_b[1280 + 64 * g:1280 + 64 * g + 64]
    kw = qkT_b[1536 + 64 * g:1536 + 64 * g + 64]
    vc = vcT_b[64 * g:64 * g + 64]

    def stack2(a):
        o = np.zeros((128, S_len), a.dtype)
        o[0:64] = a
        o[64:128, :S_len - 16] = a[:, 16:]
        return o
    d = {"qT": qT, "kc2": stack2(kc), "vc2": stack2(vc), "ksT": np.ascontiguousarray(ks), "kwT": np.ascontiguousarray(kw),
         "vs": np.ascontiguousarray(vsw_b[:, 64 * g:64 * g + 64]), "vw": np.ascontiguousarray(vsw_b[:, 256 + 64 * g:256 + 64 * g + 64]),
         "gates": np.ascontiguousarray(gates_b.reshape(S_len, 16, 3)[:, 4 * g:4 * g + 4, :].reshape(S_len, 12))}
    w1 = np.asarray(cmp_w1, np.float32).reshape(2, 2, 16, 64, 64)
    d["w1s"] = np.ascontiguousarray(w1.transpose(0, 1, 3, 2, 4).reshape(2, 128, 16, 64))
    d["w2"] = np.ascontiguousarray(np.asarray(cmp_w2, np.float32))
    p = np.asarray(cmp_pos, np.float32).reshape(2, 2, 16, 64)
    d["pos2"] = np.ascontiguousarray(p.transpose(0, 1, 3, 2).reshape(2, 128, 16))
    d.update(consts)
    return d


def build_out(T_tok, NT=512, ctx=None):
    nc = bass.Bass("TRN2", target_bir_lowering=False) if ctx is None else ctx[0].nc
    xT = _io(nc, ctx, "xT", [D, T_tok], F32, "ExternalInput")
    oT = _io(nc, ctx, "oT", [D, T_tok], BF16, "ExternalInput")
    w = _io(nc, ctx, "w", [D, D], F32, "ExternalInput")
    ng = _io(nc, ctx, "ng", [128, 8], F32, "ExternalInput")
    yT = _io(nc, ctx, "yT", [D, T_tok], F32, "ExternalOutput")
    nt = NT
    ntiles = T_tok // nt
    with _SchedCtx(nc, ctx) as (S, C, own):
        wb = S.sb([128, NCH, D], BF16, "wb")
        g = S.sb([128, 8], F32, "gains")
        S.dma("sp", g[:], ng, writes=[g])
        w_v = w.rearrange("(c p) f -> p c f", p=128)
        for c in range(NCH):
            S.dma("pool", wb[:, c, :], w_v[:, c, :], writes=[wb])
        xs = [S.sb([128, NCH, nt], F32, f"x{i}") for i in range(2)]
        os_ = [S.sb([128, NCH, nt], BF16, f"o{i}") for i in range(2)]
        y = S.sb([128, NCH, nt], F32, "y")
        sq = [S.sb([128, nt], BF16, f"sq{i}") for i in range(2)]
        rstd = S.sb([128, nt], F32, "rstd")
        tmp = [S.sb([128, nt], F32, f"tmp{i}") for i in range(2)]
        pstat = S.ps([128, 512], F32, "pstat")
        py = [S.ps([128, 512], F32, f"py{i}") for i in range(2)]
        xT_v = xT.rearrange("(c p) t -> p c t", p=128)
        oT_v = oT.rearrange("(c p) t -> p c t", p=128)
        yT_v = yT.rearrange("(c p) t -> p c t", p=128)
        toks = []

        def load(i):
            for c0 in range(0, NCH, 2):
                S.dma("sp", xs[i % 2][:, c0:c0 + 2, :], xT_v[:, c0:c0 + 2, i * nt:(i + 1) * nt], writes=[xs[i % 2]])
            for c0 in range(0, NCH, 4):
                S.dma("sp", os_[i % 2][:, c0:c0 + 4, :], oT_v[:, c0:c0 + 4, i * nt:(i + 1) * nt], writes=[os_[i % 2]])
        load(0)
        for i in range(ntiles):
            x, ob = xs[i % 2], os_[i % 2]
            if i + 1 < ntiles:
                load(i + 1)
            for m in range(NCH):
                p = py[m % 2]
                for c in range(NCH):
                    S.op("pe", lambda e, p=p, c=c, m=m, ob=ob: e.matmul(p[:, 0:nt], lhsT=wb[:, c, m * 128:(m + 1) * 128], rhs=ob[:, c, :], start=(c == 0), stop=(c == NCH - 1)),
                         reads=[wb, ob], writes=[p])
                S.op("act", lambda e, p=p, m=m: e.activation(out=y[:, m, :], in_=p[:, 0:nt], func=AF.Copy), reads=[p], writes=[y])

            def sqy(c):
                s = sq[c % 2]
                S.op("pool", lambda e, s=s, c=c: e.tensor_tensor(out=s[:], in0=y[:, c, :], in1=y[:, c, :], op=ALU.mult), reads=[y], writes=[s])
                return s, s[:]
            rms_stats(S, C, sqy, NCH, pstat, rstd, nt, D)
            for m in range(NCH):
                t = tmp[m % 2]
                S.op("dve", lambda e, m=m, t=t: e.scalar_tensor_tensor(out=t[:], in0=y[:, m, :], scalar=g[:, m:m + 1], in1=rstd[:], op0=ALU.mult, op1=ALU.mult),
                     reads=[y, g, rstd], writes=[t])
                S.op("pool", lambda e, m=m, t=t, x=x: e.tensor_tensor(out=x[:, m, :], in0=t[:], in1=x[:, m, :], op=ALU.add), reads=[t, x], writes=[x])
            for c0 in range(0, NCH, 2):
                toks.append(S.dma("sp", yT_v[:, c0:c0 + 2, i * nt:(i + 1) * nt], x[:, c0:c0 + 2, :], reads=[x]))
        S.final_wait("sp", toks)
        if own:
            S.emit()
    return nc


def build_ple(T_tok, NT=512, ctx=None):
    nc = bass.Bass("TRN2", target_bir_lowering=False) if ctx is None else ctx[0].nc
    xT = _io(nc, ctx, "xT", [D, T_tok], F32, "ExternalInput")
    pT = _io(nc, ctx, "pT", [256, T_tok], F32, "ExternalInput")
    w_p = _io(nc, ctx, "w_p", [256, D], F32, "ExternalInput")
    w_g = _io(nc, ctx, "w_g", [D, D], F32, "ExternalInput")
    ng = _io(nc, ctx, "ng", [128, 16], F32, "ExternalInput")
    yT = _io(nc, ctx, "yT", [D, T_tok], F32, "ExternalOutput")
    nt = NT
    ntiles = T_tok // nt
    with _SchedCtx(nc, ctx) as (S, C, own):
        wg = S.sb([128, NCH, D], BF16, "wg")
        wp = S.sb([128, 2, D], BF16, "wp")
        g = S.sb([128, 16], F32, "gains")
        S.dma("sp", g[:], ng, writes=[g])
        wg_v = w_g.rearrange("(c p) f -> p c f", p=128)
        wp_v = w_p.rearrange("(c p) f -> p c f", p=128)
        for c in range(NCH):
            S.dma("pool", wg[:, c, :], wg_v[:, c, :], writes=[wg])
        for c in range(2):
            S.dma("pool", wp[:, c, :], wp_v[:, c, :], writes=[wp])
        xs = [S.sb([128, NCH, nt], F32, f"x{i}") for i in range(2)]
        pb = [S.sb([128, 2, nt], BF16, f"p{i}") for i in range(2)]
        xn = S.sb([128, NCH, nt], BF16, "xn")
        v = S.sb([128, NCH, nt], F32, "v")
        gate = [S.sb([128, nt], F32, f"gate{i}") for i in range(2)]
        sq = [S.sb([128, nt], BF16, f"sq{i}") for i in range(2)]
        rstd = S.sb([128, nt], F32, "rstd")
        rstd2 = S.sb([128, nt], F32, "rstd2")
        tmp = [S.sb([128, nt], F32, f"tmp{i}") for i in range(2)]
        pstat = S.ps([128, 512], F32, "pstat")
        pgp = [S.ps([128, 512], F32, f"pgp{i}") for i in range(2)]
        pep = [S.ps([128, 512], F32, f"pep{i}") for i in range(2)]
        xT_v = xT.rearrange("(c p) t -> p c t", p=128)
        pT_v = pT.rearrange("(c p) t -> p c t", p=128)
        yT_v = yT.rearrange("(c p) t -> p c t", p=128)
        toks = []

        def load(i):
            for c0 in range(0, NCH, 2):
                S.dma("sp", xs[i % 2][:, c0:c0 + 2, :], xT_v[:, c0:c0 + 2, i * nt:(i + 1) * nt], writes=[xs[i % 2]])
            S.dma("pool", pb[i % 2][:], pT_v[:, :, i * nt:(i + 1) * nt], writes=[pb[i % 2]])
        load(0)
        for i in range(ntiles):
            x, pp = xs[i % 2], pb[i % 2]
            if i + 1 < ntiles:
                load(i + 1)

            def sqf(c, x=x):
                s = sq[c % 2]
                S.op("pool", lambda e, s=s, c=c: e.tensor_tensor(out=s[:], in0=x[:, c, :], in1=x[:, c, :], op=ALU.mult), reads=[x], writes=[s])
                return s, s[:]
            rms_stats(S, C, sqf, NCH, pstat, rstd, nt, D)
            for c in range(NCH):
                S.op("dve", lambda e, c=c, x=x: e.scalar_tensor_tensor(out=xn[:, c, :], in0=x[:, c, :], scalar=g[:, c:c + 1], in1=rstd[:], op0=ALU.mult, op1=ALU.mult),
                     reads=[x, g, rstd], writes=[xn])
            for m in range(NCH):
                a, b = pgp[m % 2], pep[m % 2]
                for c in range(NCH):
                    S.op("pe", lambda e, a=a, c=c, m=m: e.matmul(a[:, 0:nt], lhsT=wg[:, c, m * 128:(m + 1) * 128], rhs=xn[:, c, :], start=(c == 0), stop=(c == NCH - 1)),
                         reads=[wg, xn], writes=[a])
                for c in range(2):
                    S.op("pe", lambda e, b=b, c=c, m=m, pp=pp: e.matmul(b[:, 0:nt], lhsT=wp[:, c, m * 128:(m + 1) * 128], rhs=pp[:, c, :], start=(c == 0), stop=(c == 1)),
                         reads=[wp, pp], writes=[b])
                gt_ = gate[m % 2]
                S.op("act", lambda e, a=a, gt_=gt_: e.activation(out=gt_[:], in_=a[:, 0:nt], func=AF.Sigmoid), reads=[a], writes=[gt_])
                S.op("dve", lambda e, b=b, gt_=gt_, m=m: e.tensor_tensor(out=v[:, m, :], in0=b[:, 0:nt], in1=gt_[:], op=ALU.mult), reads=[b, gt_], writes=[v])

            def sqv(c):
                s = sq[c % 2]
                S.op("pool", lambda e, s=s, c=c: e.tensor_tensor(out=s[:], in0=v[:, c, :], in1=v[:, c, :], op=ALU.mult), reads=[v], writes=[s])
                return s, s[:]
            rms_stats(S, C, sqv, NCH, pstat, rstd2, nt, D)
            for m in range(NCH):
                t = tmp[m % 2]
                S.op("dve", lambda e, m=m, t=t: e.scalar_tensor_tensor(out=t[:], in0=v[:, m, :], scalar=g[:, 8 + m:9 + m], in1=rstd2[:], op0=ALU.mult, op1=ALU.mult),
                     reads=[v, g, rstd2], writes=[t])
                S.op("pool", lambda e, m=m, t=t, x=x: e.tensor_tensor(out=x[:, m, :], in0=t[:], in1=x[:, m, :], op=ALU.add), reads=[t, x], writes=[x])
            for c0 in range(0, NCH, 2):
                toks.append(S.dma("sp", yT_v[:, c0:c0 + 2, i * nt:(i + 1) * nt], x[:, c0:c0 + 2, :], reads=[x]))
        S.final_wait("sp", toks)
        if own:
            S.emit()
    return nc


def build_hgrnin(T_tok, NT=256, ctx=None):
    nc = bass.Bass("TRN2", target_bir_lowering=False) if ctx is None else ctx[0].nc
    xT = _io(nc, ctx, "xT", [D, T_tok], F32, "ExternalInput")
    w = _io(nc, ctx, "w", [D, 4096], F32, "ExternalInput")
    ng = _io(nc, ctx, "ng", [128, 8], F32, "ExternalInput")
    qfT = _io(nc, ctx, "qfT", [2048, T_tok], F32, "ExternalOutput")
    iv = _io(nc, ctx, "iv", [T_tok, D], BF16, "ExternalOutput")
    gs = _io(nc, ctx, "gs", [T_tok, D], F32, "ExternalOutput")
    nt = NT
    ntiles = T_tok // nt
    nsub = nt // 128
    with _SchedCtx(nc, ctx) as (S, C, own):
        wb = S.sb([128, NCH, 4096], BF16, "wb")
        g = S.sb([128, 8], F32, "gains")
        S.dma("sp", g[:], ng, writes=[g])
        w_v = w.rearrange("(c p) f -> p c f", p=128)
        for c in range(NCH):
            S.dma("pool", wb[:, c, :], w_v[:, c, :], writes=[wb])
        xs = [S.sb([128, NCH, nt], F32, f"x{i}") for i in range(2)]
        hn = S.sb([128, NCH, nt], BF16, "hn")
        sq = [S.sb([128, nt], BF16, f"sq{i}") for i in range(2)]
        rstd = S.sb([128, nt], F32, "rstd")
        ofm = [S.sb([128, 16, nt], F32, f"ofm{i}") for i in range(2)]
        oiv = [S.sb([128, nsub, D], BF16, f"oiv{i}") for i in range(2)]
        ogs = [S.sb([128, nsub, D], F32, f"ogs{i}") for i in range(2)]
        pstat = S.ps([128, 512], F32, "pstat")
        pa = [S.ps([128, 512], F32, f"pa{i}") for i in range(2)]
        pt = [S.ps([128, 512], F32, f"pt{i}") for i in range(2)]
        xT_v = xT.rearrange("(c p) t -> p c t", p=128)
        qf_v = qfT.rearrange("(c p) t -> p c t", p=128)
        iv_v = iv.rearrange("(s p) f -> p s f", p=128)
        gs_v = gs.rearrange("(s p) f -> p s f", p=128)
        toks = []

        def load(i):
            S.dma("sp", xs[i % 2][:], xT_v[:, :, i * nt:(i + 1) * nt], writes=[xs[i % 2]])
        load(0)
        for i in range(ntiles):
            x = xs[i % 2]
            if i + 1 < ntiles:
                load(i + 1)

            def sqf(c, x=x):
                s = sq[c % 2]
                S.op("pool", lambda e, s=s, c=c: e.tensor_tensor(out=s[:], in0=x[:, c, :], in1=x[:, c, :], op=ALU.mult), reads=[x], writes=[s])
                return s, s[:]
            rms_stats(S, C, sqf, NCH, pstat, rstd, nt, D)
            for c in range(NCH):
                S.op("dve", lambda e, c=c, x=x: e.scalar_tensor_tensor(out=hn[:, c, :], in0=x[:, c, :], scalar=g[:, c:c + 1], in1=rstd[:], op0=ALU.mult, op1=ALU.mult),
                     reads=[x, g, rstd], writes=[hn])
            of = ofm[i % 2]
            for j in range(16):
                a = pa[j % 2]
                for c in range(NCH):
                    S.op("pe", lambda e, a=a, c=c, j=j: e.matmul(a[:, 0:nt], lhsT=wb[:, c, j * 128:(j + 1) * 128], rhs=hn[:, c, :], start=(c == 0), stop=(c == NCH - 1)),
                         reads=[wb, hn], writes=[a])
                fn = AF.Silu if j < 8 else AF.Copy
                S.op("act", lambda e, a=a, j=j, of=of, fn=fn: e.activation(out=of[:, j, :], in_=a[:, 0:nt], func=fn), reads=[a], writes=[of])
            oi, og = oiv[i % 2], ogs[i % 2]
            for s in range(nsub):
                for hf in range(4):
                    p = pt[hf % 2]
                    for c in range(NCH):
                        S.op("pe", lambda e, p=p, c=c, s=s, hf=hf: e.matmul(p[:, 0:512], lhsT=hn[:, c, s * 128:(s + 1) * 128], rhs=wb[:, c, 2048 + hf * 512:2048 + (hf + 1) * 512], start=(c == 0), stop=(c == NCH - 1)),
                             reads=[wb, hn], writes=[p])
                    if hf < 2:
                        S.op("dve", lambda e, p=p, s=s, hf=hf, oi=oi: e.tensor_copy(out=oi[:, s, hf * 512:(hf + 1) * 512], in_=p[:, 0:512]), reads=[p], writes=[oi])
                    else:
                        S.op("act", lambda e, p=p, s=s, hf=hf, og=og: e.activation(out=og[:, s, (hf - 2) * 512:(hf - 1) * 512], in_=p[:, 0:512], func=AF.Silu), reads=[p], writes=[og])
            sl = slice(i * nt, (i + 1) * nt)
            toks.append(S.dma("sp", qf_v[:, 0:8, sl], of[:, 0:8, :], reads=[of]))
            toks.append(S.dma("sp", qf_v[:, 8:16, sl], of[:, 8:16, :], reads=[of]))
            toks.append(S.dma("sp", iv_v[:, i * nsub:(i + 1) * nsub, :], oi[:], reads=[oi]))
            toks.append(S.dma("sp", gs_v[:, i * nsub:(i + 1) * nsub, :], og[:], reads=[og]))
        S.final_wait("sp", toks)
        if own:
            S.emit()
    return nc


def build_hgrn(S_len, TB=1024, ctx=None):
    CH = 64
    nch = TB // CH
    nblk = S_len // TB
    nc = bass.Bass("TRN2", target_bir_lowering=False) if ctx is None else ctx[0].nc
    def din(name, shape, dt):
        return _io(nc, ctx, name, list(shape), dt, "ExternalInput")
    qT = din("qT", [256, S_len], F32)
    fT = din("fT", [256, S_len], F32)
    v = din("v", [S_len, 256], BF16)
    gs = din("gs", [S_len, 256], F32)
    lg = din("lg", [128, 2, 2], F32)
    gn = din("gn", [64, 128], F32)
    smask = din("smask", [128, TB], F32)
    cmask = din("cmask", [64, 64], F32)
    c_ident = din("ident", [128, 128], BF16)
    o = _io(nc, ctx, "o", [S_len, 256], BF16, "ExternalOutput")
    with _SchedCtx(nc, ctx) as (S, C, own):
        ident = S.sb([128, 128], BF16, "ident_sb")
        sm = S.sb([128, TB], F32, "smask_sb")
        cm = S.sb([64, 64], F32, "cmask_sb")
        gnt = S.sb([64, 128], F32, "gn_sb")
        lgt = S.sb([128, 2, 2], F32, "lg_sb")
        lb = S.sb([128, 2], F32, "lb")
        oml = S.sb([128, 2], F32, "oml")
        noml = S.sb([128, 2], F32, "noml")
        for (t, src) in ((ident, c_ident), (sm, smask), (cm, cmask), (gnt, gn), (lgt, lg)):
            S.dma("sp", t[:], src, writes=[t])
        S.op("dve", lambda e: e.tensor_tensor(out=lb[:], in0=lgt[:, 1, :], in1=lgt[:, 0, :], op=ALU.subtract), reads=[lgt], writes=[lb])
        S.op("act", lambda e: e.activation(out=lb[:], in_=lb[:], func=AF.Sigmoid), reads=[lb], writes=[lb])
        S.op("dve", lambda e: e.tensor_scalar(out=oml[:], in0=lb[:], scalar1=-1.0, scalar2=1.0, op0=ALU.mult, op1=ALU.add), reads=[lb], writes=[oml])
        S.op("dve", lambda e: e.tensor_scalar(out=noml[:], in0=oml[:], scalar1=-1.0, scalar2=None, op0=ALU.mult), reads=[oml], writes=[noml])
        def mk(name, shape, dt):
            return [S.sb(shape, dt, f"{name}{h}") for h in range(2)]
        fr = mk("fr", [128, TB], F32)
        qf = mk("qf", [128, TB], F32)
        t1 = mk("t1", [128, TB], F32)
        t2 = mk("t2", [128, TB], F32)
        t3 = mk("t3", [128, TB], F32)
        cum = mk("cum", [128, TB], F32)
        Qh = mk("Qh", [128, TB], BF16)
        Qt = mk("Qt", [128, TB], BF16)
        Kh = mk("Kh", [128, TB], BF16)
        Kt = [[S.sb([128, TB], BF16, f"Kt{h}_{i}") for i in range(4)] for h in range(2)]
        aa = mk("aa", [128, nch], F32)
        vb = mk("vb", [64, nch, 128], BF16)
        gsb = mk("gsb", [64, nch, 128], F32)
        ob = mk("ob", [64, nch, 128], F32)
        osq = mk("osq", [64, nch, 128], F32)
        ofin = mk("ofin", [64, nch, 128], BF16)
        ssum = mk("ssum", [64, nch], F32)
        state = mk("state", [128, 128], F32)
        sref = [[S.sb([128, 128], BF16, f"sref{h}_{i}") for i in range(2)] for h in range(2)]
        ATm = [[S.sb([64, 64], BF16, f"ATm{h}_{i}") for i in range(2)] for h in range(2)]
        Ktok = [[S.sb([64, 128], BF16, f"Ktok{h}_{i}") for i in range(2)] for h in range(2)]
        pA = [S.ps([128, 512], F32, f"pA{h}") for h in range(2)]
        pP = [S.ps([128, 512], F32, f"pP{h}") for h in range(2)]
        pO = [S.ps([128, 512], F32, f"pO{h}") for h in range(2)]
        for h in range(2):
            S.op("pool", lambda e, h=h: e.memset(state[h][:], 0.0), writes=[state[h]])
        v_v = v.rearrange("(c p) e -> p c e", p=CH)
        gs_v = gs.rearrange("(c p) e -> p c e", p=CH)
        o_v = o.rearrange("(c p) e -> p c e", p=CH)
        out_toks = []
        CLAMP = 1e30
        for b in range(nblk):
            tsl = slice(b * TB, (b + 1) * TB)
            csl = slice(b * nch, (b + 1) * nch)
            for h in range(2):
                hs = slice(h * 128, (h + 1) * 128)
                S.dma("sp", fr[h][:], fT[hs, tsl], writes=[fr[h]])
                S.dma("sp", qf[h][:], qT[hs, tsl], writes=[qf[h]])
                S.dma("sp", vb[h][:], v_v[:, csl, hs], writes=[vb[h]])
                S.dma("sp", gsb[h][:], gs_v[:, csl, hs], writes=[gsb[h]])
            for h in range(2):
                f_, q_, a1, a2, a3, cu = fr[h], qf[h], t1[h], t2[h], t3[h], cum[h]
                S.op("act", lambda e, f_=f_: e.activation(out=f_[:], in_=f_[:], func=AF.Sigmoid), reads=[f_], writes=[f_])
                S.op("dve", lambda e, f_=f_, a1=a1, h=h: e.tensor_scalar(out=a1[:], in0=f_[:], scalar1=oml[:, h:h + 1], scalar2=lb[:, h:h + 1], op0=ALU.mult, op1=ALU.add), reads=[f_, oml, lb], writes=[a1])
                S.op("pool", lambda e, f_=f_, a2=a2, h=h: e.tensor_scalar(out=a2[:], in0=f_[:], scalar1=noml[:, h:h + 1], scalar2=oml[:, h:h + 1], op0=ALU.mult, op1=ALU.add), reads=[f_, oml, noml], writes=[a2])
                S.op("act", lambda e, a1=a1: e.activation(out=a1[:], in_=a1[:], func=AF.Ln), reads=[a1], writes=[a1])
                S.op("dve", lambda e, a1=a1, cu=cu: e.tensor_tensor_scan(out=cu[:], data0=sm[:], data1=a1[:], initial=0.0, op0=ALU.mult, op1=ALU.add), reads=[sm, a1], writes=[cu])
                cu_v = cu[:].rearrange("p (c s) -> p c s", s=CH)
                cu_v16 = cu[:].rearrange("p (c s) -> p c s", s=16)
                S.op("act", lambda e, a1=a1, cu=cu: e.activation(out=a1[:], in_=cu[:], func=AF.Exp), reads=[cu], writes=[a1])
                S.op("pool", lambda e, q_=q_, a1=a1, h=h: e.tensor_tensor(out=Qh[h][:], in0=q_[:], in1=a1[:], op=ALU.mult), reads=[q_, a1], writes=[Qh[h]])
                S.op("dve", lambda e, a3=a3, cu_v16=cu_v16: e.tensor_tensor(out=a3[:].rearrange("p (c s) -> p c s", s=16), in0=cu_v16, in1=cu_v16[:, :, 0:1].to_broadcast([128, TB // 16, 16]), op=ALU.subtract), reads=[cu], writes=[a3])
                S.op("act", lambda e, a3=a3: e.activation(out=a3[:], in_=a3[:], func=AF.Exp), reads=[a3], writes=[a3])
                S.op("pool", lambda e, q_=q_, a3=a3, h=h: e.tensor_tensor(out=Qt[h][:], in0=q_[:], in1=a3[:], op=ALU.mult), reads=[q_, a3], writes=[Qt[h]])
                S.op("dve", lambda e, a1=a1, cu_v=cu_v: e.tensor_tensor(out=a1[:].rearrange("p (c s) -> p c s", s=CH), in0=cu_v, in1=cu_v[:, :, CH - 1:CH].to_broadcast([128, nch, CH]), op=ALU.subtract), reads=[cu], writes=[a1])
                S.op("act", lambda e, a1=a1: e.activation(out=a1[:], in_=a1[:], func=AF.Exp, scale=-1.0), reads=[a1], writes=[a1])
                S.op("pool", lambda e, a1=a1, a2=a2, h=h: e.tensor_tensor(out=Kh[h][:], in0=a2[:], in1=a1[:], op=ALU.mult), reads=[a1, a2], writes=[Kh[h]])
                for i in range(4):
                    w_ = a3 if i % 2 == 0 else a1
                    S.op("dve", lambda e, w_=w_, cu_v=cu_v, i=i: e.tensor_tensor(out=w_[:].rearrange("p (c s) -> p c s", s=CH), in0=cu_v, in1=cu_v[:, :, 16 * i:16 * i + 1].to_broadcast([128, nch, CH]), op=ALU.subtract), reads=[cu], writes=[w_])
                    S.op("pool", lambda e, w_=w_: e.tensor_scalar(out=w_[:], in0=w_[:], scalar1=-80.0, scalar2=None, op0=ALU.max), reads=[w_], writes=[w_])
                    S.op("act", lambda e, w_=w_: e.activation(out=w_[:], in_=w_[:], func=AF.Exp, scale=-1.0), reads=[w_], writes=[w_])
                    S.op("dve", lambda e, w_=w_, a2=a2, h=h, i=i: e.tensor_tensor(out=Kt[h][i][:], in0=w_[:], in1=a2[:], op=ALU.mult), reads=[w_, a2], writes=[Kt[h][i]])
                S.op("act", lambda e, cu_v=cu_v, h=h: e.activation(out=aa[h][:], in_=cu_v[:, :, CH - 1], func=AF.Exp), reads=[cu], writes=[aa[h]])
            for c in range(nch):
                for h in range(2):
                    cs_ = slice(c * CH, (c + 1) * CH)
                    i2 = c % 2
                    for i in range(4):
                        S.op("pe", lambda e, h=h, cs_=cs_, i=i, c=c: e.matmul(pA[h][0:64, 16 * i:16 * i + 16], lhsT=Kt[h][i][:, cs_], rhs=Qt[h][:, c * CH + 16 * i:c * CH + 16 * i + 16], start=True, stop=True),
                             reads=[Kt[h][i], Qt[h]], writes=[pA[h]])
                    atm = ATm[h][i2]
                    S.op("dve", lambda e, h=h, atm=atm: e.tensor_tensor(out=atm[:], in0=pA[h][0:64, 0:64], in1=cm[:], op=ALU.mult), reads=[pA[h], cm], writes=[atm])
                    ktv = pA[h][0:64, 256:320].bitcast(BF16)
                    S.op("pe", lambda e, h=h, cs_=cs_, ktv=ktv: e.transpose(out=ktv, in_=Kh[h][:, cs_], identity=ident[:]), reads=[Kh[h], ident], writes=[pA[h]])
                    kt_ = Ktok[h][i2]
                    S.op("act", lambda e, ktv=ktv, kt_=kt_, h=h: e.activation(out=kt_[:], in_=ktv, func=AF.Copy), reads=[pA[h]], writes=[kt_])
                    sr = sref[h][i2]
                    S.op("act", lambda e, h=h, sr=sr: e.activation(out=sr[:], in_=state[h][:], func=AF.Copy), reads=[state[h]], writes=[sr])
                    S.op("pe", lambda e, h=h, atm=atm, c=c: e.matmul(pO[h][0:64, 0:128], lhsT=atm[:], rhs=vb[h][:, c, :], start=True, stop=False), reads=[atm, vb[h]], writes=[pO[h]])
                    S.op("pe", lambda e, h=h, sr=sr, cs_=cs_: e.matmul(pO[h][0:64, 0:128], lhsT=Qh[h][:, cs_], rhs=sr[:], start=False, stop=True), reads=[Qh[h], sr], writes=[pO[h]])
                    S.op("act", lambda e, h=h, c=c: e.activation(out=ob[h][:, c, :], in_=pO[h][0:64, 0:128], func=AF.Copy), reads=[pO[h]], writes=[ob[h]])
                    S.op("pe", lambda e, h=h, kt_=kt_, c=c: e.matmul(pP[h][:, 0:128], lhsT=kt_[:], rhs=vb[h][:, c, :], start=True, stop=True), reads=[kt_, vb[h]], writes=[pP[h]])
                    S.op("dve", lambda e, h=h, c=c: e.scalar_tensor_tensor(out=state[h][:], in0=state[h][:], scalar=aa[h][:, c:c + 1], in1=pP[h][:, 0:128], op0=ALU.mult, op1=ALU.add), reads=[state[h], aa[h], pP[h]], writes=[state[h]])
            for h in range(2):
                hs = slice(h * 128, (h + 1) * 128)
                S.op("pool", lambda e, h=h: e.tensor_tensor(out=osq[h][:], in0=ob[h][:], in1=ob[h][:], op=ALU.mult), reads=[ob[h]], writes=[osq[h]])
                S.op("dve", lambda e, h=h: e.tensor_reduce(out=ssum[h][:], in_=osq[h][:], axis=AX.X, op=ALU.add), reads=[osq[h]], writes=[ssum[h]])
                S.op("act", lambda e, h=h: e.activation(out=ssum[h][:], in_=ssum[h][:], func=AF.Sqrt, scale=1.0 / 128, bias=EPS), reads=[ssum[h]], writes=[ssum[h]])
                S.op("dve", lambda e, h=h: e.reciprocal(out=ssum[h][:], in_=ssum[h][:]), reads=[ssum[h]], writes=[ssum[h]])
                S.op("dve", lambda e, h=h: e.tensor_tensor(out=ob[h][:], in0=ob[h][:], in1=ssum[h][:].unsqueeze(2).to_broadcast([64, nch, 128]), op=ALU.mult), reads=[ob[h], ssum[h]], writes=[ob[h]])
                S.op("pool", lambda e, h=h: e.tensor_tensor(out=osq[h][:], in0=gsb[h][:], in1=gnt[:].unsqueeze(1).to_broadcast([64, nch, 128]), op=ALU.mult), reads=[gsb[h], gnt], writes=[osq[h]])
                S.op("dve", lambda e, h=h: e.tensor_tensor(out=ofin[h][:], in0=ob[h][:], in1=osq[h][:], op=ALU.mult), reads=[ob[h], osq[h]], writes=[ofin[h]])
                out_toks.append(S.dma("sp", o_v[:, csl, hs], ofin[h][:], reads=[ofin[h]]))
        S.final_wait("sp", out_toks)
        if own:
            S.emit()
    return nc


def hgrn_consts(TB=1024):
    sm = np.ones((128, TB), np.float32)
    sm[:, ::64] = 0.0
    s = np.arange(64)[:, None]
    t = np.arange(64)[None, :]
    return {"smask": sm, "cmask": (s <= t).astype(np.float32), "ident": np.eye(128, dtype=np.float32).astype(NPBF)}


B_, S_, TPC = 2, 16384, 4096
_NC_CACHE = {}


def _prog(key, fn):
    if key not in _NC_CACHE:
        _NC_CACHE[key] = fn()
    return _NC_CACHE[key]


def _run(nc, in_maps):
    res = run_bass_kernel_spmd(nc, in_maps, core_ids=list(range(NCORES)))
    return res.results


def kernel(**inputs):
    return kernel5(**inputs)


_NOCC = False
A1_ROWS = 2608


def nsain2_phase(ctx, T_tok, NT=256):
    nc = ctx[0].nc
    A = ctx[2]
    xT, w_fm, ng, cs, all1 = A["xT"], A["w_fm"], A["ng"], A["cs"], A["all1"]
    nt = NT
    ntiles = T_tok // nt
    NF = 34
    with _SchedCtx(nc, ctx) as (S, C, own):
        wf = S.sb([128, NCH, NF * 128 + 48], BF16, "wf")
        g = S.sb([128, 8], F32, "gains")
        S.dma("sp", g[:], ng, writes=[g])
        w_fm_v = w_fm.rearrange("(c p) f -> p c f", p=128)
        for c in range(NCH):
            S.dma("pool", wf[:, c, :], w_fm_v[:, c, :], writes=[wf])
        xs = [S.sb([128, NCH, nt], F32, f"x{i}") for i in range(2)]
        cst = [S.sb([128, 2, nt], F32, f"cs{i}") for i in range(2)]
        hn = S.sb([128, NCH, nt], BF16, "hn")
        sq = [S.sb([128, nt], BF16, f"sq{i}") for i in range(2)]
        rstd = S.sb([128, nt], F32, "rstd")
        t1 = [S.sb([128, nt], F32, f"t1_{i}") for i in range(2)]
        t2 = [S.sb([128, nt], F32, f"t2_{i}") for i in range(2)]
        oall = [S.sb([128, 21, nt], BF16, f"oall{i}") for i in range(2)]
        pstat = S.ps([128, 512], F32, "pstat")
        pa = [S.ps([128, 512], F32, f"pa{i}") for i in range(2)]
        pb = [S.ps([128, 512], F32, f"pb{i}") for i in range(2)]
        xT_v = xT.rearrange("(c p) t -> p c t", p=128)
        a_v = all1[0:2560, :].rearrange("(c p) t -> p c t", p=128)
        toks = []

        def load(i):
            S.dma("sp", xs[i % 2][:], xT_v[:, :, i * nt:(i + 1) * nt], writes=[xs[i % 2]])
            S.dma("sp", cst[i % 2][:], cs[:, :, i * nt:(i + 1) * nt], writes=[cst[i % 2]])
        load(0)
        for i in range(ntiles):
            x = xs[i % 2]
            cs_t = cst[i % 2]
            if i + 1 < ntiles:
                load(i + 1)

            def sqf(c, x=x):
                s_ = sq[c % 2]
                S.op("pool", lambda e, s_=s_, c=c: e.tensor_tensor(out=s_[:], in0=x[:, c, :], in1=x[:, c, :], op=ALU.mult), reads=[x], writes=[s_])
                return s_, s_[:]
            rms_stats(S, C, sqf, NCH, pstat, rstd, nt, D)
            for c in range(NCH):
                S.op("dve", lambda e, c=c, x=x: e.scalar_tensor_tensor(out=hn[:, c, :], in0=x[:, c, :], scalar=g[:, c:c + 1], in1=rstd[:], op0=ALU.mult, op1=ALU.mult),
                     reads=[x, g, rstd], writes=[hn])
            oq = oall[i % 2]
            for j in range(NSA_NROPE):
                a, b = pa[j % 2], pb[j % 2]
                for c in range(NCH):
                    S.op("pe", lambda e, a=a, c=c, j=j: e.matmul(a[:, 0:nt], lhsT=wf[:, c, j * 128:(j + 1) * 128], rhs=hn[:, c, :], start=(c == 0), stop=(c == NCH - 1)),
                         reads=[wf, hn], writes=[a])
                for c in range(NCH):
                    S.op("pe", lambda e, b=b, c=c, j=j: e.matmul(b[:, 0:nt], lhsT=wf[:, c, (14 + j) * 128:(15 + j) * 128], rhs=hn[:, c, :], start=(c == 0), stop=(c == NCH - 1)),
                         reads=[wf, hn], writes=[b])
                u1, u2 = t1[j % 2], t2[j % 2]
                S.op("dve", lambda e, a=a, u1=u1, cs_t=cs_t: e.tensor_tensor(out=u1[:], in0=a[:, 0:nt], in1=cs_t[:, 0, :], op=ALU.mult), reads=[a, cs_t], writes=[u1])
                S.op("dve", lambda e, b=b, u2=u2, cs_t=cs_t: e.tensor_tensor(out=u2[:], in0=b[:, 0:nt], in1=cs_t[:, 1, :], op=ALU.mult), reads=[b, cs_t], writes=[u2])
                S.op("pool", lambda e, u1=u1, u2=u2, j=j, oq=oq: e.tensor_tensor(out=oq[:, j, :], in0=u1[:], in1=u2[:], op=ALU.add), reads=[u1, u2], writes=[oq])
            for j in range(6):
                a = pa[j % 2]
                for c in range(NCH):
                    S.op("pe", lambda e, a=a, c=c, j=j: e.matmul(a[:, 0:nt], lhsT=wf[:, c, (28 + j) * 128:(29 + j) * 128], rhs=hn[:, c, :], start=(c == 0), stop=(c == NCH - 1)),
                         reads=[wf, hn], writes=[a])
                S.op("act", lambda e, a=a, j=j, oq=oq: e.activation(out=oq[:, 14 + j, :], in_=a[:, 0:nt], func=AF.Copy), reads=[a], writes=[oq])
            a = pb[0]
            for c in range(NCH):
                S.op("pe", lambda e, a=a, c=c: e.matmul(a[0:48, 0:nt], lhsT=wf[:, c, NF * 128:NF * 128 + 48], rhs=hn[:, c, :], start=(c == 0), stop=(c == NCH - 1)),
                     reads=[wf, hn], writes=[a])
            S.op("act", lambda e, a=a, oq=oq: e.activation(out=oq[0:48, 20, :], in_=a[0:48, 0:nt], func=AF.Sigmoid), reads=[a], writes=[oq])
            sl = slice(i * nt, (i + 1) * nt)
            for c0 in range(0, 20, 5):
                toks.append(S.dma("sp", a_v[:, c0:c0 + 5, sl], oq[:, c0:c0 + 5, :], reads=[oq]))
            toks.append(S.dma("sp", all1[2560:2608, sl], oq[0:48, 20, :], reads=[oq]))
        S.final_wait("sp", toks)


def nsa_w_layout2(w):
    w = np.asarray(w, np.float32)
    rope_cols = np.concatenate([np.arange(0, 1024), np.arange(1024, 1280), np.arange(1536, 1792), np.arange(2048, 2304)])
    sw = rope_cols.reshape(-1, 2, 32)[:, ::-1, :].reshape(-1)
    return np.ascontiguousarray(np.concatenate([w[:, rope_cols], w[:, sw], w[:, 1280:1536], w[:, 1792:2048], w[:, 2304:2560], w[:, 2560:2608]], axis=1))


def nsaprep_phase(ctx, S_len, TPC_):
    nc = ctx[0].nc
    A = ctx[2]
    gat = A["gat1"]
    nk = S_len // TPC_
    CB = 512
    with _SchedCtx(nc, ctx) as (S, C, own):
        selq = S.sb([128, 8, 4, 64], BF16, "selq")
        selk = S.sb([128, 2, 64], BF16, "selk")
        selg = S.sb([48, 12], BF16, "selg")
        S.dma("sp", selq[:], A["selq"], writes=[selq])
        S.dma("sp", selk[:], A["selk"], writes=[selk])
        S.dma("sp", selg[:], A["selg"], writes=[selg])
        zt = S.sb([64, 16], BF16, "zt")
        S.op("pool", lambda e: e.memset(zt[:], 0.0), writes=[zt])
        toks = []
        for nm in ("kc2", "vc2"):
            toks.append(S.dma("sp", A[nm][64:128, S_len - 16:S_len], zt[:], reads=[zt]))
        X = [S.sb([128, 21, CB], BF16, f"X{i}") for i in range(2)]
        stq = [S.sb([64, 4, CB], BF16, f"stq{i}") for i in range(2)]
        stk = [S.sb([64, 4, CB], BF16, f"stk{i}") for i in range(2)]
        stv = [S.sb([128, 2, 4, 64], BF16, f"stv{i}") for i in range(2)]
        stg = [S.sb([128, 4, 12], F32, f"stg{i}") for i in range(2)]
        pq = [S.ps([128, 512], F32, f"pq{i}") for i in range(2)]
        pv = [S.ps([128, 512], F32, f"pv{i}") for i in range(2)]
        blocks = [(k, cb) for k in range(nk) for cb in range(TPC_ // CB)]

        def load(n):
            k, cb = blocks[n]
            x = X[n % 2]
            src = gat[k * A1_ROWS:k * A1_ROWS + 2560, cb * CB:(cb + 1) * CB].rearrange("(c p) t -> p c t", p=128)
            for c0 in range(0, 20, 5):
                S.dma("sp", x[:, c0:c0 + 5, :], src[:, c0:c0 + 5, :], reads=[A["GAT1"]], writes=[x])
            S.dma("sp", x[0:48, 20, :], gat[k * A1_ROWS + 2560:k * A1_ROWS + 2608, cb * CB:(cb + 1) * CB], reads=[A["GAT1"]], writes=[x])
        load(0)
        cnt = 0
        for n, (k, cb) in enumerate(blocks):
            x = X[n % 2]
            if n + 1 < len(blocks):
                load(n + 1)
            t0 = k * TPC_ + cb * CB
            sq_, sk_, sv_, sg_ = stq[n % 2], stk[n % 2], stv[n % 2], stg[n % 2]
            for h in range(4):
                p = pq[cnt % 2]
                cnt += 1
                for c in range(8):
                    S.op("pe", lambda e, p=p, c=c, h=h, x=x: e.matmul(p[0:64, 0:CB], lhsT=selq[:, c, h, :], rhs=x[:, c, :], start=(c == 0), stop=(c == 7)), reads=[selq, x], writes=[p])
                S.op("act" if h % 2 else "dve", (lambda e, p=p, h=h, sq_=sq_: e.activation(out=sq_[:, h, :], in_=p[0:64, 0:CB], func=AF.Copy)) if h % 2 else (lambda e, p=p, h=h, sq_=sq_: e.tensor_copy(out=sq_[:, h, :], in_=p[0:64, 0:CB])), reads=[p], writes=[sq_])
            for i4, c0 in enumerate((8, 10, 12, 14)):
                p = pq[cnt % 2]
                cnt += 1
                for c in range(2):
                    S.op("pe", lambda e, p=p, c=c, c0=c0, x=x: e.matmul(p[0:64, 0:CB], lhsT=selk[:, c, :], rhs=x[:, c0 + c, :], start=(c == 0), stop=(c == 1)), reads=[selk, x], writes=[p])
                S.op("act" if i4 % 2 else "dve", (lambda e, p=p, i4=i4, sk_=sk_: e.activation(out=sk_[:, i4, :], in_=p[0:64, 0:CB], func=AF.Copy)) if i4 % 2 else (lambda e, p=p, i4=i4, sk_=sk_: e.tensor_copy(out=sk_[:, i4, :], in_=p[0:64, 0:CB])), reads=[p], writes=[sk_])
            for wch, c0 in enumerate((16, 18)):
                p = pv[wch]
                for sub in range(4):
                    for c in range(2):
                        S.op("pe", lambda e, p=p, c=c, c0=c0, sub=sub, x=x: e.matmul(p[:, sub * 64:(sub + 1) * 64], lhsT=x[:, c0 + c, sub * 128:(sub + 1) * 128], rhs=selk[:, c, :], start=(c == 0), stop=(c == 1)), reads=[selk, x], writes=[p])
                S.op("dve", lambda e, p=p, wch=wch, sv_=sv_: e.tensor_copy(out=sv_[:, wch, :, :], in_=p[:, 0:256].rearrange("p (s d) -> p s d", s=4)), reads=[p], writes=[sv_])
            p = pv[0]
            for sub in range(4):
                S.op("pe", lambda e, p=p, sub=sub, x=x: e.matmul(p[:, 256 + sub * 12:256 + (sub + 1) * 12], lhsT=x[0:48, 20, sub * 128:(sub + 1) * 128], rhs=selg[:], start=True, stop=True), reads=[selg, x], writes=[p])
            S.op("act", lambda e, p=p, sg_=sg_: e.activation(out=sg_[:], in_=p[:, 256:304].rearrange("p (s d) -> p s d", s=4), func=AF.Copy), reads=[p], writes=[sg_])
            toks.append(S.dma("sp", A["qT"][:, :, t0:t0 + CB], sq_[:], reads=[sq_]))
            toks.append(S.dma("sp", A["kc2"][0:64, t0:t0 + CB], sk_[:, 0, :], reads=[sk_]))
            toks.append(S.dma("sp", A["ksT"][:, t0:t0 + CB], sk_[:, 1, :], reads=[sk_]))
            toks.append(S.dma("sp", A["kwT"][:, t0:t0 + CB], sk_[:, 2, :], reads=[sk_]))
            toks.append(S.dma("sp", A["vc2"][0:64, t0:t0 + CB], sk_[:, 3, :], reads=[sk_]))
            for (nm, i4) in (("kc2", 0), ("vc2", 3)):
                if t0 == 0:
                    toks.append(S.dma("sp", A[nm][64:128, 0:CB - 16], sk_[:, i4, 16:CB], reads=[sk_]))
                else:
                    toks.append(S.dma("sp", A[nm][64:128, t0 - 16:t0 + CB - 16], sk_[:, i4, :], reads=[sk_]))
            toks.append(S.dma("sp", A["vs"][t0:t0 + CB, :].rearrange("(s p) d -> p s d", p=128), sv_[:, 0, :, :], reads=[sv_]))
            toks.append(S.dma("sp", A["vw"][t0:t0 + CB, :].rearrange("(s p) d -> p s d", p=128), sv_[:, 1, :, :], reads=[sv_]))
            toks.append(S.dma("sp", A["gates"][t0:t0 + CB, :].rearrange("(s p) d -> p s d", p=128), sg_[:], reads=[sg_]))
        S.final_wait("sp", toks)


def nsaprep_sel(g):
    selq = np.zeros((128, 8, 4, 64), np.float32)
    for h in range(4):
        hd = 4 * g + h
        c, off = hd // 2, (hd % 2) * 64
        selq[off + np.arange(64), c, h, np.arange(64)] = 1.0
    selk = np.zeros((128, 2, 64), np.float32)
    selk[(g % 2) * 64 + np.arange(64), g // 2, np.arange(64)] = 1.0
    selg = np.zeros((48, 12), np.float32)
    selg[12 * g + np.arange(12), np.arange(12)] = 1.0
    return {"selq": selq.astype(NPBF), "selk": selk.astype(NPBF), "selg": selg.astype(NPBF)}


def out2_phase(ctx, S_len, TPC_, NT=256):
    nc = ctx[0].nc
    A = ctx[2]
    xT, w, ng, yT, ogat = A["xT"], A["w"], A["ng"], A["yT"], A["ogat"]
    nt = NT
    ntiles = TPC_ // nt
    nsub = nt // 128
    with _SchedCtx(nc, ctx) as (S, C, own):
        wb = S.sb([128, NCH, D], BF16, "wb")
        g = S.sb([128, 8], F32, "gains")
        wsel = S.sb([128, 4, 128], BF16, "wsel")
        S.dma("sp", g[:], ng, writes=[g])
        S.dma("sp", wsel[:], A["wsel"], writes=[wsel])
        w_v = w.rearrange("(c p) f -> p c f", p=128)
        for c in range(NCH):
            S.dma("pool", wb[:, c, :], w_v[:, c, :], writes=[wb])
        xs = [S.sb([128, NCH, nt], F32, f"x{i}") for i in range(2)]
        cand = [S.sb([128, 4, 4, 256], BF16, f"cand{i}") for i in range(2)]
        os_ = S.sb([128, NCH, nt], BF16, "os")
        y = S.sb([128, NCH, nt], F32, "y")
        sq = [S.sb([128, nt], BF16, f"sq{i}") for i in range(2)]
        rstd = S.sb([128, nt], F32, "rstd")
        tmp = [S.sb([128, nt], F32, f"tmp{i}") for i in range(2)]
        pstat = S.ps([128, 512], F32, "pstat")
        py = [S.ps([128, 512], F32, f"py{i}") for i in range(2)]
        psel = [S.ps([128, 512], F32, f"psel{i}") for i in range(2)]
        xT_v = xT.rearrange("(c p) t -> p c t", p=128)
        yT_v = yT.rearrange("(c p) t -> p c t", p=128)
        og_v = ogat.rearrange("(g r t) f -> t g r f", g=4, r=S_len // TPC_)
        toks = []
        subs = [(i, s_) for i in range(ntiles) for s_ in range(nsub)]

        def load_x(i):
            for c0 in range(0, NCH, 4):
                S.dma("sp", xs[i % 2][:, c0:c0 + 4, :], xT_v[:, c0:c0 + 4, i * nt:(i + 1) * nt], writes=[xs[i % 2]])

        def load_c(n):
            i, s_ = subs[n]
            t0 = i * nt + s_ * 128
            for gg in range(4):
                S.dma("sp", cand[n % 2][:, gg, :, :], og_v[t0:t0 + 128, gg, :, :], reads=[A["OGAT"]], writes=[cand[n % 2]])
        load_x(0)
        load_c(0)
        n = 0
        for i in range(ntiles):
            x = xs[i % 2]
            if i + 1 < ntiles:
                load_x(i + 1)
            for s_ in range(nsub):
                cd = cand[n % 2]
                if n + 1 < len(subs):
                    load_c(n + 1)
                n += 1
                for half in range(2):
                    p = psel[half]
                    for q4 in range(4):
                        ch = half * 4 + q4
                        gg, fc = ch // 2, ch % 2
                        for r_ in range(4):
                            S.op("pe", lambda e, p=p, q4=q4, gg=gg, fc=fc, r_=r_, cd=cd: e.matmul(p[:, q4 * 128:(q4 + 1) * 128], lhsT=cd[:, gg, r_, fc * 128:(fc + 1) * 128], rhs=wsel[:, r_, :], start=(r_ == 0), stop=(r_ == 3)),
                                 reads=[cd, wsel], writes=[p])
                    if half == 0:
                        S.op("act", lambda e, p=p, s_=s_: e.activation(out=os_[:, 0:4, s_ * 128:(s_ + 1) * 128], in_=p[:, 0:512].rearrange("p (c t) -> p c t", c=4), func=AF.Copy), reads=[p], writes=[os_])
                    else:
                        S.op("dve", lambda e, p=p, s_=s_: e.tensor_copy(out=os_[:, 4:8, s_ * 128:(s_ + 1) * 128], in_=p[:, 0:512].rearrange("p (c t) -> p c t", c=4)), reads=[p], writes=[os_])
            for m in range(NCH):
                p = py[m % 2]
                for c in range(NCH):
                    S.op("pe", lambda e, p=p, c=c, m=m: e.matmul(p[:, 0:nt], lhsT=wb[:, c, m * 128:(m + 1) * 128], rhs=os_[:, c, :], start=(c == 0), stop=(c == NCH - 1)),
                         reads=[wb, os_], writes=[p])
                S.op("act", lambda e, p=p, m=m: e.activation(out=y[:, m, :], in_=p[:, 0:nt], func=AF.Copy), reads=[p], writes=[y])

            def sqy(c):
                s2 = sq[c % 2]
                S.op("pool", lambda e, s2=s2, c=c: e.tensor_tensor(out=s2[:], in0=y[:, c, :], in1=y[:, c, :], op=ALU.mult), reads=[y], writes=[s2])
                return s2, s2[:]
            rms_stats(S, C, sqy, NCH, pstat, rstd, nt, D)
            for m in range(NCH):
                t = tmp[m % 2]
                S.op("dve", lambda e, m=m, t=t: e.scalar_tensor_tensor(out=t[:], in0=y[:, m, :], scalar=g[:, m:m + 1], in1=rstd[:], op0=ALU.mult, op1=ALU.mult),
                     reads=[y, g, rstd], writes=[t])
                S.op("pool", lambda e, m=m, t=t, x=x: e.tensor_tensor(out=x[:, m, :], in0=t[:], in1=x[:, m, :], op=ALU.add), reads=[t, x], writes=[x])
            for c0 in range(0, NCH, 4):
                toks.append(S.dma("sp", yT_v[:, c0:c0 + 4, i * nt:(i + 1) * nt], x[:, c0:c0 + 4, :], reads=[x]))
        S.final_wait("sp", toks)


def out2_sel(r):
    w = np.zeros((128, 4, 128), np.float32)
    w[np.arange(128), r, np.arange(128)] = 1.0
    return w.astype(NPBF)


def hgrnin2_phase(ctx, T_tok, NT=256):
    nc = ctx[0].nc
    A = ctx[2]
    xT, w, ng, a3a, a3b = A["xT"], A["w"], A["ng"], A["all3a"], A["all3b"]
    nt = NT
    ntiles = T_tok // nt
    with _SchedCtx(nc, ctx) as (S, C, own):
        wb = S.sb([128, NCH, 4096], BF16, "wb")
        g = S.sb([128, 8], F32, "gains")
        S.dma("sp", g[:], ng, writes=[g])
        w_v = w.rearrange("(c p) f -> p c f", p=128)
        for c in range(NCH):
            S.dma("pool", wb[:, c, :], w_v[:, c, :], writes=[wb])
        xs = [S.sb([128, NCH, nt], F32, f"x{i}") for i in range(2)]
        hn = S.sb([128, NCH, nt], BF16, "hn")
        sq = [S.sb([128, nt], BF16, f"sq{i}") for i in range(2)]
        rstd = S.sb([128, nt], F32, "rstd")
        of = [S.sb([128, 8, nt], F32, f"of{i}") for i in range(2)]
        ob = [S.sb([128, 24, nt], BF16, f"ob{i}") for i in range(2)]
        pstat = S.ps([128, 512], F32, "pstat")
        pa = [S.ps([128, 512], F32, f"pa{i}") for i in range(3)]
        xT_v = xT.rearrange("(c p) t -> p c t", p=128)
        a_v = a3a.rearrange("(c p) t -> p c t", p=128)
        b_v = a3b.rearrange("(c p) t -> p c t", p=128)
        toks = []

        def load(i):
            for c0 in range(0, NCH, 4):
                S.dma("sp", xs[i % 2][:, c0:c0 + 4, :], xT_v[:, c0:c0 + 4, i * nt:(i + 1) * nt], writes=[xs[i % 2]])
        load(0)
        for i in range(ntiles):
            x = xs[i % 2]
            if i + 1 < ntiles:
                load(i + 1)

            def sqf(c, x=x):
                s_ = sq[c % 2]
                S.op("pool", lambda e, s_=s_, c=c: e.tensor_tensor(out=s_[:], in0=x[:, c, :], in1=x[:, c, :], op=ALU.mult), reads=[x], writes=[s_])
                return s_, s_[:]
            rms_stats(S, C, sqf, NCH, pstat, rstd, nt, D)
            for c in range(NCH):
                S.op("dve", lambda e, c=c, x=x: e.scalar_tensor_tensor(out=hn[:, c, :], in0=x[:, c, :], scalar=g[:, c:c + 1], in1=rstd[:], op0=ALU.mult, op1=ALU.mult),
                     reads=[x, g, rstd], writes=[hn])
            o_f, o_b = of[i % 2], ob[i % 2]
            for j in range(32):
                a = pa[j % 3]
                for c in range(NCH):
                    S.op("pe", lambda e, a=a, c=c, j=j: e.matmul(a[:, 0:nt], lhsT=wb[:, c, j * 128:(j + 1) * 128], rhs=hn[:, c, :], start=(c == 0), stop=(c == NCH - 1)),
                         reads=[wb, hn], writes=[a])
                if j < 8:
                    S.op("act", lambda e, a=a, j=j, o_b=o_b: e.activation(out=o_b[:, j, :], in_=a[:, 0:nt], func=AF.Silu), reads=[a], writes=[o_b])
                elif j < 16:
                    S.op("dve", lambda e, a=a, j=j, o_f=o_f: e.tensor_copy(out=o_f[:, j - 8, :], in_=a[:, 0:nt]), reads=[a], writes=[o_f])
                elif j < 24:
                    S.op("dve", lambda e, a=a, j=j, o_b=o_b: e.tensor_copy(out=o_b[:, j - 8, :], in_=a[:, 0:nt]), reads=[a], writes=[o_b])
                else:
                    S.op("act", lambda e, a=a, j=j, o_b=o_b: e.activation(out=o_b[:, j - 8, :], in_=a[:, 0:nt], func=AF.Silu), reads=[a], writes=[o_b])
            sl = slice(i * nt, (i + 1) * nt)
            for c0 in range(0, 8, 4):
                toks.append(S.dma("sp", a_v[:, c0:c0 + 4, sl], o_f[:, c0:c0 + 4, :], reads=[o_f]))
            for c0 in range(0, 24, 6):
                toks.append(S.dma("sp", b_v[:, c0:c0 + 6, sl], o_b[:, c0:c0 + 6, :], reads=[o_b]))
        S.final_wait("sp", toks)


def hgrnprep_phase(ctx, S_len, TPC_):
    nc = ctx[0].nc
    A = ctx[2]
    ga, gb = A["gat3a"], A["gat3b"]
    nk = S_len // TPC_
    CB = 512
    with _SchedCtx(nc, ctx) as (S, C, own):
        wI = S.sb([128, 8, 2, 128], BF16, "wI")
        wsc = S.sb([128, 16], F32, "wsc")
        S.dma("sp", wI[:], A["wI"], writes=[wI])
        S.dma("sp", wsc[:], A["wsc"], writes=[wsc])
        Xb = [S.sb([128, 24, CB], BF16, f"Xb{i}") for i in range(2)]
        Xf = [S.sb([128, 8, CB], F32, f"Xf{i}") for i in range(2)]
        sq_ = [S.sb([128, 2, CB], F32, f"sq{i}") for i in range(2)]
        sf_ = [S.sb([128, 2, CB], F32, f"sf{i}") for i in range(2)]
        sv_ = [S.sb([128, 4, 256], BF16, f"sv{i}") for i in range(2)]
        sg_ = [S.sb([128, 4, 256], F32, f"sg{i}") for i in range(2)]
        pq = [S.ps([128, 512], F32, f"pq{i}") for i in range(2)]
        pv = [S.ps([128, 512], F32, f"pv{i}") for i in range(4)]
        blocks = [(k, cb) for k in range(nk) for cb in range(TPC_ // CB)]
        toks = []

        def load(n):
            k, cb = blocks[n]
            sb_ = gb[k * 3072:(k + 1) * 3072, cb * CB:(cb + 1) * CB].rearrange("(c p) t -> p c t", p=128)
            sa_ = ga[k * 1024:(k + 1) * 1024, cb * CB:(cb + 1) * CB].rearrange("(c p) t -> p c t", p=128)
            for c0 in range(0, 24, 6):
                S.dma("sp", Xb[n % 2][:, c0:c0 + 6, :], sb_[:, c0:c0 + 6, :], reads=[A["GAT3B"]], writes=[Xb[n % 2]])
            for c0 in range(0, 8, 2):
                S.dma("sp", Xf[n % 2][:, c0:c0 + 2, :], sa_[:, c0:c0 + 2, :], reads=[A["GAT3A"]], writes=[Xf[n % 2]])
        load(0)
        for n, (k, cb) in enumerate(blocks):
            xb, xf = Xb[n % 2], Xf[n % 2]
            if n + 1 < len(blocks):
                load(n + 1)
            t0 = k * TPC_ + cb * CB
            q_o, f_o, v_o, g_o = sq_[n % 2], sf_[n % 2], sv_[n % 2], sg_[n % 2]
            for hh in range(2):
                p = pq[hh]
                for c in range(8):
                    S.op("pe", lambda e, p=p, c=c, hh=hh, xb=xb: e.matmul(p[:, 0:CB], lhsT=wI[:, c, hh, :], rhs=xb[:, c, :], start=(c == 0), stop=(c == 7)), reads=[wI, xb], writes=[p])
                S.op("act", lambda e, p=p, hh=hh, q_o=q_o: e.activation(out=q_o[:, hh, :], in_=p[:, 0:CB], func=AF.Copy), reads=[p], writes=[q_o])
                for c in range(8):
                    if c == 0:
                        S.op("dve", lambda e, hh=hh, xf=xf, f_o=f_o: e.tensor_scalar(out=f_o[:, hh, :], in0=xf[:, 0, :], scalar1=wsc[:, hh:hh + 1], scalar2=None, op0=ALU.mult), reads=[xf, wsc], writes=[f_o])
                    else:
                        S.op("dve", lambda e, hh=hh, c=c, xf=xf, f_o=f_o: e.scalar_tensor_tensor(out=f_o[:, hh, :], in0=xf[:, c, :], scalar=wsc[:, 2 * c + hh:2 * c + hh + 1], in1=f_o[:, hh, :], op0=ALU.mult, op1=ALU.add), reads=[xf, wsc, f_o], writes=[f_o])
                for wch in range(2):
                    p = pv[wch * 2 + hh]
                    for sub in range(4):
                        for c in range(8):
                            S.op("pe", lambda e, p=p, c=c, hh=hh, sub=sub, wch=wch, xb=xb: e.matmul(p[:, sub * 128:(sub + 1) * 128], lhsT=xb[:, 8 + 8 * wch + c, sub * 128:(sub + 1) * 128], rhs=wI[:, c, hh, :], start=(c == 0), stop=(c == 7)), reads=[wI, xb], writes=[p])
                    dst = v_o if wch == 0 else g_o
                    if wch == 0:
                        S.op("act", lambda e, p=p, hh=hh, dst=dst: e.activation(out=dst[:, :, hh * 128:(hh + 1) * 128], in_=p[:, 0:512].rearrange("p (s d) -> p s d", s=4), func=AF.Copy), reads=[p], writes=[dst])
                    else:
                        S.op("dve", lambda e, p=p, hh=hh, dst=dst: e.tensor_copy(out=dst[:, :, hh * 128:(hh + 1) * 128], in_=p[:, 0:512].rearrange("p (s d) -> p s d", s=4)), reads=[p], writes=[dst])
            toks.append(S.dma("sp", A["qT"][:, t0:t0 + CB].rearrange("(h p) t -> p h t", p=128), q_o[:], reads=[q_o]))
            toks.append(S.dma("sp", A["fT"][:, t0:t0 + CB].rearrange("(h p) t -> p h t", p=128), f_o[:], reads=[f_o]))
            toks.append(S.dma("sp", A["v"][t0:t0 + CB, :].rearrange("(s p) d -> p s d", p=128), v_o[:], reads=[v_o]))
            toks.append(S.dma("sp", A["gs"][t0:t0 + CB, :].rearrange("(s p) d -> p s d", p=128), g_o[:], reads=[g_o]))
        S.final_wait("sp", toks)


def hgrnprep_sel(r2):
    wI = np.zeros((128, 8, 2, 128), np.float32)
    wsc = np.zeros((128, 16), np.float32)
    for hh in range(2):
        c = 2 * r2 + hh
        wI[np.arange(128), c, hh, np.arange(128)] = 1.0
        wsc[:, 2 * c + hh] = 1.0
    return {"wI": wI.astype(NPBF), "wsc": wsc}


def build_fused(S_len, TPC_):
    nc = bass.Bass("TRN2", target_bir_lowering=False)
    nk = S_len // TPC_
    groups = [[0, 1, 2, 3], [4, 5, 6, 7]]
    I32 = mybir.dt.int32

    def ein(name, shape, dt):
        return nc.dram_tensor(name, list(shape), dt, kind="ExternalInput").ap()

    def itn(name, shape, dt):
        return nc.dram_tensor(name, list(shape), dt).ap()
    nsel = S_len // 64
    ncmp = S_len // 16 - 1
    nct = (ncmp + 127) // 128
    bk = min(128, nsel)
    E = {}
    E["xT"] = ein("xT", [D, TPC_], F32)
    for l in range(2):
        E[f"pT{l}"] = ein(f"pT{l}", [256, TPC_], F32)
        E[f"ng{l}"] = ein(f"ng{l}", [128, 64], F32)
        for k in range(2):
            E[f"fwi{l}{k}"] = ein(f"fwi{l}{k}", [D, 2 * DFF], F32)
            E[f"fwo{l}{k}"] = ein(f"fwo{l}{k}", [DFF, D], F32)
        E[f"wp{l}"] = ein(f"wp{l}", [256, D], F32)
        E[f"wg{l}"] = ein(f"wg{l}", [D, D], F32)
    E["nsa_wfm"] = ein("nsa_wfm", [D, 34 * 128 + 48], F32)
    E["nsa_wo"] = ein("nsa_wo", [D, D], F32)
    E["cs"] = ein("cs", [128, 2, TPC_], F32)
    E["hg_w"] = ein("hg_w", [D, 4096], F32)
    E["hg_wo"] = ein("hg_wo", [D, D], F32)
    for nm, shp, dt in (("selq", [128, 8, 4, 64], BF16), ("selk", [128, 2, 64], BF16), ("selg", [48, 12], BF16), ("wsel", [128, 4, 128], BF16),
                        ("wI", [128, 8, 2, 128], BF16), ("wsc", [128, 16], F32),
                        ("w1s", [2, 128, 16, 64], F32), ("w2", [2, 64, 64], F32), ("pos2", [2, 128, 16], F32),
                        ("ident", [128, 128], BF16), ("tri_le", [128, 128], BF16), ("tri_gt", [128, 128], BF16), ("cb", [128, 17, 128], BF16),
                        ("ebig", [bk, (bk // 2) * 128], BF16), ("mmat", [128, nct, nsel], BF16), ("fb", [128, 2 * nsel], F32),
                        ("lg", [128, 2, 2], F32), ("gn", [64, 128], F32), ("smask", [128, 1024], F32), ("cmask", [64, 64], F32)):
        E[nm] = ein(nm, shp, dt)
    yT = nc.dram_tensor("yT", [D, TPC_], F32, kind="ExternalOutput").ap()
    xa = itn("x_a", [D, TPC_], F32)
    xb_ = itn("x_b", [D, TPC_], F32)
    all1 = itn("all1", [A1_ROWS, TPC_], BF16)
    gat1 = itn("gat1", [nk * A1_ROWS, TPC_], BF16)
    m1 = {"qT": itn("m1_qT", [64, 4, S_len], BF16), "kc2": itn("m1_kc2", [128, S_len], BF16), "vc2": itn("m1_vc2", [128, S_len], BF16),
          "ksT": itn("m1_ksT", [64, S_len], BF16), "kwT": itn("m1_kwT", [64, S_len], BF16), "vs": itn("m1_vs", [S_len, 64], BF16),
          "vw": itn("m1_vw", [S_len, 64], BF16), "gates": itn("m1_gates", [S_len, 12], F32)}
    o1 = itn("o1", [S_len, 256], BF16)
    og1 = itn("og1", [nk * S_len, 256], BF16)
    a3a = itn("all3a", [1024, TPC_], F32)
    a3b = itn("all3b", [3072, TPC_], BF16)
    g3a = itn("gat3a", [nk * 1024, TPC_], F32)
    g3b = itn("gat3b", [nk * 3072, TPC_], BF16)
    m2 = {"qT": itn("m2_qT", [256, S_len], F32), "fT": itn("m2_fT", [256, S_len], F32), "v": itn("m2_v", [S_len, 256], BF16), "gs": itn("m2_gs", [S_len, 256], F32)}
    o2 = itn("o2", [S_len, 256], BF16)
    og2 = itn("og2", [nk * S_len, 256], BF16)
    with ExitStack() as es:
        S = Sched(nc, es)
        D_ALL1, D_GAT1, D_O1, D_OG1 = T(all1, "all1"), T(gat1, "gat1"), T(o1, "o1"), T(og1, "og1")
        D_A3A, D_A3B, D_G3A, D_G3B, D_O2, D_OG2 = T(a3a, "a3a"), T(a3b, "a3b"), T(g3a, "g3a"), T(g3b, "g3b"), T(o2, "o2"), T(og2, "og2")

        def ng(l, a, b):
            return E[f"ng{l}"][:, a * 8:b * 8]

        def ctx(**aps):
            return (S, None, aps)

        def gather(src, dst, Tsrc, Tdst):
            S.begin_phase()
            if _NOCC:
                rows = src.shape[0]
                for k_ in range(nk):
                    for r0 in range(0, rows, 512):
                        r1 = min(rows, r0 + 512)
                        S.dma("sp", dst[k_ * rows + r0:k_ * rows + r1, :], src[r0:r1, :], reads=[Tsrc], writes=[Tdst])
            else:
                S.collective("AllGather", groups, src, dst, reads=[Tsrc], writes=[Tdst])
            S.end_phase()
        build_ffn(TPC_, 256, ctx=ctx(xT=E["xT"], w_in=E["fwi00"], w_out=E["fwo00"], ng=ng(0, 0, 2), yT=xa))
        nsain2_phase(ctx(xT=xa, w_fm=E["nsa_wfm"], ng=ng(0, 2, 3), cs=E["cs"], all1=all1), TPC_, 256)
        gather(all1, gat1, D_ALL1, D_GAT1)
        pa_ = dict(m1)
        pa_.update(gat1=gat1, GAT1=D_GAT1, selq=E["selq"], selk=E["selk"], selg=E["selg"])
        nsaprep_phase(ctx(**pa_), S_len, TPC_)
        aa_ = dict(m1)
        aa_.update({k_: E[k_] for k_ in ("w1s", "w2", "pos2", "ident", "tri_le", "tri_gt", "cb", "ebig", "mmat", "fb")})
        aa_["o"] = o1
        build_nsa_attn(S_len, ctx=ctx(**aa_))
        gather(o1, og1, D_O1, D_OG1)
        out2_phase(ctx(xT=xa, w=E["nsa_wo"], ng=ng(0, 3, 4), yT=xb_, ogat=og1, OGAT=D_OG1, wsel=E["wsel"]), S_len, TPC_, 256)
        build_ffn(TPC_, 256, ctx=ctx(xT=xb_, w_in=E["fwi01"], w_out=E["fwo01"], ng=ng(0, 4, 6), yT=xa))
        build_ple(TPC_, 256, ctx=ctx(xT=xa, pT=E["pT0"], w_p=E["wp0"], w_g=E["wg0"], ng=ng(0, 6, 8), yT=xb_))
        build_ffn(TPC_, 256, ctx=ctx(xT=xb_, w_in=E["fwi10"], w_out=E["fwo10"], ng=ng(1, 0, 2), yT=xa))
        hgrnin2_phase(ctx(xT=xa, w=E["hg_w"], ng=ng(1, 2, 3), all3a=a3a, all3b=a3b), TPC_, 256)
        gather(a3a, g3a, D_A3A, D_G3A)
        gather(a3b, g3b, D_A3B, D_G3B)
        pb_ = dict(m2)
        pb_.update(gat3a=g3a, gat3b=g3b, GAT3A=D_G3A, GAT3B=D_G3B, wI=E["wI"], wsc=E["wsc"])
        hgrnprep_phase(ctx(**pb_), S_len, TPC_)
        hb_ = dict(m2)
        hb_.update({k_: E[k_] for k_ in ("lg", "gn", "smask", "cmask", "ident")})
        hb_["o"] = o2
        build_hgrn(S_len, 1024, ctx=ctx(**hb_))
        gather(o2, og2, D_O2, D_OG2)
        out2_phase(ctx(xT=xa, w=E["hg_wo"], ng=ng(1, 3, 4), yT=xb_, ogat=og2, OGAT=D_OG2, wsel=E["wsel"]), S_len, TPC_, 256)
        build_ffn(TPC_, 256, ctx=ctx(xT=xb_, w_in=E["fwi11"], w_out=E["fwo11"], ng=ng(1, 4, 6), yT=xa))
        build_ple(TPC_, 256, ctx=ctx(xT=xa, pT=E["pT1"], w_p=E["wp1"], w_g=E["wg1"], ng=ng(1, 6, 8), yT=yT))
    return nc


def fused_inputs(S_len, TPC_, x, p, norm_gains, ffn_w_in, ffn_w_out, ple_w_in, ple_w_gate, nsa_w_in, nsa_w_out,
                 nsa_cmp_pos, nsa_cmp_w1, nsa_cmp_w2, hgrn_w_in, hgrn_w_out, hgrn_norm, hgrn_lb_logits):
    f32 = np.float32
    c_ = lambda a: np.ascontiguousarray(np.asarray(a, f32))
    nk = S_len // TPC_
    shared = {}
    for l in range(2):
        shared[f"ng{l}"] = ng_layout(np.asarray(norm_gains, f32)[l], range(8))
        for k in range(2):
            shared[f"fwi{l}{k}"] = c_(ffn_w_in[l, k])
            shared[f"fwo{l}{k}"] = c_(ffn_w_out[l, k])
        shared[f"wp{l}"] = c_(ple_w_in[l])
        shared[f"wg{l}"] = c_(ple_w_gate[l])
    shared["nsa_wfm"] = nsa_w_layout2(np.asarray(nsa_w_in, f32)[0])
    shared["nsa_wo"] = c_(np.asarray(nsa_w_out)[0])
    shared["hg_w"] = c_(np.asarray(hgrn_w_in)[0])
    shared["hg_wo"] = c_(np.asarray(hgrn_w_out)[0])
    w1 = np.asarray(nsa_cmp_w1, f32)[0].reshape(2, 2, 16, 64, 64)
    shared["w1s"] = np.ascontiguousarray(w1.transpose(0, 1, 3, 2, 4).reshape(2, 128, 16, 64))
    shared["w2"] = c_(np.asarray(nsa_cmp_w2)[0])
    pp = np.asarray(nsa_cmp_pos, f32)[0].reshape(2, 2, 16, 64)
    shared["pos2"] = np.ascontiguousarray(pp.transpose(0, 1, 3, 2).reshape(2, 128, 16))
    shared.update(nsa_consts(S_len))
    shared.update(hgrn_consts(1024))
    shared["gn"] = np.ascontiguousarray(np.tile(np.asarray(hgrn_norm, f32)[0][None, :], (64, 1)))
    lgn = np.asarray(hgrn_lb_logits, f32)
    x = np.asarray(x, f32)
    p = np.asarray(p, f32)
    in_maps = []
    for core in range(NCORES):
        b, r = core // nk, core % nk
        t0 = r * TPC_
        d = dict(shared)
        d["xT"] = np.ascontiguousarray(x[b, t0:t0 + TPC_].T)
        for l in range(2):
            d[f"pT{l}"] = np.ascontiguousarray(p[l, b, t0:t0 + TPC_].T)
        d["cs"] = rope_tables(np.arange(t0, t0 + TPC_))
        d.update(nsaprep_sel(r))
        d["wsel"] = out2_sel(r)
        d.update(hgrnprep_sel(r))
        d["lg"] = np.ascontiguousarray(lgn[:, 256 * r:256 * r + 256].reshape(2, 2, 128).transpose(2, 0, 1))
        in_maps.append(d)
    return in_maps


def _decl(nc):
    def ein(name, shape, dt):
        return nc.dram_tensor(name, list(shape), dt, kind="ExternalInput").ap()

    def eout(name, shape, dt):
        return nc.dram_tensor(name, list(shape), dt, kind="ExternalOutput").ap()

    def itn(name, shape, dt):
        return nc.dram_tensor(name, list(shape), dt).ap()
    return ein, eout, itn


def build_launch_a(TPC_):
    nc = bass.Bass("TRN2", target_bir_lowering=False)
    ein, eout, itn = _decl(nc)
    xT = ein("xT", [D, TPC_], F32)
    fwi, fwo = ein("fwi", [D, 2 * DFF], F32), ein("fwo", [DFF, D], F32)
    ngf, ngn = ein("ngf", [128, 16], F32), ein("ngn", [128, 8], F32)
    w_fm, w_tm, cs = ein("w_fm", [D, 30 * 128], F32), ein("w_tm", [D, 560], F32), ein("cs", [128, 2, TPC_], F32)
    x1 = eout("x1", [D, TPC_], F32)
    outs = {"qkT": eout("qkT", [NSA_NROPE * 128, TPC_], BF16), "vcT": eout("vcT", [256, TPC_], BF16),
            "vsw": eout("vsw", [TPC_, 512], BF16), "gates": eout("gates", [TPC_, 48], F32)}
    with ExitStack() as es:
        S = Sched(nc, es)
        build_ffn(TPC_, 256, ctx=(S, None, dict(xT=xT, w_in=fwi, w_out=fwo, ng=ngf, yT=x1)))
        a = dict(xT=x1, w_fm=w_fm, w_tm=w_tm, ng=ngn, cs=cs)
        a.update(outs)
        build_nsain(TPC_, 256, ctx=(S, None, a))
    return nc


def build_launch_c(TPC_):
    nc = bass.Bass("TRN2", target_bir_lowering=False)
    ein, eout, itn = _decl(nc)
    xT, oT = ein("xT", [D, TPC_], F32), ein("oT", [D, TPC_], BF16)
    wo, ngo = ein("wo", [D, D], F32), ein("ngo", [128, 8], F32)
    fwi1, fwo1, ngf1 = ein("fwi1", [D, 2 * DFF], F32), ein("fwo1", [DFF, D], F32), ein("ngf1", [128, 16], F32)
    pT, wp, wg, ngp = ein("pT", [256, TPC_], F32), ein("wp", [256, D], F32), ein("wg", [D, D], F32), ein("ngp", [128, 16], F32)
    fwi2, fwo2, ngf2 = ein("fwi2", [D, 2 * DFF], F32), ein("fwo2", [DFF, D], F32), ein("ngf2", [128, 16], F32)
    hw, ngh = ein("hw", [D, 4096], F32), ein("ngh", [128, 8], F32)
    xa, xb_ = itn("xa", [D, TPC_], F32), itn("xb", [D, TPC_], F32)
    x5 = eout("x5", [D, TPC_], F32)
    qfT, iv, gs = eout("qfT", [2048, TPC_], F32), eout("iv", [TPC_, D], BF16), eout("gs", [TPC_, D], F32)
    with ExitStack() as es:
        S = Sched(nc, es)
        build_out(TPC_, 256, ctx=(S, None, dict(xT=xT, oT=oT, w=wo, ng=ngo, yT=xa)))
        build_ffn(TPC_, 256, ctx=(S, None, dict(xT=xa, w_in=fwi1, w_out=fwo1, ng=ngf1, yT=xb_)))
        build_ple(TPC_, 256, ctx=(S, None, dict(xT=xb_, pT=pT, w_p=wp, w_g=wg, ng=ngp, yT=xa)))
        build_ffn(TPC_, 256, ctx=(S, None, dict(xT=xa, w_in=fwi2, w_out=fwo2, ng=ngf2, yT=x5)))
        build_hgrnin(TPC_, 256, ctx=(S, None, dict(xT=x5, w=hw, ng=ngh, qfT=qfT, iv=iv, gs=gs)))
    return nc


def build_launch_e(TPC_):
    nc = bass.Bass("TRN2", target_bir_lowering=False)
    ein, eout, itn = _decl(nc)
    xT, oT = ein("xT", [D, TPC_], F32), ein("oT", [D, TPC_], BF16)
    wo, ngo = ein("wo", [D, D], F32), ein("ngo", [128, 8], F32)
    fwi1, fwo1, ngf1 = ein("fwi1", [D, 2 * DFF], F32), ein("fwo1", [DFF, D], F32), ein("ngf1", [128, 16], F32)
    pT, wp, wg, ngp = ein("pT", [256, TPC_], F32), ein("wp", [256, D], F32), ein("wg", [D, D], F32), ein("ngp", [128, 16], F32)
    xa, xb_ = itn("xa", [D, TPC_], F32), itn("xb", [D, TPC_], F32)
    yT = eout("yT", [D, TPC_], F32)
    with ExitStack() as es:
        S = Sched(nc, es)
        build_out(TPC_, 256, ctx=(S, None, dict(xT=xT, oT=oT, w=wo, ng=ngo, yT=xa)))
        build_ffn(TPC_, 256, ctx=(S, None, dict(xT=xa, w_in=fwi1, w_out=fwo1, ng=ngf1, yT=xb_)))
        build_ple(TPC_, 256, ctx=(S, None, dict(xT=xb_, pT=pT, w_p=wp, w_g=wg, ng=ngp, yT=yT)))
    return nc


def kernel5(x, p, norm_gains, ffn_w_in, ffn_w_out, ple_w_in, ple_w_gate, nsa_w_in, nsa_w_out,
            nsa_cmp_pos, nsa_cmp_w1, nsa_cmp_w2, hgrn_w_in, hgrn_w_out, hgrn_norm, hgrn_lb_logits, S_len=S_, TPC_=TPC):
    f32 = np.float32
    c_ = lambda a: np.ascontiguousarray(np.asarray(a, f32))
    x = np.asarray(x, f32)
    p = np.asarray(p, f32)
    norm_gains = np.asarray(norm_gains, f32)
    nk = S_len // TPC_
    cores = [(r // nk, (r % nk) * TPC_) for r in range(NCORES)]

    def to_rows(o_cores):
        res = []
        for (b, t0) in cores:
            blk = np.concatenate([o_cores[b * 4 + g][t0:t0 + TPC_] for g in range(4)], axis=1)
            res.append(np.ascontiguousarray(blk.T))
        return res
    nc = _prog(("A", TPC_), lambda: build_launch_a(TPC_))
    w_fm, w_tm = nsa_w_layout(np.asarray(nsa_w_in, f32)[0])
    sh = dict(fwi=c_(ffn_w_in[0, 0]), fwo=c_(ffn_w_out[0, 0]), ngf=ng_layout(norm_gains[0], [0, 1]), ngn=ng_layout(norm_gains[0], [2]), w_fm=w_fm, w_tm=w_tm)
    r = _run(nc, [dict(sh, xT=np.ascontiguousarray(x[b, t0:t0 + TPC_].T), cs=rope_tables(np.arange(t0, t0 + TPC_))) for (b, t0) in cores])
    x1 = [q["x1"] for q in r]
    consts = nsa_consts(S_len)
    in_maps = []
    for b in range(B_):
        qk = np.concatenate([r[b * nk + k]["qkT"] for k in range(nk)], axis=1)
        vc = np.concatenate([r[b * nk + k]["vcT"] for k in range(nk)], axis=1)
        vsw = np.concatenate([r[b * nk + k]["vsw"] for k in range(nk)], axis=0)
        gt = np.concatenate([r[b * nk + k]["gates"] for k in range(nk)], axis=0)
        for g in range(4):
            in_maps.append(nsa_attn_inputs(qk, vc, vsw, gt, np.asarray(nsa_cmp_pos, f32)[0], np.asarray(nsa_cmp_w1, f32)[0],
                                           np.asarray(nsa_cmp_w2, f32)[0], g, consts))
    nc = _prog(("attn", S_len), lambda: build_nsa_attn(S_len))
    r = _run(nc, in_maps)
    oT = to_rows([q["o"] for q in r])
    nc = _prog(("C", TPC_), lambda: build_launch_c(TPC_))
    sh = dict(wo=c_(np.asarray(nsa_w_out)[0]), ngo=ng_layout(norm_gains[0], [3]),
              fwi1=c_(ffn_w_in[0, 1]), fwo1=c_(ffn_w_out[0, 1]), ngf1=ng_layout(norm_gains[0], [4, 5]),
              wp=c_(ple_w_in[0]), wg=c_(ple_w_gate[0]), ngp=ng_layout(norm_gains[0], [6, 7]),
              fwi2=c_(ffn_w_in[1, 0]), fwo2=c_(ffn_w_out[1, 0]), ngf2=ng_layout(norm_gains[1], [0, 1]),
              hw=c_(np.asarray(hgrn_w_in)[0]), ngh=ng_layout(norm_gains[1], [2]))
    r = _run(nc, [dict(sh, xT=x1[i], oT=oT[i], pT=np.ascontiguousarray(p[0, b, t0:t0 + TPC_].T)) for i, (b, t0) in enumerate(cores)])
    x5 = [q["x5"] for q in r]
    hc = hgrn_consts(1024)
    lgn = np.asarray(hgrn_lb_logits, f32)
    gn = np.ascontiguousarray(np.tile(np.asarray(hgrn_norm, f32)[0][None, :], (64, 1)))
    in_maps = []
    for b in range(B_):
        qf = np.concatenate([r[b * nk + k]["qfT"] for k in range(nk)], axis=1)
        iv = np.concatenate([r[b * nk + k]["iv"] for k in range(nk)], axis=0)
        gs = np.concatenate([r[b * nk + k]["gs"] for k in range(nk)], axis=0)
        for r2 in range(4):
            hs = slice(256 * r2, 256 * r2 + 256)
            d = {"qT": np.ascontiguousarray(qf[hs]), "fT": np.ascontiguousarray(qf[1024 + 256 * r2:1024 + 256 * r2 + 256]),
                 "v": np.ascontiguousarray(iv[:, hs]), "gs": np.ascontiguousarray(gs[:, hs]),
                 "lg": np.ascontiguousarray(lgn[:, hs].reshape(2, 2, 128).transpose(2, 0, 1)), "gn": gn}
            d.update(hc)
            in_maps.append(d)
    nc = _prog(("hgrn", S_len), lambda: build_hgrn(S_len, 1024))
    r = _run(nc, in_maps)
    oT = to_rows([q["o"] for q in r])
    nc = _prog(("E", TPC_), lambda: build_launch_e(TPC_))
    sh = dict(wo=c_(np.asarray(hgrn_w_out)[0]), ngo=ng_layout(norm_gains[1], [3]),
              fwi1=c_(ffn_w_in[1, 1]), fwo1=c_(ffn_w_out[1, 1]), ngf1=ng_layout(norm_gains[1], [4, 5]),
              wp=c_(ple_w_in[1]), wg=c_(ple_w_gate[1]), ngp=ng_layout(norm_gains[1], [6, 7]))
    r = _run(nc, [dict(sh, xT=x5[i], oT=oT[i], pT=np.ascontiguousarray(p[1, b, t0:t0 + TPC_].T)) for i, (b, t0) in enumerate(cores)])
    out = np.empty((B_, S_len, D), f32)
    for i, (b, t0) in enumerate(cores):
        out[b, t0:t0 + TPC_] = r[i]["yT"].T
    return out
```

```python
import numpy as np
import ml_dtypes
import concourse.bass as bass
import concourse.mybir as mybir
from concourse.bass_utils import run_bass_kernel_spmd
from contextlib import ExitStack

F32 = mybir.dt.float32
BF16 = mybir.dt.bfloat16
AF = mybir.ActivationFunctionType
ALU = mybir.AluOpType
AX = mybir.AxisListType
NPBF = ml_dtypes.bfloat16

D = 1024
DFF = 2816
NCH = 8
NFC = DFF // 128
EPS = 1e-6
SEM_MAX = 30000
CC_INC = 1
NCORES = 8


class T:
    def __init__(self, h, name):
        self.h = h
        self.name = name
        self.w = None
        self.rs = {}
        self.dsem = None
        self.dcnt = 0

    def __getitem__(self, k):
        return self.h[k]


class Sched:
    ENGS = ("pe", "act", "dve", "pool", "sp")

    def __init__(self, nc, es):
        self.nc = nc
        self.es = es
        self.streams = {e: [] for e in self.ENGS}
        self.sem = {}
        self.cnt = {}
        self.nsem = 0
        for e in self.ENGS:
            self._newsem(e)
        self.waited = {}
        self.ntile = 0
        self.dsem = None
        self.dcnt = 0
        self.pes = es
        self.pool = {}
        self.phase_tiles = []
        self.phase_dtoks = {}

    def _mksem(self, name):
        self.nsem += 1
        return self.es.enter_context(self.nc.semaphore(name))

    def _newsem(self, e):
        self.sem[e] = self._mksem(f"s_{e}_{self.nsem}")
        self.cnt[e] = 0

    def sb(self, shape, dt, name=None):
        self.ntile += 1
        name = f"sb{self.ntile}_" + (name or "t")
        h = self.pes.enter_context(self.nc.sbuf_tensor(name, list(shape), dt))
        t = T(h, name)
        t.scoped = True
        self.phase_tiles.append(t)
        return t

    def ps(self, shape, dt=F32, name=None):
        self.ntile += 1
        name = f"ps{self.ntile}_" + (name or "p")
        h = self.pes.enter_context(self.nc.psum_tensor(name, list(shape), dt))
        t = T(h, name)
        t.scoped = True
        self.phase_tiles.append(t)
        return t

    def dram(self, name, shape, dt):
        h = self.nc.dram_tensor(name, list(shape), dt)
        return T(h.ap(), name)

    def _deps(self, eng, reads, writes, same_eng_sync):
        deps = []
        for t in reads:
            if t.w is not None:
                deps.append(t.w)
        for t in writes:
            if t.w is not None:
                deps.append(t.w)
            deps.extend(t.rs.values())
        best = {}
        for (sem, val, src) in deps:
            if src == eng and not same_eng_sync:
                continue
            if id(sem) not in best or best[id(sem)][1] < val:
                best[id(sem)] = (sem, val)
        waits = []
        for (sem, val) in best.values():
            key = (eng, id(sem))
            if self.waited.get(key, 0) >= val:
                continue
            self.waited[key] = val
            waits.append((sem, val))
        return waits

    def op(self, eng, fn, reads=(), writes=(), sync_same=None):
        if sync_same is None:
            sync_same = eng != "pe"
        waits = self._deps(eng, reads, writes, sync_same)
        if self.cnt[eng] >= SEM_MAX:
            self._newsem(eng)
        self.cnt[eng] += 1
        tok = (self.sem[eng], self.cnt[eng], eng)
        self.streams[eng].append((waits, fn, (self.sem[eng], 1)))
        for t in reads:
            t.rs[id(tok[0])] = tok
        for t in writes:
            t.w = tok
            t.rs = {}
        return tok

    def dma(self, q, out, in_, reads=(), writes=(), semt=None, **kw):
        waits = self._deps(q, reads, writes, True)
        t = semt if semt is not None else (list(writes) + list(reads))[0]
        kind = "sw" if q == "pool" else "hw"
        if t.dsem is None:
            t.dsem = {}
        ds = t.dsem.get(kind)
        if ds is None or ds[1] >= SEM_MAX:
            pl = self.pool.setdefault(kind, [])
            if getattr(t, "scoped", False) and pl and pl[-1][1] < SEM_MAX:
                ds = pl.pop()
            else:
                ds = [self._mksem(f"d{kind}_{self.nsem}"), 0]
            t.dsem[kind] = ds
        ds[1] += 16
        tok = (ds[0], ds[1], "dma")
        sem = ds[0]
        self.phase_dtoks[id(sem)] = tok
        self.streams[q].append((waits, lambda e: e.dma_start(out=out, in_=in_, **kw), (sem, 16)))
        for r in reads:
            r.rs[id(tok[0])] = tok
        for w in writes:
            w.w = tok
            w.rs = {}
        return tok

    def collective(self, kind, groups, in_ap, out_ap, reads=(), writes=()):
        waits = self._deps("pool", reads, writes, True)
        t = list(writes)[0]
        if t.dsem is None:
            t.dsem = {}
        ds = t.dsem.get("cc")
        if ds is None:
            ds = [self._mksem(f"c_{self.nsem}"), 0]
            t.dsem["cc"] = ds
        ds[1] += CC_INC
        tok = (ds[0], ds[1], "dma")
        sem = ds[0]
        self.phase_dtoks[id(sem)] = tok
        self.streams["pool"].append((waits, lambda e: e.collective_compute(kind, ALU.bypass, replica_groups=groups, ins=[in_ap.opt()], outs=[out_ap.opt()]), (sem, CC_INC)))
        for r in reads:
            r.rs[id(tok[0])] = tok
        for w in writes:
            w.w = tok
            w.rs = {}
        return tok

    def begin_phase(self):
        self.pes = ExitStack()
        self.phase_tiles = []
        self.phase_dtoks = {}

    def end_phase(self):
        toks = [(self.sem[e], self.cnt[e], e) for e in self.ENGS if self.cnt[e] > 0]
        toks += list(self.phase_dtoks.values())
        for e in self.ENGS:
            ws = []
            for (sm, v, src) in toks:
                key = (e, id(sm))
                if src == e or self.waited.get(key, 0) >= v:
                    continue
                self.waited[key] = v
                ws.append((sm, v))
            self.streams[e].append((ws, None, None))
        self.emit()
        for t in self.phase_tiles:
            if t.dsem:
                for kind, ds in t.dsem.items():
                    self.pool.setdefault(kind, []).append(ds)
                t.dsem = None
        self.pes.close()
        self.pes = self.es
        self.phase_tiles = []
        self.phase_dtoks = {}

    def final_wait(self, eng, toks):
        best = {}
        for (s, v, _) in toks:
            if id(s) not in best or best[id(s)][1] < v:
                best[id(s)] = (s, v)
        self.streams[eng].append((list(best.values()), None, None))

    def emit(self):
        nc = self.nc
        streams = self.streams
        with nc.Block() as block:
            def run(e, name):
                for (waits, fn, inc) in streams[name]:
                    for (sem, val) in waits:
                        e.wait_ge(sem, val)
                    if fn is not None:
                        ins = fn(e)
                        if inc is not None:
                            ins.then_inc(inc[0], inc[1])

            @block.tensor
            def _(e):
                run(e, "pe")

            @block.scalar
            def _(e):
                run(e, "act")

            @block.vector
            def _(e):
                run(e, "dve")

            @block.gpsimd
            def _(e):
                run(e, "pool")

            @block.sync
            def _(e):
                run(e, "sp")
        self.streams = {e: [] for e in self.ENGS}


def _io(nc, ctx, name, shape, dt, kind):
    if ctx is not None and name in ctx[2]:
        return ctx[2][name]
    return nc.dram_tensor(name, list(shape), dt, kind=kind).ap()


class _SchedCtx:
    def __init__(self, nc, ctx):
        self.nc, self.ctx = nc, ctx

    def __enter__(self):
        if self.ctx is None:
            self.es = ExitStack()
            self.es.__enter__()
            S = Sched(self.nc, self.es)
            return S, Consts(S), True
        S = self.ctx[0]
        S.begin_phase()
        return S, Consts(S), False

    def __exit__(self, *a):
        if self.ctx is None:
            return self.es.__exit__(*a)
        if a[0] is None:
            self.ctx[0].end_phase()
        return False


class Consts:
    def __init__(self, S):
        self.ones = S.sb([128, 128], BF16, "c_ones")
        S.op("pool", lambda e: e.memset(self.ones[:], 1.0), writes=[self.ones])


def rms_stats(S, C, sq_tiles_fn, nchunks, pstat, rstd, nt, dim):
    for c in range(nchunks):
        t, ap = sq_tiles_fn(c)
        S.op("pe", lambda e, ap=ap, c=c: e.matmul(pstat[:, 0:nt], lhsT=C.ones[:], rhs=ap, start=(c == 0), stop=(c == nchunks - 1)),
             reads=[C.ones, t], writes=[pstat])
    S.op("act", lambda e: e.activation(out=rstd[:, 0:nt], in_=pstat[:, 0:nt], func=AF.Sqrt, scale=1.0 / dim, bias=EPS),
         reads=[pstat], writes=[rstd])
    S.op("dve", lambda e: e.reciprocal(out=rstd[:, 0:nt], in_=rstd[:, 0:nt]), reads=[rstd], writes=[rstd])


def build_ffn(T_tok, NT=256, ctx=None):
    nc = bass.Bass("TRN2", target_bir_lowering=False) if ctx is None else ctx[0].nc
    xT = _io(nc, ctx, "xT", [D, T_tok], F32, "ExternalInput")
    w_in = _io(nc, ctx, "w_in", [D, 2 * DFF], F32, "ExternalInput")
    w_out = _io(nc, ctx, "w_out", [DFF, D], F32, "ExternalInput")
    ng = _io(nc, ctx, "ng", [128, 16], F32, "ExternalInput")
    yT = _io(nc, ctx, "yT", [D, T_tok], F32, "ExternalOutput")
    with _SchedCtx(nc, ctx) as (S, C, own):
        toks = ffn_phase(S, C, xT, yT, w_in, w_out, ng, T_tok, NT)
        S.final_wait("sp", toks)
        if own:
            S.emit()
    return nc


def ffn_phase(S, C, xT, yT, w_in, w_out, ng, T_tok, NT):
    nt = NT
    ntiles = T_tok // nt
    win = S.sb([128, NCH, 2 * DFF], BF16, "win")
    wout = S.sb([128, NFC, D], BF16, "wout")
    g = S.sb([128, 16], F32, "gains")
    S.dma("sp", g[:], ng, writes=[g])
    w_in_v = w_in.rearrange("(c p) f -> p c f", p=128)
    w_out_v = w_out.rearrange("(j p) f -> p j f", p=128)
    for c in range(NCH):
        S.dma("pool", win[:, c, :], w_in_v[:, c, :], writes=[win])
    for j in range(0, NFC, 2):
        S.dma("pool", wout[:, j:j + 2, :], w_out_v[:, j:j + 2, :], writes=[wout])
    xs = [S.sb([128, NCH, nt], F32, f"x{i}") for i in range(2)]
    xn = S.sb([128, NCH, nt], BF16, "xn")
    sq = [S.sb([128, nt], BF16, f"sq{i}") for i in range(2)]
    act = S.sb([128, NFC, nt], BF16, "act")
    y = S.sb([128, NCH, nt], F32, "y")
    sg = [S.sb([128, nt], F32, f"sg{i}") for i in range(2)]
    rstd = S.sb([128, nt], F32, "rstd")
    rstd2 = S.sb([128, nt], F32, "rstd2")
    tmp = [S.sb([128, nt], F32, f"tmp{i}") for i in range(2)]
    pstat = S.ps([128, 512], F32, "pstat")
    pg = [S.ps([128, 512], F32, f"pg{i}") for i in range(2)]
    pu = [S.ps([128, 512], F32, f"pu{i}") for i in range(2)]
    py = [S.ps([128, 512], F32, f"py{i}") for i in range(2)]
    xT_v = xT.rearrange("(c p) t -> p c t", p=128)
    yT_v = yT.rearrange("(c p) t -> p c t", p=128)
    out_toks = []

    def load(i):
        x = xs[i % 2]
        S.dma("sp", x[:], xT_v[:, :, i * nt:(i + 1) * nt], writes=[x])

    load(0)
    for i in range(ntiles):
        x = xs[i % 2]
        if i + 1 < ntiles:
            load(i + 1)
        def sqf(c, x=x):
            s = sq[c % 2]
            S.op("pool", lambda e, s=s, c=c: e.tensor_tensor(out=s[:], in0=x[:, c, :], in1=x[:, c, :], op=ALU.mult), reads=[x], writes=[s])
            return s, s[:]
        rms_stats(S, C, sqf, NCH, pstat, rstd, nt, D)
        for c in range(NCH):
            S.op("dve", lambda e, c=c, x=x: e.scalar_tensor_tensor(out=xn[:, c, :], in0=x[:, c, :], scalar=g[:, c:c + 1], in1=rstd[:], op0=ALU.mult, op1=ALU.mult),
                 reads=[x, g, rstd], writes=[xn])
        for j in range(NFC):
            a, b = pg[j % 2], pu[j % 2]
            for c in range(NCH):
                S.op("pe", lambda e, a=a, c=c, j=j: e.matmul(a[:, 0:nt], lhsT=win[:, c, j * 128:(j + 1) * 128], rhs=xn[:, c, :], start=(c == 0), stop=(c == NCH - 1)),
                     reads=[win, xn], writes=[a])
            for c in range(NCH):
                S.op("pe", lambda e, b=b, c=c, j=j: e.matmul(b[:, 0:nt], lhsT=win[:, c, DFF + j * 128:DFF + (j + 1) * 128], rhs=xn[:, c, :], start=(c == 0), stop=(c == NCH - 1)),
                     reads=[win, xn], writes=[b])
            s = sg[j % 2]
            S.op("act", lambda e, a=a, s=s: e.activation(out=s[:], in_=a[:, 0:nt], func=AF.Silu), reads=[a], writes=[s])
            S.op("dve", lambda e, b=b, s=s, j=j: e.tensor_tensor(out=act[:, j, :], in0=b[:, 0:nt], in1=s[:], op=ALU.mult), reads=[b, s], writes=[act])
        ysq = []
        for m in range(NCH):
            p = py[m % 2]
            for j in range(NFC):
                S.op("pe", lambda e, p=p, j=j, m=m: e.matmul(p[:, 0:nt], lhsT=wout[:, j, m * 128:(m + 1) * 128], rhs=act[:, j, :], start=(j == 0), stop=(j == NFC - 1)),
                     reads=[wout, act], writes=[p])
            S.op("act", lambda e, p=p, m=m: e.activation(out=y[:, m, :], in_=p[:, 0:nt], func=AF.Copy), reads=[p], writes=[y])
        def sqy(c):
            s = sq[c % 2]
            S.op("pool", lambda e, s=s, c=c: e.tensor_tensor(out=s[:], in0=y[:, c, :], in1=y[:, c, :], op=ALU.mult), reads=[y], writes=[s])
            return s, s[:]
        rms_stats(S, C, sqy, NCH, pstat, rstd2, nt, D)
        for m in range(NCH):
            t = tmp[m % 2]
            S.op("dve", lambda e, m=m, t=t: e.scalar_tensor_tensor(out=t[:], in0=y[:, m, :], scalar=g[:, 8 + m:9 + m], in1=rstd2[:], op0=ALU.mult, op1=ALU.mult),
                 reads=[y, g, rstd2], writes=[t])
            S.op("dve", lambda e, m=m, t=t, x=x: e.scalar_tensor_tensor(out=x[:, m, :], in0=t[:], scalar=0.5, in1=x[:, m, :], op0=ALU.mult, op1=ALU.add),
                 reads=[t, x], writes=[x])
        out_toks.append(S.dma("sp", yT_v[:, :, i * nt:(i + 1) * nt], x[:], reads=[x]))
    return out_toks


def ng_layout(norm_gains_l, idxs):
    a = np.asarray(norm_gains_l, dtype=np.float32)[list(idxs)]
    a = a.reshape(len(idxs), NCH, 128).transpose(2, 0, 1).reshape(128, len(idxs) * NCH)
    return np.ascontiguousarray(a)


NSA_NROPE = 14


def build_nsain(T_tok, NT=256, ctx=None):
    nc = bass.Bass("TRN2", target_bir_lowering=False) if ctx is None else ctx[0].nc
    xT = _io(nc, ctx, "xT", [D, T_tok], F32, "ExternalInput")
    w_fm = _io(nc, ctx, "w_fm", [D, 30 * 128], F32, "ExternalInput")
    w_tm = _io(nc, ctx, "w_tm", [D, 560], F32, "ExternalInput")
    ng = _io(nc, ctx, "ng", [128, 8], F32, "ExternalInput")
    cs = _io(nc, ctx, "cs", [128, 2, T_tok], F32, "ExternalInput")
    qkT = _io(nc, ctx, "qkT", [NSA_NROPE * 128, T_tok], BF16, "ExternalOutput")
    vcT = _io(nc, ctx, "vcT", [256, T_tok], BF16, "ExternalOutput")
    vsw = _io(nc, ctx, "vsw", [T_tok, 512], BF16, "ExternalOutput")
    gates = _io(nc, ctx, "gates", [T_tok, 48], F32, "ExternalOutput")
    nt = NT
    ntiles = T_tok // nt
    nsub = nt // 128
    with _SchedCtx(nc, ctx) as (S, C, own):
        wf = S.sb([128, NCH, 30 * 128], BF16, "wf")
        wt = S.sb([128, NCH, 560], BF16, "wt")
        g = S.sb([128, 8], F32, "gains")
        S.dma("sp", g[:], ng, writes=[g])
        w_fm_v = w_fm.rearrange("(c p) f -> p c f", p=128)
        w_tm_v = w_tm.rearrange("(c p) f -> p c f", p=128)
        for c in range(NCH):
            S.dma("pool", wf[:, c, :], w_fm_v[:, c, :], writes=[wf])
        for c in range(NCH):
            S.dma("pool", wt[:, c, :], w_tm_v[:, c, :], writes=[wt])
        xs = [S.sb([128, NCH, nt], F32, f"x{i}") for i in range(2)]
        cst = [S.sb([128, 2, nt], F32, f"cs{i}") for i in range(2)]
        hn = S.sb([128, NCH, nt], BF16, "hn")
        sq = [S.sb([128, nt], BF16, f"sq{i}") for i in range(2)]
        rstd = S.sb([128, nt], F32, "rstd")
        t1 = [S.sb([128, nt], F32, f"t1_{i}") for i in range(2)]
        t2 = [S.sb([128, nt], F32, f"t2_{i}") for i in range(2)]
        oqk = [S.sb([128, NSA_NROPE, nt], BF16, f"oqk{i}") for i in range(2)]
        ovc = [S.sb([128, 2, nt], BF16, f"ovc{i}") for i in range(2)]
        ovs = [S.sb([128, nsub, 512], BF16, f"ovs{i}") for i in range(2)]
        ogt = [S.sb([128, nsub, 48], F32, f"ogt{i}") for i in range(2)]
        pstat = S.ps([128, 512], F32, "pstat")
        pa = [S.ps([128, 512], F32, f"pa{i}") for i in range(2)]
        pb = [S.ps([128, 512], F32, f"pb{i}") for i in range(2)]
        pt = [S.ps([128, 512], F32, f"pt{i}") for i in range(2)]
        pgt = T(pstat.h[:, 256:512], "pgt_alias")
        pgt = pstat
        xT_v = xT.rearrange("(c p) t -> p c t", p=128)
        qk_v = qkT.rearrange("(c p) t -> p c t", p=128)
        vc_v = vcT.rearrange("(c p) t -> p c t", p=128)
        vsw_v = vsw.rearrange("(s p) f -> p s f", p=128)
        gt_v = gates.rearrange("(s p) f -> p s f", p=128)
        toks = []

        def load(i):
            S.dma("sp", xs[i % 2][:], xT_v[:, :, i * nt:(i + 1) * nt], writes=[xs[i % 2]])
            S.dma("sp", cst[i % 2][:], cs[:, :, i * nt:(i + 1) * nt], writes=[cst[i % 2]])

        load(0)
        for i in range(ntiles):
            x = xs[i % 2]
            cs_t = cst[i % 2]
            if i + 1 < ntiles:
                load(i + 1)

            def sqf(c, x=x):
                s = sq[c % 2]
                S.op("pool", lambda e, s=s, c=c: e.tensor_tensor(out=s[:], in0=x[:, c, :], in1=x[:, c, :], op=ALU.mult), reads=[x], writes=[s])
                return s, s[:]
            rms_stats(S, C, sqf, NCH, pstat, rstd, nt, D)
            for c in range(NCH):
                S.op("dve", lambda e, c=c, x=x: e.scalar_tensor_tensor(out=hn[:, c, :], in0=x[:, c, :], scalar=g[:, c:c + 1], in1=rstd[:], op0=ALU.mult, op1=ALU.mult),
                     reads=[x, g, rstd], writes=[hn])
            oq = oqk[i % 2]
            ov = ovc[i % 2]
            for j in range(NSA_NROPE):
                a, b = pa[j % 2], pb[j % 2]
                for c in range(NCH):
                    S.op("pe", lambda e, a=a, c=c, j=j: e.matmul(a[:, 0:nt], lhsT=wf[:, c, j * 128:(j + 1) * 128], rhs=hn[:, c, :], start=(c == 0), stop=(c == NCH - 1)),
                         reads=[wf, hn], writes=[a])
                for c in range(NCH):
                    S.op("pe", lambda e, b=b, c=c, j=j: e.matmul(b[:, 0:nt], lhsT=wf[:, c, (14 + j) * 128:(15 + j) * 128], rhs=hn[:, c, :], start=(c == 0), stop=(c == NCH - 1)),
                         reads=[wf, hn], writes=[b])
                u1, u2 = t1[j % 2], t2[j % 2]
                S.op("dve", lambda e, a=a, u1=u1, cs_t=cs_t: e.tensor_tensor(out=u1[:], in0=a[:, 0:nt], in1=cs_t[:, 0, :], op=ALU.mult), reads=[a, cs_t], writes=[u1])
                S.op("dve", lambda e, b=b, u2=u2, cs_t=cs_t: e.tensor_tensor(out=u2[:], in0=b[:, 0:nt], in1=cs_t[:, 1, :], op=ALU.mult), reads=[b, cs_t], writes=[u2])
                S.op("pool", lambda e, u1=u1, u2=u2, j=j, oq=oq: e.tensor_tensor(out=oq[:, j, :], in0=u1[:], in1=u2[:], op=ALU.add), reads=[u1, u2], writes=[oq])
            for j in range(2):
                a = pa[j % 2]
                for c in range(NCH):
                    S.op("pe", lambda e, a=a, c=c, j=j: e.matmul(a[:, 0:nt], lhsT=wf[:, c, (28 + j) * 128:(29 + j) * 128], rhs=hn[:, c, :], start=(c == 0), stop=(c == NCH - 1)),
                         reads=[wf, hn], writes=[a])
                S.op("act", lambda e, a=a, j=j, ov=ov: e.activation(out=ov[:, j, :], in_=a[:, 0:nt], func=AF.Copy), reads=[a], writes=[ov])
            osw = ovs[i % 2]
            og = ogt[i % 2]
            for s in range(nsub):
                p = pt[s % 2]
                for c in range(NCH):
                    S.op("pe", lambda e, p=p, c=c, s=s: e.matmul(p[:, 0:512], lhsT=hn[:, c, s * 128:(s + 1) * 128], rhs=wt[:, c, 0:512], start=(c == 0), stop=(c == NCH - 1)),
                         reads=[wt, hn], writes=[p])
                S.op("act", lambda e, p=p, s=s, osw=osw: e.activation(out=osw[:, s, :], in_=p[:, 0:512], func=AF.Copy), reads=[p], writes=[osw])
                for c in range(NCH):
                    S.op("pe", lambda e, c=c, s=s: e.matmul(pgt[:, 256:304], lhsT=hn[:, c, s * 128:(s + 1) * 128], rhs=wt[:, c, 512:560], start=(c == 0), stop=(c == NCH - 1)),
                         reads=[wt, hn], writes=[pgt])
                S.op("act", lambda e, s=s, og=og: e.activation(out=og[:, s, :], in_=pgt[:, 256:304], func=AF.Sigmoid), reads=[pgt], writes=[og])
            sl = slice(i * nt, (i + 1) * nt)
            toks.append(S.dma("sp", qk_v[:, 0:7, sl], oq[:, 0:7, :], reads=[oq]))
            toks.append(S.dma("sp", qk_v[:, 7:14, sl], oq[:, 7:14, :], reads=[oq]))
            toks.append(S.dma("sp", vc_v[:, :, sl], ov[:], reads=[ov]))
            toks.append(S.dma("sp", vsw_v[:, i * nsub:(i + 1) * nsub, :], osw[:], reads=[osw]))
            toks.append(S.dma("sp", gt_v[:, i * nsub:(i + 1) * nsub, :], og[:], reads=[og]))
        S.final_wait("sp", toks)
        if own:
            S.emit()
    return nc


def rope_tables(pos):
    half = 32
    inv = (10000.0 ** (-np.arange(half, dtype=np.float32) / half)).astype(np.float32)
    ang = pos.astype(np.float32)[None, :] * inv[:, None]
    cos = np.cos(ang).astype(np.float32)
    sin = np.sin(ang).astype(np.float32)
    r = np.arange(128)
    f = r % 32
    sign = np.where((r % 64) < 32, -1.0, 1.0).astype(np.float32)
    out = np.empty((128, 2, len(pos)), np.float32)
    out[:, 0, :] = cos[f]
    out[:, 1, :] = sin[f] * sign[:, None]
    return out


def nsa_w_layout(w):
    w = np.asarray(w, np.float32)
    rope_cols = np.concatenate([np.arange(0, 1024), np.arange(1024, 1280), np.arange(1536, 1792), np.arange(2048, 2304)])
    sw = rope_cols.reshape(-1, 2, 32)[:, ::-1, :].reshape(-1)
    w_fm = np.concatenate([w[:, rope_cols], w[:, sw], w[:, 1280:1536]], axis=1)
    w_tm = np.concatenate([w[:, 1792:2048], w[:, 2304:2560], w[:, 2560:2608]], axis=1)
    return np.ascontiguousarray(w_fm), np.ascontiguousarray(w_tm)


BIG = 30000.0


def nsa_consts(S_len):
    nsel = S_len // 64
    ncmp = S_len // 16 - 1
    nct = (ncmp + 127) // 128
    bk = min(128, nsel)
    c = {}
    c["ident"] = np.eye(128, dtype=np.float32).astype(NPBF)
    kl = np.arange(128)[:, None]
    ql = np.arange(128)[None, :]
    c["tri_le"] = np.where(kl <= ql, 0.0, -BIG).astype(NPBF)
    c["tri_gt"] = np.where(kl > ql, 0.0, -BIG).astype(NPBF)
    m = np.arange(17)[None, :, None]
    cb = np.where(16 * kl[:, :, None] + 31 <= 128 * m + ql[None, :, :], 0.0, -BIG)
    c["cb"] = cb.astype(NPBF)
    cc = np.arange(nct * 128)[:, None]
    nn = np.arange(nsel)[None, :]
    mm = ((cc >= 4 * nn - 1) & (cc <= 4 * nn + 3) & (cc < ncmp)).astype(np.float32)
    bw = min(64, nsel)
    keyb = (np.arange(S_len) // 64) % bw
    c["erow"] = (np.arange(64)[:, None] == keyb[None, :]).astype(np.float32).astype(NPBF)
    c["mmat"] = np.ascontiguousarray(mm.reshape(nct, 128, nsel).transpose(1, 0, 2)).astype(NPBF)
    rel = np.arange(2 * nsel)[None, :] - nsel
    cur = (np.arange(128)[:, None] >= 64).astype(np.int64)
    fb = np.where((rel == cur) | (rel == cur - 1), 100.0, np.where(rel > cur, -100.0, 0.0))
    c["fb"] = fb.astype(np.float32)
    return c


def build_nsa_attn(S_len, ctx=None):
    nsel = S_len // 64
    ncmp = S_len // 16 - 1
    nct = (ncmp + 127) // 128
    ncp = nct * 128
    nq = S_len // 128
    bk = min(128, nsel)
    nbc = (nsel + 127) // 128
    ni = bk // 2
    VX = 65 + nsel
    nc = bass.Bass("TRN2", target_bir_lowering=False) if ctx is None else ctx[0].nc
    def din(name, shape, dt):
        return _io(nc, ctx, name, list(shape), dt, "ExternalInput")
    qT = din("qT", [64, 4, S_len], BF16)
    kc2 = din("kc2", [128, S_len], BF16)
    vc2 = din("vc2", [128, S_len], BF16)
    ksT = din("ksT", [64, S_len], BF16)
    kwT = din("kwT", [64, S_len], BF16)
    vs = din("vs", [S_len, 64], BF16)
    vw = din("vw", [S_len, 64], BF16)
    gates = din("gates", [S_len, 12], F32)
    w1s = din("w1s", [2, 128, 16, 64], F32)
    w2 = din("w2", [2, 64, 64], F32)
    pos2 = din("pos2", [2, 128, 16], F32)
    c_ident = din("ident", [128, 128], BF16)
    c_tle = din("tri_le", [128, 128], BF16)
    c_tgt = din("tri_gt", [128, 128], BF16)
    c_cb = din("cb", [128, 17, 128], BF16)
    c_mm = din("mmat", [128, nct, nsel], BF16)
    c_fb = din("fb", [128, 2 * nsel], F32)
    c_er = din("erow", [64, S_len], BF16)
    bw = min(64, nsel)
    nbc2 = (nsel + 63) // 64
    o = _io(nc, ctx, "o", [S_len, 256], BF16, "ExternalOutput")
    with _SchedCtx(nc, ctx) as (S, C, own):
        ident = S.sb([128, 128], BF16, "ident_sb")
        tle = S.sb([128, 128], BF16, "tle")
        tgt = S.sb([128, 128], BF16, "tgt")
        cb = S.sb([128, 17, 128], BF16, "cb")
        fb = S.sb([128, 2 * nsel], F32, "fb")
        ks_sb = S.sb([128, S_len], BF16, "ks_sb")
        kw_sb = S.sb([64, S_len], BF16, "kw_sb")
        vsx = S.sb([128, nq, 65], BF16, "vsx")
        vwx = S.sb([128, nq, 65], BF16, "vwx")
        vcx = S.sb([128, nct, VX], BF16, "vcx")
        kcmpT = S.sb([64, ncp], BF16, "kcmpT")
        w1b = S.sb([128, 2, 16, 64], BF16, "w1b")
        w2b = S.sb([64, 2, 64], BF16, "w2b")
        p2f = S.sb([128, 2, 16], F32, "p2f")
        p2b = S.sb([128, 2, 16], BF16, "p2b")
        GS = min(S_len, 8192)
        stage = S.sb([128, GS], BF16, "stage")
        hbias = S.sb([64, 2], F32, "hbias")
        actT = S.sb([64, 512], BF16, "actT")
        for (t, src) in ((ident, c_ident), (tle, c_tle), (tgt, c_tgt), (fb, c_fb)):
            S.dma("sp", t[:], src, writes=[t])
        for m0 in range(0, 17, 6):
            m1 = min(17, m0 + 6)
            S.dma("sp", cb[:, m0:m1, :], c_cb[:, m0:m1, :], writes=[cb])
        for (t, src) in ((ks_sb, ksT), (kw_sb, kwT), (ks_sb, c_er)):
            st = min(4096, S_len)
            r0 = 64 if src is c_er else 0
            for s0 in range(0, S_len, st):
                S.dma("sp", t[r0:r0 + 64, s0:s0 + st], src[:, s0:s0 + st], writes=[t])
        for (t, src) in ((vsx, vs), (vwx, vw)):
            S.op("pool", lambda e, t=t: e.memset(t[:, :, 64:65], 1.0), writes=[t])
            srcv = src.rearrange("(n p) d -> p n d", p=128)
            for n0 in range(0, nq, 8):
                S.dma("sp", t[:, n0:n0 + 8, 0:64], srcv[:, n0:n0 + 8, :], writes=[t])
        S.op("pool", lambda e: e.memset(vcx[:, :, 64:65], 1.0), writes=[vcx])
        for ct in range(nct):
            S.dma("sp", vcx[:, ct, 65:VX], c_mm[:, ct, :], writes=[vcx])
        for kv in range(2):
            S.dma("pool", w1b[:, kv, :, :], w1s[kv], writes=[w1b])
            S.dma("pool", w2b[:, kv, :], w2[kv], writes=[w2b])
            S.dma("sp", p2f[:, kv, :], pos2[kv], writes=[p2f])
        S.op("dve", lambda e: e.tensor_copy(out=p2b[:], in_=p2f[:]), reads=[p2f], writes=[p2b])
        pss = [S.ps([128, 512], F32, f"pss{i}") for i in range(2)]
        acc = [S.ps([128, 512], F32, f"acc{i}") for i in range(4)]
        pmisc = S.ps([128, 512], F32, "pmisc")
        for kv in range(2):
            src = kc2 if kv == 0 else vc2
            for j in range(16):
                S.op("pe", lambda e, j=j, kv=kv: e.matmul(pmisc[0:64, 0:1], lhsT=w1b[:, kv, j, :], rhs=p2b[:, kv, j:j + 1], start=(j == 0), stop=(j == 15)),
                     reads=[w1b, p2b], writes=[pmisc])
            S.op("dve", lambda e, kv=kv: e.tensor_copy(out=hbias[:, kv:kv + 1], in_=pmisc[0:64, 0:1]), reads=[pmisc], writes=[hbias])
            for g0 in range(0, ncp, 512):
                gn = min(512, ncp - g0)
                t0 = g0 * 16
                if t0 % GS == 0:
                    for s0 in range(0, GS, 2048):
                        S.dma("sp", stage[:, s0:s0 + 2048], src[:, t0 + s0:t0 + s0 + 2048], writes=[stage])
                tb = t0 % GS
                ph = pss[0]
                for j in range(16):
                    S.op("pe", lambda e, j=j, kv=kv, tb=tb, gn=gn, ph=ph: e.matmul(ph[0:64, 0:gn], lhsT=w1b[:, kv, j, :], rhs=stage[:, tb + j:tb + j + 16 * (gn - 1) + 1:16], start=(j == 0), stop=(j == 15)),
                         reads=[w1b, stage], writes=[ph])
                S.op("act", lambda e, kv=kv, gn=gn, ph=ph: e.activation(out=actT[:, 0:gn], in_=ph[0:64, 0:gn], func=AF.Silu, bias=hbias[:, kv:kv + 1]), reads=[ph, hbias], writes=[actT])
                if kv == 0:
                    pk = pss[1]
                    S.op("pe", lambda e, gn=gn, pk=pk: e.matmul(pk[0:64, 0:gn], lhsT=w2b[:, 0, :], rhs=actT[:, 0:gn], start=True, stop=True), reads=[w2b, actT], writes=[pk])
                    S.op("dve", lambda e, gn=gn, g0=g0, pk=pk: e.tensor_copy(out=kcmpT[:, g0:g0 + gn], in_=pk[0:64, 0:gn]), reads=[pk], writes=[kcmpT])
                else:
                    for s in range(gn // 128):
                        S.op("pe", lambda e, s=s: e.matmul(pmisc[:, 0:64], lhsT=actT[:, s * 128:(s + 1) * 128], rhs=w2b[:, 1, :], start=True, stop=True), reads=[w2b, actT], writes=[pmisc])
                        ct = g0 // 128 + s
                        S.op("dve", lambda e, ct=ct: e.tensor_copy(out=vcx[:, ct, 0:64], in_=pmisc[:, 0:64]), reads=[pmisc], writes=[vcx])
        qt = [S.sb([64, 4, 128], BF16, f"qt{i}") for i in range(2)]
        gt = [S.sb([128, 12], F32, f"gt{i}") for i in range(2)]
        pT = [S.sb([128, 512], BF16, f"pT{i}") for i in range(4)]
        ot = [S.sb([128, 256], F32, f"ot{i}") for i in range(2)]
        otb = [S.sb([128, 256], BF16, f"otb{i}") for i in range(2)]
        imp = S.sb([128, nsel], F32, "imp")
        scr = S.sb([128, nsel], F32, "scr")
        nb = S.sb([128, nsel], BF16, "nb")
        m8 = S.sb([128, 16], F32, "m8")
        nbpad = S.sb([128, nbc2, 128], BF16, "nbpad")
        S.op("pool", lambda e: e.memset(nbpad[:], 0.0), writes=[nbpad])
        qa = [[S.sb([128, 4, 128], BF16, f"qa{i}_{cc}") for cc in range(nbc2)] for i in range(2)]
        rz = S.sb([128, 4], F32, "rz")
        wgt = S.sb([128, 4], F32, "wgt")
        gv = gates.rearrange("(n p) f -> p n f", p=128)
        ov = o.rearrange("(n p) f -> p n f", p=128)
        out_toks = []
        pcount = [0]

        def load_q(j):
            S.dma("sp", qt[j % 2][:], qT[:, :, j * 128:(j + 1) * 128], writes=[qt[j % 2]])
            S.dma("sp", gt[j % 2][:], gv[:, j, :], writes=[gt[j % 2]])

        def branch(q, tiles, width, first_branch, gcol, g_t, o_t):
            n = len(tiles)
            sc = []

            def emit_scores(k):
                ps = pss[k % 2]
                mms = tiles[k][0]
                for idx, (lhsT_t, lhsT_ap, rhs_t, rhs_ap) in enumerate(mms):
                    S.op("pe", lambda e, ps=ps, a=lhsT_ap, b=rhs_ap, idx=idx, nm=len(mms): e.matmul(ps[:, 0:512].rearrange("p (h q) -> p h q", h=4), lhsT=a, rhs=b, start=(idx == 0), stop=(idx == nm - 1)),
                         reads=[lhsT_t, rhs_t], writes=[ps])

            def emit_exp(k):
                ps = pss[k % 2]
                p = pT[pcount[0] % 4]
                pcount[0] += 1
                S.op("act", lambda e, ps=ps, p=p: e.activation(out=p[:], in_=ps[:, 0:512], func=AF.Exp, scale=0.125), reads=[ps], writes=[p])
                return p

            def emit_pv(k, p):
                vt, vap = tiles[k][1]
                for h in range(4):
                    S.op("pe", lambda e, h=h, p=p, vap=vap, k=k: e.matmul(acc[h][:, 0:width], lhsT=p[:, h * 128:(h + 1) * 128], rhs=vap, start=(k == 0), stop=(k == n - 1)),
                         reads=[p, vt], writes=[acc[h]])

            emit_scores(0)
            for k in range(n):
                if k + 1 < n:
                    emit_scores(k + 1)
                p = emit_exp(k)
                emit_pv(k, p)
            for h in range(4):
                S.op("dve", lambda e, h=h: e.tensor_scalar(out=rz[:, h:h + 1], in0=acc[h][:, 64:65], scalar1=1e-30, scalar2=None, op0=ALU.max), reads=[acc[h]], writes=[rz])
            S.op("dve", lambda e: e.reciprocal(out=rz[:], in_=rz[:]), reads=[rz], writes=[rz])
            S.op("dve", lambda e: e.tensor_tensor(out=wgt[:], in0=rz[:], in1=g_t[:, gcol:12:3], op=ALU.mult), reads=[rz, g_t], writes=[wgt])
            for h in range(4):
                if first_branch:
                    S.op("dve", lambda e, h=h: e.tensor_scalar(out=o_t[:, h * 64:(h + 1) * 64], in0=acc[h][:, 0:64], scalar1=wgt[:, h:h + 1], scalar2=None, op0=ALU.mult),
                         reads=[acc[h], wgt], writes=[o_t])
                else:
                    S.op("dve", lambda e, h=h: e.scalar_tensor_tensor(out=o_t[:, h * 64:(h + 1) * 64], in0=acc[h][:, 0:64], scalar=wgt[:, h:h + 1], in1=o_t[:, h * 64:(h + 1) * 64], op0=ALU.mult, op1=ALU.add),
                         reads=[acc[h], wgt, o_t], writes=[o_t])

        load_q(0)
        for j in range(nq):
            q = qt[j % 2]
            g_t = gt[j % 2]
            o_t = ot[j % 2]
            if j + 1 < nq:
                load_q(j + 1)
            qap = q[:]
            nct_j = min(nct, (8 * j + 6) // 128 + 1)
            tiles = []
            for ct in range(nct_j):
                mms = [(kcmpT, kcmpT[:, ct * 128:(ct + 1) * 128], q, qap)]
                m = j - 16 * ct
                if m <= 16:
                    mms.append((ident, ident[:], cb, cb[:, max(m, 0), :].unsqueeze(1).to_broadcast([128, 4, 128])))
                tiles.append((mms, (vcx, vcx[:, ct, :])))
            branch(q, tiles, VX, True, 0, g_t, o_t)
            for h in range(4):
                if h == 0:
                    S.op("dve", lambda e: e.tensor_scalar(out=imp[:], in0=acc[0][:, 65:VX], scalar1=rz[:, 0:1], scalar2=None, op0=ALU.mult), reads=[acc[0], rz], writes=[imp])
                else:
                    S.op("dve", lambda e, h=h: e.scalar_tensor_tensor(out=imp[:], in0=acc[h][:, 65:VX], scalar=rz[:, h:h + 1], in1=imp[:], op0=ALU.mult, op1=ALU.add), reads=[acc[h], rz, imp], writes=[imp])
            S.op("dve", lambda e, j=j: e.tensor_tensor(out=imp[:], in0=imp[:], in1=fb[:, nsel - 2 * j:2 * nsel - 2 * j], op=ALU.add), reads=[imp, fb], writes=[imp])
            S.op("dve", lambda e: e.memset(imp[:, 0:1], 100.0), reads=[imp], writes=[imp])
            S.op("dve", lambda e: e.max(out=m8[:, 0:8], in_=imp[:]), reads=[imp], writes=[m8])
            S.op("dve", lambda e: e.match_replace(out=scr[:], in_to_replace=m8[:, 0:8], in_values=imp[:], imm_value=-1e30), reads=[imp, m8], writes=[scr])
            S.op("dve", lambda e: e.max(out=m8[:, 8:16], in_=scr[:]), reads=[scr], writes=[m8])
            S.op("dve", lambda e: e.tensor_scalar(out=nbpad[:, :, 64:64 + bw], in0=imp[:].rearrange("p (c b) -> p c b", c=nbc2), scalar1=m8[:, 15:16], scalar2=1.0, op0=ALU.is_ge, op1=ALU.subtract), reads=[imp, m8], writes=[nbpad])
            ptr = pmisc
            qas = qa[j % 2]
            for cc in range(nbc2):
                qc = qas[cc]
                S.op("pe", lambda e, cc=cc: e.transpose(out=ptr[:, 0:64].bitcast(BF16), in_=nbpad[:, cc, :], identity=ident[:]), reads=[nbpad, ident], writes=[ptr])
                S.op("dve", lambda e, qc=qc: e.tensor_scalar(out=qc[64:128, :, :], in0=ptr[64:128, 0:64].bitcast(BF16).unsqueeze(1).to_broadcast([64, 4, 128]), scalar1=BIG, scalar2=None, op0=ALU.mult), reads=[ptr], writes=[qc])
                S.op("pool", lambda e, qc=qc, q=q: e.tensor_copy(out=qc[0:64, :, :], in_=q[:]), reads=[q], writes=[qc])
            tiles = []
            for kt in range(j + 1):
                qc = qas[kt // (bw // 2)]
                mms = [(ks_sb, ks_sb[:, kt * 128:(kt + 1) * 128], qc, qc[:])]
                if kt == j:
                    mms.append((ident, ident[:], tle, tle[:].unsqueeze(1).to_broadcast([128, 4, 128])))
                tiles.append((mms, (vsx, vsx[:, kt, :])))
            branch(q, tiles, 65, False, 1, g_t, o_t)
            tiles = []
            for kt in range(max(0, j - 4), j + 1):
                mms = [(kw_sb, kw_sb[:, kt * 128:(kt + 1) * 128], q, qap)]
                if kt == j - 4:
                    mms.append((ident, ident[:], tgt, tgt[:].unsqueeze(1).to_broadcast([128, 4, 128])))
                if kt == j:
                    mms.append((ident, ident[:], tle, tle[:].unsqueeze(1).to_broadcast([128, 4, 128])))
                tiles.append((mms, (vwx, vwx[:, kt, :])))
            branch(q, tiles, 65, False, 2, g_t, o_t)
            ob_ = otb[j % 2]
            S.op("pool", lambda e, ob_=ob_, o_t=o_t: e.tensor_copy(out=ob_[:], in_=o_t[:]), reads=[o_t], writes=[ob_])
            out_toks.append(S.dma("sp", ov[:, j, :], ob_[:], reads=[ob_]))
        S.final_wait("sp", out_toks)
        if own:
            S.emit()
    return nc


def nsa_attn_inputs(qkT_b, vcT_b, vsw_b, gates_b, cmp_pos, cmp_w1, cmp_w2, g, consts):
    S_len = qkT_b.shape[1]
    qT = np.ascontiguousarray(qkT_b[256 * g:256 * g + 256].reshape(4, 64, S_len).transpose(1, 0, 2))
    kc = qkT_b[1024 + 64 * g:1024 + 64 * g + 64]
    ks = qkT_b[1280 + 64 * g:1280 + 64 * g + 64]
    kw = qkT_b[1536 + 64 * g:1536 + 64 * g + 64]
    vc = vcT_b[64 * g:64 * g + 64]

    def stack2(a):
        o = np.zeros((128, S_len), a.dtype)
        o[0:64] = a
        o[64:128, :S_len - 16] = a[:, 16:]
        return o
    d = {"qT": qT, "kc2": stack2(kc), "vc2": stack2(vc), "ksT": np.ascontiguousarray(ks), "kwT": np.ascontiguousarray(kw),
         "vs": np.ascontiguousarray(vsw_b[:, 64 * g:64 * g + 64]), "vw": np.ascontiguousarray(vsw_b[:, 256 + 64 * g:256 + 64 * g + 64]),
         "gates": np.ascontiguousarray(gates_b.reshape(S_len, 16, 3)[:, 4 * g:4 * g + 4, :].reshape(S_len, 12))}
    w1 = np.asarray(cmp_w1, np.float32).reshape(2, 2, 16, 64, 64)
    d["w1s"] = np.ascontiguousarray(w1.transpose(0, 1, 3, 2, 4).reshape(2, 128, 16, 64))
    d["w2"] = np.ascontiguousarray(np.asarray(cmp_w2, np.float32))
    p = np.asarray(cmp_pos, np.float32).reshape(2, 2, 16, 64)
    d["pos2"] = np.ascontiguousarray(p.transpose(0, 1, 3, 2).reshape(2, 128, 16))
    d.update(consts)
    return d


def build_out(T_tok, NT=512, ctx=None):
    nc = bass.Bass("TRN2", target_bir_lowering=False) if ctx is None else ctx[0].nc
    xT = _io(nc, ctx, "xT", [D, T_tok], F32, "ExternalInput")
    oT = _io(nc, ctx, "oT", [D, T_tok], BF16, "ExternalInput")
    w = _io(nc, ctx, "w", [D, D], F32, "ExternalInput")
    ng = _io(nc, ctx, "ng", [128, 8], F32, "ExternalInput")
    yT = _io(nc, ctx, "yT", [D, T_tok], F32, "ExternalOutput")
    nt = NT
    ntiles = T_tok // nt
    with _SchedCtx(nc, ctx) as (S, C, own):
        wb = S.sb([128, NCH, D], BF16, "wb")
        g = S.sb([128, 8], F32, "gains")
        S.dma("sp", g[:], ng, writes=[g])
        w_v = w.rearrange("(c p) f -> p c f", p=128)
        for c in range(NCH):
            S.dma("pool", wb[:, c, :], w_v[:, c, :], writes=[wb])
        xs = [S.sb([128, NCH, nt], F32, f"x{i}") for i in range(2)]
        os_ = [S.sb([128, NCH, nt], BF16, f"o{i}") for i in range(2)]
        y = S.sb([128, NCH, nt], F32, "y")
        sq = [S.sb([128, nt], BF16, f"sq{i}") for i in range(2)]
        rstd = S.sb([128, nt], F32, "rstd")
        tmp = [S.sb([128, nt], F32, f"tmp{i}") for i in range(2)]
        pstat = S.ps([128, 512], F32, "pstat")
        py = [S.ps([128, 512], F32, f"py{i}") for i in range(2)]
        xT_v = xT.rearrange("(c p) t -> p c t", p=128)
        oT_v = oT.rearrange("(c p) t -> p c t", p=128)
        yT_v = yT.rearrange("(c p) t -> p c t", p=128)
        toks = []

        def load(i):
            for c0 in range(0, NCH, 2):
                S.dma("sp", xs[i % 2][:, c0:c0 + 2, :], xT_v[:, c0:c0 + 2, i * nt:(i + 1) * nt], writes=[xs[i % 2]])
            for c0 in range(0, NCH, 4):
                S.dma("sp", os_[i % 2][:, c0:c0 + 4, :], oT_v[:, c0:c0 + 4, i * nt:(i + 1) * nt], writes=[os_[i % 2]])
        load(0)
        for i in range(ntiles):
            x, ob = xs[i % 2], os_[i % 2]
            if i + 1 < ntiles:
                load(i + 1)
            for m in range(NCH):
                p = py[m % 2]
                for c in range(NCH):
                    S.op("pe", lambda e, p=p, c=c, m=m, ob=ob: e.matmul(p[:, 0:nt], lhsT=wb[:, c, m * 128:(m + 1) * 128], rhs=ob[:, c, :], start=(c == 0), stop=(c == NCH - 1)),
                         reads=[wb, ob], writes=[p])
                S.op("act", lambda e, p=p, m=m: e.activation(out=y[:, m, :], in_=p[:, 0:nt], func=AF.Copy), reads=[p], writes=[y])

            def sqy(c):
                s = sq[c % 2]
                S.op("pool", lambda e, s=s, c=c: e.tensor_tensor(out=s[:], in0=y[:, c, :], in1=y[:, c, :], op=ALU.mult), reads=[y], writes=[s])
                return s, s[:]
            rms_stats(S, C, sqy, NCH, pstat, rstd, nt, D)
            for m in range(NCH):
                t = tmp[m % 2]
                S.op("dve", lambda e, m=m, t=t: e.scalar_tensor_tensor(out=t[:], in0=y[:, m, :], scalar=g[:, m:m + 1], in1=rstd[:], op0=ALU.mult, op1=ALU.mult),
                     reads=[y, g, rstd], writes=[t])
                S.op("pool", lambda e, m=m, t=t, x=x: e.tensor_tensor(out=x[:, m, :], in0=t[:], in1=x[:, m, :], op=ALU.add), reads=[t, x], writes=[x])
            for c0 in range(0, NCH, 2):
                toks.append(S.dma("sp", yT_v[:, c0:c0 + 2, i * nt:(i + 1) * nt], x[:, c0:c0 + 2, :], reads=[x]))
        S.final_wait("sp", toks)
        if own:
            S.emit()
    return nc


def build_ple(T_tok, NT=512, ctx=None):
    nc = bass.Bass("TRN2", target_bir_lowering=False) if ctx is None else ctx[0].nc
    xT = _io(nc, ctx, "xT", [D, T_tok], F32, "ExternalInput")
    pT = _io(nc, ctx, "pT", [256, T_tok], F32, "ExternalInput")
    w_p = _io(nc, ctx, "w_p", [256, D], F32, "ExternalInput")
    w_g = _io(nc, ctx, "w_g", [D, D], F32, "ExternalInput")
    ng = _io(nc, ctx, "ng", [128, 16], F32, "ExternalInput")
    yT = _io(nc, ctx, "yT", [D, T_tok], F32, "ExternalOutput")
    nt = NT
    ntiles = T_tok // nt
    with _SchedCtx(nc, ctx) as (S, C, own):
        wg = S.sb([128, NCH, D], BF16, "wg")
        wp = S.sb([128, 2, D], BF16, "wp")
        g = S.sb([128, 16], F32, "gains")
        S.dma("sp", g[:], ng, writes=[g])
        wg_v = w_g.rearrange("(c p) f -> p c f", p=128)
        wp_v = w_p.rearrange("(c p) f -> p c f", p=128)
        for c in range(NCH):
            S.dma("pool", wg[:, c, :], wg_v[:, c, :], writes=[wg])
        for c in range(2):
            S.dma("pool", wp[:, c, :], wp_v[:, c, :], writes=[wp])
        xs = [S.sb([128, NCH, nt], F32, f"x{i}") for i in range(2)]
        pb = [S.sb([128, 2, nt], BF16, f"p{i}") for i in range(2)]
        xn = S.sb([128, NCH, nt], BF16, "xn")
        v = S.sb([128, NCH, nt], F32, "v")
        gate = [S.sb([128, nt], F32, f"gate{i}") for i in range(2)]
        sq = [S.sb([128, nt], BF16, f"sq{i}") for i in range(2)]
        rstd = S.sb([128, nt], F32, "rstd")
        rstd2 = S.sb([128, nt], F32, "rstd2")
        tmp = [S.sb([128, nt], F32, f"tmp{i}") for i in range(2)]
        pstat = S.ps([128, 512], F32, "pstat")
        pgp = [S.ps([128, 512], F32, f"pgp{i}") for i in range(2)]
        pep = [S.ps([128, 512], F32, f"pep{i}") for i in range(2)]
        xT_v = xT.rearrange("(c p) t -> p c t", p=128)
        pT_v = pT.rearrange("(c p) t -> p c t", p=128)
        yT_v = yT.rearrange("(c p) t -> p c t", p=128)
        toks = []

        def load(i):
            for c0 in range(0, NCH, 2):
                S.dma("sp", xs[i % 2][:, c0:c0 + 2, :], xT_v[:, c0:c0 + 2, i * nt:(i + 1) * nt], writes=[xs[i % 2]])
            S.dma("pool", pb[i % 2][:], pT_v[:, :, i * nt:(i + 1) * nt], writes=[pb[i % 2]])
        load(0)
        for i in range(ntiles):
            x, pp = xs[i % 2], pb[i % 2]
            if i + 1 < ntiles:
                load(i + 1)

            def sqf(c, x=x):
                s = sq[c % 2]
                S.op("pool", lambda e, s=s, c=c: e.tensor_tensor(out=s[:], in0=x[:, c, :], in1=x[:, c, :], op=ALU.mult), reads=[x], writes=[s])
                return s, s[:]
            rms_stats(S, C, sqf, NCH, pstat, rstd, nt, D)
            for c in range(NCH):
                S.op("dve", lambda e, c=c, x=x: e.scalar_tensor_tensor(out=xn[:, c, :], in0=x[:, c, :], scalar=g[:, c:c + 1], in1=rstd[:], op0=ALU.mult, op1=ALU.mult),
                     reads=[x, g, rstd], writes=[xn])
            for m in range(NCH):
                a, b = pgp[m % 2], pep[m % 2]
                for c in range(NCH):
                    S.op("pe", lambda e, a=a, c=c, m=m: e.matmul(a[:, 0:nt], lhsT=wg[:, c, m * 128:(m + 1) * 128], rhs=xn[:, c, :], start=(c == 0), stop=(c == NCH - 1)),
                         reads=[wg, xn], writes=[a])
                for c in range(2):
                    S.op("pe", lambda e, b=b, c=c, m=m, pp=pp: e.matmul(b[:, 0:nt], lhsT=wp[:, c, m * 128:(m + 1) * 128], rhs=pp[:, c, :], start=(c == 0), stop=(c == 1)),
                         reads=[wp, pp], writes=[b])
                gt_ = gate[m % 2]
                S.op("act", lambda e, a=a, gt_=gt_: e.activation(out=gt_[:], in_=a[:, 0:nt], func=AF.Sigmoid), reads=[a], writes=[gt_])
                S.op("dve", lambda e, b=b, gt_=gt_, m=m: e.tensor_tensor(out=v[:, m, :], in0=b[:, 0:nt], in1=gt_[:], op=ALU.mult), reads=[b, gt_], writes=[v])

            def sqv(c):
                s = sq[c % 2]
                S.op("pool", lambda e, s=s, c=c: e.tensor_tensor(out=s[:], in0=v[:, c, :], in1=v[:, c, :], op=ALU.mult), reads=[v], writes=[s])
                return s, s[:]
            rms_stats(S, C, sqv, NCH, pstat, rstd2, nt, D)
            for m in range(NCH):
                t = tmp[m % 2]
                S.op("dve", lambda e, m=m, t=t: e.scalar_tensor_tensor(out=t[:], in0=v[:, m, :], scalar=g[:, 8 + m:9 + m], in1=rstd2[:], op0=ALU.mult, op1=ALU.mult),
                     reads=[v, g, rstd2], writes=[t])
                S.op("pool", lambda e, m=m, t=t, x=x: e.tensor_tensor(out=x[:, m, :], in0=t[:], in1=x[:, m, :], op=ALU.add), reads=[t, x], writes=[x])
            for c0 in range(0, NCH, 2):
                toks.append(S.dma("sp", yT_v[:, c0:c0 + 2, i * nt:(i + 1) * nt], x[:, c0:c0 + 2, :], reads=[x]))
        S.final_wait("sp", toks)
        if own:
            S.emit()
    return nc


def build_hgrnin(T_tok, NT=256, ctx=None):
    nc = bass.Bass("TRN2", target_bir_lowering=False) if ctx is None else ctx[0].nc
    xT = _io(nc, ctx, "xT", [D, T_tok], F32, "ExternalInput")
    w = _io(nc, ctx, "w", [D, 4096], F32, "ExternalInput")
    ng = _io(nc, ctx, "ng", [128, 8], F32, "ExternalInput")
    qfT = _io(nc, ctx, "qfT", [2048, T_tok], F32, "ExternalOutput")
    iv = _io(nc, ctx, "iv", [T_tok, D], BF16, "ExternalOutput")
    gs = _io(nc, ctx, "gs", [T_tok, D], F32, "ExternalOutput")
    nt = NT
    ntiles = T_tok // nt
    nsub = nt // 128
    with _SchedCtx(nc, ctx) as (S, C, own):
        wb = S.sb([128, NCH, 4096], BF16, "wb")
        g = S.sb([128, 8], F32, "gains")
        S.dma("sp", g[:], ng, writes=[g])
        w_v = w.rearrange("(c p) f -> p c f", p=128)
        for c in range(NCH):
            S.dma("pool", wb[:, c, :], w_v[:, c, :], writes=[wb])
        xs = [S.sb([128, NCH, nt], F32, f"x{i}") for i in range(2)]
        hn = S.sb([128, NCH, nt], BF16, "hn")
        sq = [S.sb([128, nt], BF16, f"sq{i}") for i in range(2)]
        rstd = S.sb([128, nt], F32, "rstd")
        ofm = [S.sb([128, 16, nt], F32, f"ofm{i}") for i in range(2)]
        oiv = [S.sb([128, nsub, D], BF16, f"oiv{i}") for i in range(2)]
        ogs = [S.sb([128, nsub, D], F32, f"ogs{i}") for i in range(2)]
        pstat = S.ps([128, 512], F32, "pstat")
        pa = [S.ps([128, 512], F32, f"pa{i}") for i in range(2)]
        pt = [S.ps([128, 512], F32, f"pt{i}") for i in range(2)]
        xT_v = xT.rearrange("(c p) t -> p c t", p=128)
        qf_v = qfT.rearrange("(c p) t -> p c t", p=128)
        iv_v = iv.rearrange("(s p) f -> p s f", p=128)
        gs_v = gs.rearrange("(s p) f -> p s f", p=128)
        toks = []

        def load(i):
            S.dma("sp", xs[i % 2][:], xT_v[:, :, i * nt:(i + 1) * nt], writes=[xs[i % 2]])
        load(0)
        for i in range(ntiles):
            x = xs[i % 2]
            if i + 1 < ntiles:
                load(i + 1)

            def sqf(c, x=x):
                s = sq[c % 2]
                S.op("pool", lambda e, s=s, c=c: e.tensor_tensor(out=s[:], in0=x[:, c, :], in1=x[:, c, :], op=ALU.mult), reads=[x], writes=[s])
                return s, s[:]
            rms_stats(S, C, sqf, NCH, pstat, rstd, nt, D)
            for c in range(NCH):
                S.op("dve", lambda e, c=c, x=x: e.scalar_tensor_tensor(out=hn[:, c, :], in0=x[:, c, :], scalar=g[:, c:c + 1], in1=rstd[:], op0=ALU.mult, op1=ALU.mult),
                     reads=[x, g, rstd], writes=[hn])
            of = ofm[i % 2]
            for j in range(16):
                a = pa[j % 2]
                for c in range(NCH):
                    S.op("pe", lambda e, a=a, c=c, j=j: e.matmul(a[:, 0:nt], lhsT=wb[:, c, j * 128:(j + 1) * 128], rhs=hn[:, c, :], start=(c == 0), stop=(c == NCH - 1)),
                         reads=[wb, hn], writes=[a])
                fn = AF.Silu if j < 8 else AF.Copy
                S.op("act", lambda e, a=a, j=j, of=of, fn=fn: e.activation(out=of[:, j, :], in_=a[:, 0:nt], func=fn), reads=[a], writes=[of])
            oi, og = oiv[i % 2], ogs[i % 2]
            for s in range(nsub):
                for hf in range(4):
                    p = pt[hf % 2]
                    for c in range(NCH):
                        S.op("pe", lambda e, p=p, c=c, s=s, hf=hf: e.matmul(p[:, 0:512], lhsT=hn[:, c, s * 128:(s + 1) * 128], rhs=wb[:, c, 2048 + hf * 512:2048 + (hf + 1) * 512], start=(c == 0), stop=(c == NCH - 1)),
                             reads=[wb, hn], writes=[p])
                    if hf < 2:
                        S.op("dve", lambda e, p=p, s=s, hf=hf, oi=oi: e.tensor_copy(out=oi[:, s, hf * 512:(hf + 1) * 512], in_=p[:, 0:512]), reads=[p], writes=[oi])
                    else:
                        S.op("act", lambda e, p=p, s=s, hf=hf, og=og: e.activation(out=og[:, s, (hf - 2) * 512:(hf - 1) * 512], in_=p[:, 0:512], func=AF.Silu), reads=[p], writes=[og])
            sl = slice(i * nt, (i + 1) * nt)
            toks.append(S.dma("sp", qf_v[:, 0:8, sl], of[:, 0:8, :], reads=[of]))
            toks.append(S.dma("sp", qf_v[:, 8:16, sl], of[:, 8:16, :], reads=[of]))
            toks.append(S.dma("sp", iv_v[:, i * nsub:(i + 1) * nsub, :], oi[:], reads=[oi]))
            toks.append(S.dma("sp", gs_v[:, i * nsub:(i + 1) * nsub, :], og[:], reads=[og]))
        S.final_wait("sp", toks)
        if own:
            S.emit()
    return nc


def build_hgrn(S_len, TB=1024, ctx=None):
    CH = 64
    nch = TB // CH
    nblk = S_len // TB
    nc = bass.Bass("TRN2", target_bir_lowering=False) if ctx is None else ctx[0].nc
    def din(name, shape, dt):
        return _io(nc, ctx, name, list(shape), dt, "ExternalInput")
    qT = din("qT", [256, S_len], F32)
    fT = din("fT", [256, S_len], F32)
    v = din("v", [S_len, 256], BF16)
    gs = din("gs", [S_len, 256], F32)
    lg = din("lg", [128, 2, 2], F32)
    gn = din("gn", [64, 128], F32)
    smask = din("smask", [128, TB], F32)
    cmask = din("cmask", [64, 64], F32)
    c_ident = din("ident", [128, 128], BF16)
    o = _io(nc, ctx, "o", [S_len, 256], BF16, "ExternalOutput")
    with _SchedCtx(nc, ctx) as (S, C, own):
        ident = S.sb([128, 128], BF16, "ident_sb")
        sm = S.sb([128, TB], F32, "smask_sb")
        cm = S.sb([64, 64], F32, "cmask_sb")
        gnt = S.sb([64, 128], F32, "gn_sb")
        lgt = S.sb([128, 2, 2], F32, "lg_sb")
        lb = S.sb([128, 2], F32, "lb")
        oml = S.sb([128, 2], F32, "oml")
        noml = S.sb([128, 2], F32, "noml")
        for (t, src) in ((ident, c_ident), (sm, smask), (cm, cmask), (gnt, gn), (lgt, lg)):
            S.dma("sp", t[:], src, writes=[t])
        S.op("dve", lambda e: e.tensor_tensor(out=lb[:], in0=lgt[:, 1, :], in1=lgt[:, 0, :], op=ALU.subtract), reads=[lgt], writes=[lb])
        S.op("act", lambda e: e.activation(out=lb[:], in_=lb[:], func=AF.Sigmoid), reads=[lb], writes=[lb])
        S.op("dve", lambda e: e.tensor_scalar(out=oml[:], in0=lb[:], scalar1=-1.0, scalar2=1.0, op0=ALU.mult, op1=ALU.add), reads=[lb], writes=[oml])
        S.op("dve", lambda e: e.tensor_scalar(out=noml[:], in0=oml[:], scalar1=-1.0, scalar2=None, op0=ALU.mult), reads=[oml], writes=[noml])
        def mk(name, shape, dt):
            return [S.sb(shape, dt, f"{name}{h}") for h in range(2)]
        fr = mk("fr", [128, TB], F32)
        qf = mk("qf", [128, TB], F32)
        t1 = mk("t1", [128, TB], F32)
        t2 = mk("t2", [128, TB], F32)
        t3 = mk("t3", [128, TB], F32)
        cum = mk("cum", [128, TB], F32)
        Qh = mk("Qh", [128, TB], BF16)
        Qt = mk("Qt", [128, TB], BF16)
        Kh = mk("Kh", [128, TB], BF16)
        Kt = [[S.sb([128, TB], BF16, f"Kt{h}_{i}") for i in range(4)] for h in range(2)]
        aa = mk("aa", [128, nch], F32)
        vb = mk("vb", [64, nch, 128], BF16)
        gsb = mk("gsb", [64, nch, 128], F32)
        ob = mk("ob", [64, nch, 128], F32)
        osq = mk("osq", [64, nch, 128], F32)
        ofin = mk("ofin", [64, nch, 128], BF16)
        ssum = mk("ssum", [64, nch], F32)
        state = mk("state", [128, 128], F32)
        sref = [[S.sb([128, 128], BF16, f"sref{h}_{i}") for i in range(2)] for h in range(2)]
        ATm = [[S.sb([64, 64], BF16, f"ATm{h}_{i}") for i in range(2)] for h in range(2)]
        Ktok = [[S.sb([64, 128], BF16, f"Ktok{h}_{i}") for i in range(2)] for h in range(2)]
        pA = [S.ps([128, 512], F32, f"pA{h}") for h in range(2)]
        pP = [S.ps([128, 512], F32, f"pP{h}") for h in range(2)]
        pO = [S.ps([128, 512], F32, f"pO{h}") for h in range(2)]
        for h in range(2):
            S.op("pool", lambda e, h=h: e.memset(state[h][:], 0.0), writes=[state[h]])
        v_v = v.rearrange("(c p) e -> p c e", p=CH)
        gs_v = gs.rearrange("(c p) e -> p c e", p=CH)
        o_v = o.rearrange("(c p) e -> p c e", p=CH)
        out_toks = []
        CLAMP = 1e30
        for b in range(nblk):
            tsl = slice(b * TB, (b + 1) * TB)
            csl = slice(b * nch, (b + 1) * nch)
            for h in range(2):
                hs = slice(h * 128, (h + 1) * 128)
                S.dma("sp", fr[h][:], fT[hs, tsl], writes=[fr[h]])
                S.dma("sp", qf[h][:], qT[hs, tsl], writes=[qf[h]])
                S.dma("sp", vb[h][:], v_v[:, csl, hs], writes=[vb[h]])
                S.dma("sp", gsb[h][:], gs_v[:, csl, hs], writes=[gsb[h]])
            for h in range(2):
                f_, q_, a1, a2, a3, cu = fr[h], qf[h], t1[h], t2[h], t3[h], cum[h]
                S.op("act", lambda e, f_=f_: e.activation(out=f_[:], in_=f_[:], func=AF.Sigmoid), reads=[f_], writes=[f_])
                S.op("dve", lambda e, f_=f_, a1=a1, h=h: e.tensor_scalar(out=a1[:], in0=f_[:], scalar1=oml[:, h:h + 1], scalar2=lb[:, h:h + 1], op0=ALU.mult, op1=ALU.add), reads=[f_, oml, lb], writes=[a1])
                S.op("pool", lambda e, f_=f_, a2=a2, h=h: e.tensor_scalar(out=a2[:], in0=f_[:], scalar1=noml[:, h:h + 1], scalar2=oml[:, h:h + 1], op0=ALU.mult, op1=ALU.add), reads=[f_, oml, noml], writes=[a2])
                S.op("act", lambda e, a1=a1: e.activation(out=a1[:], in_=a1[:], func=AF.Ln), reads=[a1], writes=[a1])
                S.op("dve", lambda e, a1=a1, cu=cu: e.tensor_tensor_scan(out=cu[:], data0=sm[:], data1=a1[:], initial=0.0, op0=ALU.mult, op1=ALU.add), reads=[sm, a1], writes=[cu])
                cu_v = cu[:].rearrange("p (c s) -> p c s", s=CH)
                cu_v16 = cu[:].rearrange("p (c s) -> p c s", s=16)
                S.op("act", lambda e, a1=a1, cu=cu: e.activation(out=a1[:], in_=cu[:], func=AF.Exp), reads=[cu], writes=[a1])
                S.op("pool", lambda e, q_=q_, a1=a1, h=h: e.tensor_tensor(out=Qh[h][:], in0=q_[:], in1=a1[:], op=ALU.mult), reads=[q_, a1], writes=[Qh[h]])
                S.op("dve", lambda e, a3=a3, cu_v16=cu_v16: e.tensor_tensor(out=a3[:].rearrange("p (c s) -> p c s", s=16), in0=cu_v16, in1=cu_v16[:, :, 0:1].to_broadcast([128, TB // 16, 16]), op=ALU.subtract), reads=[cu], writes=[a3])
                S.op("act", lambda e, a3=a3: e.activation(out=a3[:], in_=a3[:], func=AF.Exp), reads=[a3], writes=[a3])
                S.op("pool", lambda e, q_=q_, a3=a3, h=h: e.tensor_tensor(out=Qt[h][:], in0=q_[:], in1=a3[:], op=ALU.mult), reads=[q_, a3], writes=[Qt[h]])
                S.op("dve", lambda e, a1=a1, cu_v=cu_v: e.tensor_tensor(out=a1[:].rearrange("p (c s) -> p c s", s=CH), in0=cu_v, in1=cu_v[:, :, CH - 1:CH].to_broadcast([128, nch, CH]), op=ALU.subtract), reads=[cu], writes=[a1])
                S.op("act", lambda e, a1=a1: e.activation(out=a1[:], in_=a1[:], func=AF.Exp, scale=-1.0), reads=[a1], writes=[a1])
                S.op("pool", lambda e, a1=a1, a2=a2, h=h: e.tensor_tensor(out=Kh[h][:], in0=a2[:], in1=a1[:], op=ALU.mult), reads=[a1, a2], writes=[Kh[h]])
                for i in range(4):
                    w_ = a3 if i % 2 == 0 else a1
                    S.op("dve", lambda e, w_=w_, cu_v=cu_v, i=i: e.tensor_tensor(out=w_[:].rearrange("p (c s) -> p c s", s=CH), in0=cu_v, in1=cu_v[:, :, 16 * i:16 * i + 1].to_broadcast([128, nch, CH]), op=ALU.subtract), reads=[cu], writes=[w_])
                    S.op("pool", lambda e, w_=w_: e.tensor_scalar(out=w_[:], in0=w_[:], scalar1=-80.0, scalar2=None, op0=ALU.max), reads=[w_], writes=[w_])
                    S.op("act", lambda e, w_=w_: e.activation(out=w_[:], in_=w_[:], func=AF.Exp, scale=-1.0), reads=[w_], writes=[w_])
                    S.op("dve", lambda e, w_=w_, a2=a2, h=h, i=i: e.tensor_tensor(out=Kt[h][i][:], in0=w_[:], in1=a2[:], op=ALU.mult), reads=[w_, a2], writes=[Kt[h][i]])
                S.op("act", lambda e, cu_v=cu_v, h=h: e.activation(out=aa[h][:], in_=cu_v[:, :, CH - 1], func=AF.Exp), reads=[cu], writes=[aa[h]])
            for c in range(nch):
                for h in range(2):
                    cs_ = slice(c * CH, (c + 1) * CH)
                    i2 = c % 2
                    for i in range(4):
                        S.op("pe", lambda e, h=h, cs_=cs_, i=i, c=c: e.matmul(pA[h][0:64, 16 * i:16 * i + 16], lhsT=Kt[h][i][:, cs_], rhs=Qt[h][:, c * CH + 16 * i:c * CH + 16 * i + 16], start=True, stop=True),
                             reads=[Kt[h][i], Qt[h]], writes=[pA[h]])
                    atm = ATm[h][i2]
                    S.op("dve", lambda e, h=h, atm=atm: e.tensor_tensor(out=atm[:], in0=pA[h][0:64, 0:64], in1=cm[:], op=ALU.mult), reads=[pA[h], cm], writes=[atm])
                    ktv = pA[h][0:64, 256:320].bitcast(BF16)
                    S.op("pe", lambda e, h=h, cs_=cs_, ktv=ktv: e.transpose(out=ktv, in_=Kh[h][:, cs_], identity=ident[:]), reads=[Kh[h], ident], writes=[pA[h]])
                    kt_ = Ktok[h][i2]
                    S.op("act", lambda e, ktv=ktv, kt_=kt_, h=h: e.activation(out=kt_[:], in_=ktv, func=AF.Copy), reads=[pA[h]], writes=[kt_])
                    sr = sref[h][i2]
                    S.op("act", lambda e, h=h, sr=sr: e.activation(out=sr[:], in_=state[h][:], func=AF.Copy), reads=[state[h]], writes=[sr])
                    S.op("pe", lambda e, h=h, atm=atm, c=c: e.matmul(pO[h][0:64, 0:128], lhsT=atm[:], rhs=vb[h][:, c, :], start=True, stop=False), reads=[atm, vb[h]], writes=[pO[h]])
                    S.op("pe", lambda e, h=h, sr=sr, cs_=cs_: e.matmul(pO[h][0:64, 0:128], lhsT=Qh[h][:, cs_], rhs=sr[:], start=False, stop=True), reads=[Qh[h], sr], writes=[pO[h]])
                    S.op("act", lambda e, h=h, c=c: e.activation(out=ob[h][:, c, :], in_=pO[h][0:64, 0:128], func=AF.Copy), reads=[pO[h]], writes=[ob[h]])
                    S.op("pe", lambda e, h=h, kt_=kt_, c=c: e.matmul(pP[h][:, 0:128], lhsT=kt_[:], rhs=vb[h][:, c, :], start=True, stop=True), reads=[kt_, vb[h]], writes=[pP[h]])
                    S.op("dve", lambda e, h=h, c=c: e.scalar_tensor_tensor(out=state[h][:], in0=state[h][:], scalar=aa[h][:, c:c + 1], in1=pP[h][:, 0:128], op0=ALU.mult, op1=ALU.add), reads=[state[h], aa[h], pP[h]], writes=[state[h]])
            for h in range(2):
                hs = slice(h * 128, (h + 1) * 128)
                S.op("pool", lambda e, h=h: e.tensor_tensor(out=osq[h][:], in0=ob[h][:], in1=ob[h][:], op=ALU.mult), reads=[ob[h]], writes=[osq[h]])
                S.op("dve", lambda e, h=h: e.tensor_reduce(out=ssum[h][:], in_=osq[h][:], axis=AX.X, op=ALU.add), reads=[osq[h]], writes=[ssum[h]])
                S.op("act", lambda e, h=h: e.activation(out=ssum[h][:], in_=ssum[h][:], func=AF.Sqrt, scale=1.0 / 128, bias=EPS), reads=[ssum[h]], writes=[ssum[h]])
                S.op("dve", lambda e, h=h: e.reciprocal(out=ssum[h][:], in_=ssum[h][:]), reads=[ssum[h]], writes=[ssum[h]])
                S.op("dve", lambda e, h=h: e.tensor_tensor(out=ob[h][:], in0=ob[h][:], in1=ssum[h][:].unsqueeze(2).to_broadcast([64, nch, 128]), op=ALU.mult), reads=[ob[h], ssum[h]], writes=[ob[h]])
                S.op("pool", lambda e, h=h: e.tensor_tensor(out=osq[h][:], in0=gsb[h][:], in1=gnt[:].unsqueeze(1).to_broadcast([64, nch, 128]), op=ALU.mult), reads=[gsb[h], gnt], writes=[osq[h]])
                S.op("dve", lambda e, h=h: e.tensor_tensor(out=ofin[h][:], in0=ob[h][:], in1=osq[h][:], op=ALU.mult), reads=[ob[h], osq[h]], writes=[ofin[h]])
                out_toks.append(S.dma("sp", o_v[:, csl, hs], ofin[h][:], reads=[ofin[h]]))
        S.final_wait("sp", out_toks)
        if own:
            S.emit()
    return nc


def hgrn_consts(TB=1024):
    sm = np.ones((128, TB), np.float32)
    sm[:, ::64] = 0.0
    s = np.arange(64)[:, None]
    t = np.arange(64)[None, :]
    return {"smask": sm, "cmask": (s <= t).astype(np.float32), "ident": np.eye(128, dtype=np.float32).astype(NPBF)}


B_, S_, TPC = 2, 16384, 4096
_NC_CACHE = {}


def _prog(key, fn):
    if key not in _NC_CACHE:
        _NC_CACHE[key] = fn()
    return _NC_CACHE[key]


def _run(nc, in_maps):
    res = run_bass_kernel_spmd(nc, in_maps, core_ids=list(range(NCORES)))
    return res.results


def kernel(**inputs):
    return kernel5(**inputs)


_NOCC = False
A1_ROWS = 2608


def nsain2_phase(ctx, T_tok, NT=256):
    nc = ctx[0].nc
    A = ctx[2]
    xT, w_fm, ng, cs, all1 = A["xT"], A["w_fm"], A["ng"], A["cs"], A["all1"]
    nt = NT
    ntiles = T_tok // nt
    NF = 34
    with _SchedCtx(nc, ctx) as (S, C, own):
        wf = S.sb([128, NCH, NF * 128 + 48], BF16, "wf")
        g = S.sb([128, 8], F32, "gains")
        S.dma("sp", g[:], ng, writes=[g])
        w_fm_v = w_fm.rearrange("(c p) f -> p c f", p=128)
        for c in range(NCH):
            S.dma("pool", wf[:, c, :], w_fm_v[:, c, :], writes=[wf])
        xs = [S.sb([128, NCH, nt], F32, f"x{i}") for i in range(2)]
        cst = [S.sb([128, 2, nt], F32, f"cs{i}") for i in range(2)]
        hn = S.sb([128, NCH, nt], BF16, "hn")
        sq = [S.sb([128, nt], BF16, f"sq{i}") for i in range(2)]
        rstd = S.sb([128, nt], F32, "rstd")
        t1 = [S.sb([128, nt], F32, f"t1_{i}") for i in range(2)]
        t2 = [S.sb([128, nt], F32, f"t2_{i}") for i in range(2)]
        oall = [S.sb([128, 21, nt], BF16, f"oall{i}") for i in range(2)]
        pstat = S.ps([128, 512], F32, "pstat")
        pa = [S.ps([128, 512], F32, f"pa{i}") for i in range(2)]
        pb = [S.ps([128, 512], F32, f"pb{i}") for i in range(2)]
        xT_v = xT.rearrange("(c p) t -> p c t", p=128)
        a_v = all1[0:2560, :].rearrange("(c p) t -> p c t", p=128)
        toks = []

        def load(i):
            S.dma("sp", xs[i % 2][:], xT_v[:, :, i * nt:(i + 1) * nt], writes=[xs[i % 2]])
            S.dma("sp", cst[i % 2][:], cs[:, :, i * nt:(i + 1) * nt], writes=[cst[i % 2]])
        load(0)
        for i in range(ntiles):
            x = xs[i % 2]
            cs_t = cst[i % 2]
            if i + 1 < ntiles:
                load(i + 1)

            def sqf(c, x=x):
                s_ = sq[c % 2]
                S.op("pool", lambda e, s_=s_, c=c: e.tensor_tensor(out=s_[:], in0=x[:, c, :], in1=x[:, c, :], op=ALU.mult), reads=[x], writes=[s_])
                return s_, s_[:]
            rms_stats(S, C, sqf, NCH, pstat, rstd, nt, D)
            for c in range(NCH):
                S.op("dve", lambda e, c=c, x=x: e.scalar_tensor_tensor(out=hn[:, c, :], in0=x[:, c, :], scalar=g[:, c:c + 1], in1=rstd[:], op0=ALU.mult, op1=ALU.mult),
                     reads=[x, g, rstd], writes=[hn])
            oq = oall[i % 2]
            for j in range(NSA_NROPE):
                a, b = pa[j % 2], pb[j % 2]
                for c in range(NCH):
                    S.op("pe", lambda e, a=a, c=c, j=j: e.matmul(a[:, 0:nt], lhsT=wf[:, c, j * 128:(j + 1) * 128], rhs=hn[:, c, :], start=(c == 0), stop=(c == NCH - 1)),
                         reads=[wf, hn], writes=[a])
                for c in range(NCH):
                    S.op("pe", lambda e, b=b, c=c, j=j: e.matmul(b[:, 0:nt], lhsT=wf[:, c, (14 + j) * 128:(15 + j) * 128], rhs=hn[:, c, :], start=(c == 0), stop=(c == NCH - 1)),
                         reads=[wf, hn], writes=[b])
                u1, u2 = t1[j % 2], t2[j % 2]
                S.op("dve", lambda e, a=a, u1=u1, cs_t=cs_t: e.tensor_tensor(out=u1[:], in0=a[:, 0:nt], in1=cs_t[:, 0, :], op=ALU.mult), reads=[a, cs_t], writes=[u1])
                S.op("dve", lambda e, b=b, u2=u2, cs_t=cs_t: e.tensor_tensor(out=u2[:], in0=b[:, 0:nt], in1=cs_t[:, 1, :], op=ALU.mult), reads=[b, cs_t], writes=[u2])
                S.op("pool", lambda e, u1=u1, u2=u2, j=j, oq=oq: e.tensor_tensor(out=oq[:, j, :], in0=u1[:], in1=u2[:], op=ALU.add), reads=[u1, u2], writes=[oq])
            for j in range(6):
                a = pa[j % 2]
                for c in range(NCH):
                    S.op("pe", lambda e, a=a, c=c, j=j: e.matmul(a[:, 0:nt], lhsT=wf[:, c, (28 + j) * 128:(29 + j) * 128], rhs=hn[:, c, :], start=(c == 0), stop=(c == NCH - 1)),
                         reads=[wf, hn], writes=[a])
                S.op("act", lambda e, a=a, j=j, oq=oq: e.activation(out=oq[:, 14 + j, :], in_=a[:, 0:nt], func=AF.Copy), reads=[a], writes=[oq])
            a = pb[0]
            for c in range(NCH):
                S.op("pe", lambda e, a=a, c=c: e.matmul(a[0:48, 0:nt], lhsT=wf[:, c, NF * 128:NF * 128 + 48], rhs=hn[:, c, :], start=(c == 0), stop=(c == NCH - 1)),
                     reads=[wf, hn], writes=[a])
            S.op("act", lambda e, a=a, oq=oq: e.activation(out=oq[0:48, 20, :], in_=a[0:48, 0:nt], func=AF.Sigmoid), reads=[a], writes=[oq])
            sl = slice(i * nt, (i + 1) * nt)
            for c0 in range(0, 20, 5):
                toks.append(S.dma("sp", a_v[:, c0:c0 + 5, sl], oq[:, c0:c0 + 5, :], reads=[oq]))
            toks.append(S.dma("sp", all1[2560:2608, sl], oq[0:48, 20, :], reads=[oq]))
        S.final_wait("sp", toks)


def nsa_w_layout2(w):
    w = np.asarray(w, np.float32)
    rope_cols = np.concatenate([np.arange(0, 1024), np.arange(1024, 1280), np.arange(1536, 1792), np.arange(2048, 2304)])
    sw = rope_cols.reshape(-1, 2, 32)[:, ::-1, :].reshape(-1)
    return np.ascontiguousarray(np.concatenate([w[:, rope_cols], w[:, sw], w[:, 1280:1536], w[:, 1792:2048], w[:, 2304:2560], w[:, 2560:2608]], axis=1))


def nsaprep_phase(ctx, S_len, TPC_):
    nc = ctx[0].nc
    A = ctx[2]
    gat = A["gat1"]
    nk = S_len // TPC_
    CB = 512
    with _SchedCtx(nc, ctx) as (S, C, own):
        selq = S.sb([128, 8, 4, 64], BF16, "selq")
        selk = S.sb([128, 2, 64], BF16, "selk")
        selg = S.sb([48, 12], BF16, "selg")
        S.dma("sp", selq[:], A["selq"], writes=[selq])
        S.dma("sp", selk[:], A["selk"], writes=[selk])
        S.dma("sp", selg[:], A["selg"], writes=[selg])
        zt = S.sb([64, 16], BF16, "zt")
        S.op("pool", lambda e: e.memset(zt[:], 0.0), writes=[zt])
        toks = []
        for nm in ("kc2", "vc2"):
            toks.append(S.dma("sp", A[nm][64:128, S_len - 16:S_len], zt[:], reads=[zt]))
        X = [S.sb([128, 21, CB], BF16, f"X{i}") for i in range(2)]
        stq = [S.sb([64, 4, CB], BF16, f"stq{i}") for i in range(2)]
        stk = [S.sb([64, 4, CB], BF16, f"stk{i}") for i in range(2)]
        stv = [S.sb([128, 2, 4, 64], BF16, f"stv{i}") for i in range(2)]
        stg = [S.sb([128, 4, 12], F32, f"stg{i}") for i in range(2)]
        pq = [S.ps([128, 512], F32, f"pq{i}") for i in range(2)]
        pv = [S.ps([128, 512], F32, f"pv{i}") for i in range(2)]
        blocks = [(k, cb) for k in range(nk) for cb in range(TPC_ // CB)]

        def load(n):
            k, cb = blocks[n]
            x = X[n % 2]
            src = gat[k * A1_ROWS:k * A1_ROWS + 2560, cb * CB:(cb + 1) * CB].rearrange("(c p) t -> p c t", p=128)
            for c0 in range(0, 20, 5):
                S.dma("sp", x[:, c0:c0 + 5, :], src[:, c0:c0 + 5, :], reads=[A["GAT1"]], writes=[x])
            S.dma("sp", x[0:48, 20, :], gat[k * A1_ROWS + 2560:k * A1_ROWS + 2608, cb * CB:(cb + 1) * CB], reads=[A["GAT1"]], writes=[x])
        load(0)
        cnt = 0
        for n, (k, cb) in enumerate(blocks):
            x = X[n % 2]
            if n + 1 < len(blocks):
                load(n + 1)
            t0 = k * TPC_ + cb * CB
            sq_, sk_, sv_, sg_ = stq[n % 2], stk[n % 2], stv[n % 2], stg[n % 2]
            for h in range(4):
                p = pq[cnt % 2]
                cnt += 1
                for c in range(8):
                    S.op("pe", lambda e, p=p, c=c, h=h, x=x: e.matmul(p[0:64, 0:CB], lhsT=selq[:, c, h, :], rhs=x[:, c, :], start=(c == 0), stop=(c == 7)), reads=[selq, x], writes=[p])
                S.op("act" if h % 2 else "dve", (lambda e, p=p, h=h, sq_=sq_: e.activation(out=sq_[:, h, :], in_=p[0:64, 0:CB], func=AF.Copy)) if h % 2 else (lambda e, p=p, h=h, sq_=sq_: e.tensor_copy(out=sq_[:, h, :], in_=p[0:64, 0:CB])), reads=[p], writes=[sq_])
            for i4, c0 in enumerate((8, 10, 12, 14)):
                p = pq[cnt % 2]
                cnt += 1
                for c in range(2):
                    S.op("pe", lambda e, p=p, c=c, c0=c0, x=x: e.matmul(p[0:64, 0:CB], lhsT=selk[:, c, :], rhs=x[:, c0 + c, :], start=(c == 0), stop=(c == 1)), reads=[selk, x], writes=[p])
                S.op("act" if i4 % 2 else "dve", (lambda e, p=p, i4=i4, sk_=sk_: e.activation(out=sk_[:, i4, :], in_=p[0:64, 0:CB], func=AF.Copy)) if i4 % 2 else (lambda e, p=p, i4=i4, sk_=sk_: e.tensor_copy(out=sk_[:, i4, :], in_=p[0:64, 0:CB])), reads=[p], writes=[sk_])
            for wch, c0 in enumerate((16, 18)):
                p = pv[wch]
                for sub in range(4):
                    for c in range(2):
                        S.op("pe", lambda e, p=p, c=c, c0=c0, sub=sub, x=x: e.matmul(p[:, sub * 64:(sub + 1) * 64], lhsT=x[:, c0 + c, sub * 128:(sub + 1) * 128], rhs=selk[:, c, :], start=(c == 0), stop=(c == 1)), reads=[selk, x], writes=[p])
                S.op("dve", lambda e, p=p, wch=wch, sv_=sv_: e.tensor_copy(out=sv_[:, wch, :, :], in_=p[:, 0:256].rearrange("p (s d) -> p s d", s=4)), reads=[p], writes=[sv_])
            p = pv[0]
            for sub in range(4):
                S.op("pe", lambda e, p=p, sub=sub, x=x: e.matmul(p[:, 256 + sub * 12:256 + (sub + 1) * 12], lhsT=x[0:48, 20, sub * 128:(sub + 1) * 128], rhs=selg[:], start=True, stop=True), reads=[selg, x], writes=[p])
            S.op("act", lambda e, p=p, sg_=sg_: e.activation(out=sg_[:], in_=p[:, 256:304].rearrange("p (s d) -> p s d", s=4), func=AF.Copy), reads=[p], writes=[sg_])
            toks.append(S.dma("sp", A["qT"][:, :, t0:t0 + CB], sq_[:], reads=[sq_]))
            toks.append(S.dma("sp", A["kc2"][0:64, t0:t0 + CB], sk_[:, 0, :], reads=[sk_]))
            toks.append(S.dma("sp", A["ksT"][:, t0:t0 + CB], sk_[:, 1, :], reads=[sk_]))
            toks.append(S.dma("sp", A["kwT"][:, t0:t0 + CB], sk_[:, 2, :], reads=[sk_]))
            toks.append(S.dma("sp", A["vc2"][0:64, t0:t0 + CB], sk_[:, 3, :], reads=[sk_]))
            for (nm, i4) in (("kc2", 0), ("vc2", 3)):
                if t0 == 0:
                    toks.append(S.dma("sp", A[nm][64:128, 0:CB - 16], sk_[:, i4, 16:CB], reads=[sk_]))
                else:
                    toks.append(S.dma("sp", A[nm][64:128, t0 - 16:t0 + CB - 16], sk_[:, i4, :], reads=[sk_]))
            toks.append(S.dma("sp", A["vs"][t0:t0 + CB, :].rearrange("(s p) d -> p s d", p=128), sv_[:, 0, :, :], reads=[sv_]))
            toks.append(S.dma("sp", A["vw"][t0:t0 + CB, :].rearrange("(s p) d -> p s d", p=128), sv_[:, 1, :, :], reads=[sv_]))
            toks.append(S.dma("sp", A["gates"][t0:t0 + CB, :].rearrange("(s p) d -> p s d", p=128), sg_[:], reads=[sg_]))
        S.final_wait("sp", toks)


def nsaprep_sel(g):
    selq = np.zeros((128, 8, 4, 64), np.float32)
    for h in range(4):
        hd = 4 * g + h
        c, off = hd // 2, (hd % 2) * 64
        selq[off + np.arange(64), c, h, np.arange(64)] = 1.0
    selk = np.zeros((128, 2, 64), np.float32)
    selk[(g % 2) * 64 + np.arange(64), g // 2, np.arange(64)] = 1.0
    selg = np.zeros((48, 12), np.float32)
    selg[12 * g + np.arange(12), np.arange(12)] = 1.0
    return {"selq": selq.astype(NPBF), "selk": selk.astype(NPBF), "selg": selg.astype(NPBF)}


def out2_phase(ctx, S_len, TPC_, NT=256):
    nc = ctx[0].nc
    A = ctx[2]
    xT, w, ng, yT, ogat = A["xT"], A["w"], A["ng"], A["yT"], A["ogat"]
    nt = NT
    ntiles = TPC_ // nt
    nsub = nt // 128
    with _SchedCtx(nc, ctx) as (S, C, own):
        wb = S.sb([128, NCH, D], BF16, "wb")
        g = S.sb([128, 8], F32, "gains")
        wsel = S.sb([128, 4, 128], BF16, "wsel")
        S.dma("sp", g[:], ng, writes=[g])
        S.dma("sp", wsel[:], A["wsel"], writes=[wsel])
        w_v = w.rearrange("(c p) f -> p c f", p=128)
        for c in range(NCH):
            S.dma("pool", wb[:, c, :], w_v[:, c, :], writes=[wb])
        xs = [S.sb([128, NCH, nt], F32, f"x{i}") for i in range(2)]
        cand = [S.sb([128, 4, 4, 256], BF16, f"cand{i}") for i in range(2)]
        os_ = S.sb([128, NCH, nt], BF16, "os")
        y = S.sb([128, NCH, nt], F32, "y")
        sq = [S.sb([128, nt], BF16, f"sq{i}") for i in range(2)]
        rstd = S.sb([128, nt], F32, "rstd")
        tmp = [S.sb([128, nt], F32, f"tmp{i}") for i in range(2)]
        pstat = S.ps([128, 512], F32, "pstat")
        py = [S.ps([128, 512], F32, f"py{i}") for i in range(2)]
        psel = [S.ps([128, 512], F32, f"psel{i}") for i in range(2)]
        xT_v = xT.rearrange("(c p) t -> p c t", p=128)
        yT_v = yT.rearrange("(c p) t -> p c t", p=128)
        og_v = ogat.rearrange("(g r t) f -> t g r f", g=4, r=S_len // TPC_)
        toks = []
        subs = [(i, s_) for i in range(ntiles) for s_ in range(nsub)]

        def load_x(i):
            for c0 in range(0, NCH, 4):
                S.dma("sp", xs[i % 2][:, c0:c0 + 4, :], xT_v[:, c0:c0 + 4, i * nt:(i + 1) * nt], writes=[xs[i % 2]])

        def load_c(n):
            i, s_ = subs[n]
            t0 = i * nt + s_ * 128
            for gg in range(4):
                S.dma("sp", cand[n % 2][:, gg, :, :], og_v[t0:t0 + 128, gg, :, :], reads=[A["OGAT"]], writes=[cand[n % 2]])
        load_x(0)
        load_c(0)
        n = 0
        for i in range(ntiles):
            x = xs[i % 2]
            if i + 1 < ntiles:
                load_x(i + 1)
            for s_ in range(nsub):
                cd = cand[n % 2]
                if n + 1 < len(subs):
                    load_c(n + 1)
                n += 1
                for half in range(2):
                    p = psel[half]
                    for q4 in range(4):
                        ch = half * 4 + q4
                        gg, fc = ch // 2, ch % 2
                        for r_ in range(4):
                            S.op("pe", lambda e, p=p, q4=q4, gg=gg, fc=fc, r_=r_, cd=cd: e.matmul(p[:, q4 * 128:(q4 + 1) * 128], lhsT=cd[:, gg, r_, fc * 128:(fc + 1) * 128], rhs=wsel[:, r_, :], start=(r_ == 0), stop=(r_ == 3)),
                                 reads=[cd, wsel], writes=[p])
                    if half == 0:
                        S.op("act", lambda e, p=p, s_=s_: e.activation(out=os_[:, 0:4, s_ * 128:(s_ + 1) * 128], in_=p[:, 0:512].rearrange("p (c t) -> p c t", c=4), func=AF.Copy), reads=[p], writes=[os_])
                    else:
                        S.op("dve", lambda e, p=p, s_=s_: e.tensor_copy(out=os_[:, 4:8, s_ * 128:(s_ + 1) * 128], in_=p[:, 0:512].rearrange("p (c t) -> p c t", c=4)), reads=[p], writes=[os_])
            for m in range(NCH):
                p = py[m % 2]
                for c in range(NCH):
                    S.op("pe", lambda e, p=p, c=c, m=m: e.matmul(p[:, 0:nt], lhsT=wb[:, c, m * 128:(m + 1) * 128], rhs=os_[:, c, :], start=(c == 0), stop=(c == NCH - 1)),
                         reads=[wb, os_], writes=[p])
                S.op("act", lambda e, p=p, m=m: e.activation(out=y[:, m, :], in_=p[:, 0:nt], func=AF.Copy), reads=[p], writes=[y])

            def sqy(c):
                s2 = sq[c % 2]
                S.op("pool", lambda e, s2=s2, c=c: e.tensor_tensor(out=s2[:], in0=y[:, c, :], in1=y[:, c, :], op=ALU.mult), reads=[y], writes=[s2])
                return s2, s2[:]
            rms_stats(S, C, sqy, NCH, pstat, rstd, nt, D)
            for m in range(NCH):
                t = tmp[m % 2]
                S.op("dve", lambda e, m=m, t=t: e.scalar_tensor_tensor(out=t[:], in0=y[:, m, :], scalar=g[:, m:m + 1], in1=rstd[:], op0=ALU.mult, op1=ALU.mult),
                     reads=[y, g, rstd], writes=[t])
                S.op("pool", lambda e, m=m, t=t, x=x: e.tensor_tensor(out=x[:, m, :], in0=t[:], in1=x[:, m, :], op=ALU.add), reads=[t, x], writes=[x])
            for c0 in range(0, NCH, 4):
                toks.append(S.dma("sp", yT_v[:, c0:c0 + 4, i * nt:(i + 1) * nt], x[:, c0:c0 + 4, :], reads=[x]))
        S.final_wait("sp", toks)


def out2_sel(r):
    w = np.zeros((128, 4, 128), np.float32)
    w[np.arange(128), r, np.arange(128)] = 1.0
    return w.astype(NPBF)


def hgrnin2_phase(ctx, T_tok, NT=256):
    nc = ctx[0].nc
    A = ctx[2]
    xT, w, ng, a3a, a3b = A["xT"], A["w"], A["ng"], A["all3a"], A["all3b"]
    nt = NT
    ntiles = T_tok // nt
    with _SchedCtx(nc, ctx) as (S, C, own):
        wb = S.sb([128, NCH, 4096], BF16, "wb")
        g = S.sb([128, 8], F32, "gains")
        S.dma("sp", g[:], ng, writes=[g])
        w_v = w.rearrange("(c p) f -> p c f", p=128)
        for c in range(NCH):
            S.dma("pool", wb[:, c, :], w_v[:, c, :], writes=[wb])
        xs = [S.sb([128, NCH, nt], F32, f"x{i}") for i in range(2)]
        hn = S.sb([128, NCH, nt], BF16, "hn")
        sq = [S.sb([128, nt], BF16, f"sq{i}") for i in range(2)]
        rstd = S.sb([128, nt], F32, "rstd")
        of = [S.sb([128, 8, nt], F32, f"of{i}") for i in range(2)]
        ob = [S.sb([128, 24, nt], BF16, f"ob{i}") for i in range(2)]
        pstat = S.ps([128, 512], F32, "pstat")
        pa = [S.ps([128, 512], F32, f"pa{i}") for i in range(3)]
        xT_v = xT.rearrange("(c p) t -> p c t", p=128)
        a_v = a3a.rearrange("(c p) t -> p c t", p=128)
        b_v = a3b.rearrange("(c p) t -> p c t", p=128)
        toks = []

        def load(i):
            for c0 in range(0, NCH, 4):
                S.dma("sp", xs[i % 2][:, c0:c0 + 4, :], xT_v[:, c0:c0 + 4, i * nt:(i + 1) * nt], writes=[xs[i % 2]])
        load(0)
        for i in range(ntiles):
            x = xs[i % 2]
            if i + 1 < ntiles:
                load(i + 1)

            def sqf(c, x=x):
                s_ = sq[c % 2]
                S.op("pool", lambda e, s_=s_, c=c: e.tensor_tensor(out=s_[:], in0=x[:, c, :], in1=x[:, c, :], op=ALU.mult), reads=[x], writes=[s_])
                return s_, s_[:]
            rms_stats(S, C, sqf, NCH, pstat, rstd, nt, D)
            for c in range(NCH):
                S.op("dve", lambda e, c=c, x=x: e.scalar_tensor_tensor(out=hn[:, c, :], in0=x[:, c, :], scalar=g[:, c:c + 1], in1=rstd[:], op0=ALU.mult, op1=ALU.mult),
                     reads=[x, g, rstd], writes=[hn])
            o_f, o_b = of[i % 2], ob[i % 2]
            for j in range(32):
                a = pa[j % 3]
                for c in range(NCH):
                    S.op("pe", lambda e, a=a, c=c, j=j: e.matmul(a[:, 0:nt], lhsT=wb[:, c, j * 128:(j + 1) * 128], rhs=hn[:, c, :], start=(c == 0), stop=(c == NCH - 1)),
                         reads=[wb, hn], writes=[a])
                if j < 8:
                    S.op("act", lambda e, a=a, j=j, o_b=o_b: e.activation(out=o_b[:, j, :], in_=a[:, 0:nt], func=AF.Silu), reads=[a], writes=[o_b])
                elif j < 16:
                    S.op("dve", lambda e, a=a, j=j, o_f=o_f: e.tensor_copy(out=o_f[:, j - 8, :], in_=a[:, 0:nt]), reads=[a], writes=[o_f])
                elif j < 24:
                    S.op("dve", lambda e, a=a, j=j, o_b=o_b: e.tensor_copy(out=o_b[:, j - 8, :], in_=a[:, 0:nt]), reads=[a], writes=[o_b])
                else:
                    S.op("act", lambda e, a=a, j=j, o_b=o_b: e.activation(out=o_b[:, j - 8, :], in_=a[:, 0:nt], func=AF.Silu), reads=[a], writes=[o_b])
            sl = slice(i * nt, (i + 1) * nt)
            for c0 in range(0, 8, 4):
                toks.append(S.dma("sp", a_v[:, c0:c0 + 4, sl], o_f[:, c0:c0 + 4, :], reads=[o_f]))
            for c0 in range(0, 24, 6):
                toks.append(S.dma("sp", b_v[:, c0:c0 + 6, sl], o_b[:, c0:c0 + 6, :], reads=[o_b]))
        S.final_wait("sp", toks)


def hgrnprep_phase(ctx, S_len, TPC_):
    nc = ctx[0].nc
    A = ctx[2]
    ga, gb = A["gat3a"], A["gat3b"]
    nk = S_len // TPC_
    CB = 512
    with _SchedCtx(nc, ctx) as (S, C, own):
        wI = S.sb([128, 8, 2, 128], BF16, "wI")
        wsc = S.sb([128, 16], F32, "wsc")
        S.dma("sp", wI[:], A["wI"], writes=[wI])
        S.dma("sp", wsc[:], A["wsc"], writes=[wsc])
        Xb = [S.sb([128, 24, CB], BF16, f"Xb{i}") for i in range(2)]
        Xf = [S.sb([128, 8, CB], F32, f"Xf{i}") for i in range(2)]
        sq_ = [S.sb([128, 2, CB], F32, f"sq{i}") for i in range(2)]
        sf_ = [S.sb([128, 2, CB], F32, f"sf{i}") for i in range(2)]
        sv_ = [S.sb([128, 4, 256], BF16, f"sv{i}") for i in range(2)]
        sg_ = [S.sb([128, 4, 256], F32, f"sg{i}") for i in range(2)]
        pq = [S.ps([128, 512], F32, f"pq{i}") for i in range(2)]
        pv = [S.ps([128, 512], F32, f"pv{i}") for i in range(4)]
        blocks = [(k, cb) for k in range(nk) for cb in range(TPC_ // CB)]
        toks = []

        def load(n):
            k, cb = blocks[n]
            sb_ = gb[k * 3072:(k + 1) * 3072, cb * CB:(cb + 1) * CB].rearrange("(c p) t -> p c t", p=128)
            sa_ = ga[k * 1024:(k + 1) * 1024, cb * CB:(cb + 1) * CB].rearrange("(c p) t -> p c t", p=128)
            for c0 in range(0, 24, 6):
                S.dma("sp", Xb[n % 2][:, c0:c0 + 6, :], sb_[:, c0:c0 + 6, :], reads=[A["GAT3B"]], writes=[Xb[n % 2]])
            for c0 in range(0, 8, 2):
                S.dma("sp", Xf[n % 2][:, c0:c0 + 2, :], sa_[:, c0:c0 + 2, :], reads=[A["GAT3A"]], writes=[Xf[n % 2]])
        load(0)
        for n, (k, cb) in enumerate(blocks):
            xb, xf = Xb[n % 2], Xf[n % 2]
            if n + 1 < len(blocks):
                load(n + 1)
            t0 = k * TPC_ + cb * CB
            q_o, f_o, v_o, g_o = sq_[n % 2], sf_[n % 2], sv_[n % 2], sg_[n % 2]
            for hh in range(2):
                p = pq[hh]
                for c in range(8):
                    S.op("pe", lambda e, p=p, c=c, hh=hh, xb=xb: e.matmul(p[:, 0:CB], lhsT=wI[:, c, hh, :], rhs=xb[:, c, :], start=(c == 0), stop=(c == 7)), reads=[wI, xb], writes=[p])
                S.op("act", lambda e, p=p, hh=hh, q_o=q_o: e.activation(out=q_o[:, hh, :], in_=p[:, 0:CB], func=AF.Copy), reads=[p], writes=[q_o])
                for c in range(8):
                    if c == 0:
                        S.op("dve", lambda e, hh=hh, xf=xf, f_o=f_o: e.tensor_scalar(out=f_o[:, hh, :], in0=xf[:, 0, :], scalar1=wsc[:, hh:hh + 1], scalar2=None, op0=ALU.mult), reads=[xf, wsc], writes=[f_o])
                    else:
                        S.op("dve", lambda e, hh=hh, c=c, xf=xf, f_o=f_o: e.scalar_tensor_tensor(out=f_o[:, hh, :], in0=xf[:, c, :], scalar=wsc[:, 2 * c + hh:2 * c + hh + 1], in1=f_o[:, hh, :], op0=ALU.mult, op1=ALU.add), reads=[xf, wsc, f_o], writes=[f_o])
                for wch in range(2):
                    p = pv[wch * 2 + hh]
                    for sub in range(4):
                        for c in range(8):
                            S.op("pe", lambda e, p=p, c=c, hh=hh, sub=sub, wch=wch, xb=xb: e.matmul(p[:, sub * 128:(sub + 1) * 128], lhsT=xb[:, 8 + 8 * wch + c, sub * 128:(sub + 1) * 128], rhs=wI[:, c, hh, :], start=(c == 0), stop=(c == 7)), reads=[wI, xb], writes=[p])
                    dst = v_o if wch == 0 else g_o
                    if wch == 0:
                        S.op("act", lambda e, p=p, hh=hh, dst=dst: e.activation(out=dst[:, :, hh * 128:(hh + 1) * 128], in_=p[:, 0:512].rearrange("p (s d) -> p s d", s=4), func=AF.Copy), reads=[p], writes=[dst])
                    else:
                        S.op("dve", lambda e, p=p, hh=hh, dst=dst: e.tensor_copy(out=dst[:, :, hh * 128:(hh + 1) * 128], in_=p[:, 0:512].rearrange("p (s d) -> p s d", s=4)), reads=[p], writes=[dst])
            toks.append(S.dma("sp", A["qT"][:, t0:t0 + CB].rearrange("(h p) t -> p h t", p=128), q_o[:], reads=[q_o]))
            toks.append(S.dma("sp", A["fT"][:, t0:t0 + CB].rearrange("(h p) t -> p h t", p=128), f_o[:], reads=[f_o]))
            toks.append(S.dma("sp", A["v"][t0:t0 + CB, :].rearrange("(s p) d -> p s d", p=128), v_o[:], reads=[v_o]))
            toks.append(S.dma("sp", A["gs"][t0:t0 + CB, :].rearrange("(s p) d -> p s d", p=128), g_o[:], reads=[g_o]))
        S.final_wait("sp", toks)


def hgrnprep_sel(r2):
    wI = np.zeros((128, 8, 2, 128), np.float32)
    wsc = np.zeros((128, 16), np.float32)
    for hh in range(2):
        c = 2 * r2 + hh
        wI[np.arange(128), c, hh, np.arange(128)] = 1.0
        wsc[:, 2 * c + hh] = 1.0
    return {"wI": wI.astype(NPBF), "wsc": wsc}


def build_fused(S_len, TPC_):
    nc = bass.Bass("TRN2", target_bir_lowering=False)
    nk = S_len // TPC_
    groups = [[0, 1, 2, 3], [4, 5, 6, 7]]
    I32 = mybir.dt.int32

    def ein(name, shape, dt):
        return nc.dram_tensor(name, list(shape), dt, kind="ExternalInput").ap()

    def itn(name, shape, dt):
        return nc.dram_tensor(name, list(shape), dt).ap()
    nsel = S_len // 64
    ncmp = S_len // 16 - 1
    nct = (ncmp + 127) // 128
    bk = min(128, nsel)
    E = {}
    E["xT"] = ein("xT", [D, TPC_], F32)
    for l in range(2):
        E[f"pT{l}"] = ein(f"pT{l}", [256, TPC_], F32)
        E[f"ng{l}"] = ein(f"ng{l}", [128, 64], F32)
        for k in range(2):
            E[f"fwi{l}{k}"] = ein(f"fwi{l}{k}", [D, 2 * DFF], F32)
            E[f"fwo{l}{k}"] = ein(f"fwo{l}{k}", [DFF, D], F32)
        E[f"wp{l}"] = ein(f"wp{l}", [256, D], F32)
        E[f"wg{l}"] = ein(f"wg{l}", [D, D], F32)
    E["nsa_wfm"] = ein("nsa_wfm", [D, 34 * 128 + 48], F32)
    E["nsa_wo"] = ein("nsa_wo", [D, D], F32)
    E["cs"] = ein("cs", [128, 2, TPC_], F32)
    E["hg_w"] = ein("hg_w", [D, 4096], F32)
    E["hg_wo"] = ein("hg_wo", [D, D], F32)
    for nm, shp, dt in (("selq", [128, 8, 4, 64], BF16), ("selk", [128, 2, 64], BF16), ("selg", [48, 12], BF16), ("wsel", [128, 4, 128], BF16),
                        ("wI", [128, 8, 2, 128], BF16), ("wsc", [128, 16], F32),
                        ("w1s", [2, 128, 16, 64], F32), ("w2", [2, 64, 64], F32), ("pos2", [2, 128, 16], F32),
                        ("ident", [128, 128], BF16), ("tri_le", [128, 128], BF16), ("tri_gt", [128, 128], BF16), ("cb", [128, 17, 128], BF16),
                        ("erow", [64, S_len], BF16), ("mmat", [128, nct, nsel], BF16), ("fb", [128, 2 * nsel], F32),
                        ("lg", [128, 2, 2], F32), ("gn", [64, 128], F32), ("smask", [128, 1024], F32), ("cmask", [64, 64], F32)):
        E[nm] = ein(nm, shp, dt)
    yT = nc.dram_tensor("yT", [D, TPC_], F32, kind="ExternalOutput").ap()
    xa = itn("x_a", [D, TPC_], F32)
    xb_ = itn("x_b", [D, TPC_], F32)
    all1 = itn("all1", [A1_ROWS, TPC_], BF16)
    gat1 = itn("gat1", [nk * A1_ROWS, TPC_], BF16)
    m1 = {"qT": itn("m1_qT", [64, 4, S_len], BF16), "kc2": itn("m1_kc2", [128, S_len], BF16), "vc2": itn("m1_vc2", [128, S_len], BF16),
          "ksT": itn("m1_ksT", [64, S_len], BF16), "kwT": itn("m1_kwT", [64, S_len], BF16), "vs": itn("m1_vs", [S_len, 64], BF16),
          "vw": itn("m1_vw", [S_len, 64], BF16), "gates": itn("m1_gates", [S_len, 12], F32)}
    o1 = itn("o1", [S_len, 256], BF16)
    og1 = itn("og1", [nk * S_len, 256], BF16)
    a3a = itn("all3a", [1024, TPC_], F32)
    a3b = itn("all3b", [3072, TPC_], BF16)
    g3a = itn("gat3a", [nk * 1024, TPC_], F32)
    g3b = itn("gat3b", [nk * 3072, TPC_], BF16)
    m2 = {"qT": itn("m2_qT", [256, S_len], F32), "fT": itn("m2_fT", [256, S_len], F32), "v": itn("m2_v", [S_len, 256], BF16), "gs": itn("m2_gs", [S_len, 256], F32)}
    o2 = itn("o2", [S_len, 256], BF16)
    og2 = itn("og2", [nk * S_len, 256], BF16)
    with ExitStack() as es:
        S = Sched(nc, es)
        D_ALL1, D_GAT1, D_O1, D_OG1 = T(all1, "all1"), T(gat1, "gat1"), T(o1, "o1"), T(og1, "og1")
        D_A3A, D_A3B, D_G3A, D_G3B, D_O2, D_OG2 = T(a3a, "a3a"), T(a3b, "a3b"), T(g3a, "g3a"), T(g3b, "g3b"), T(o2, "o2"), T(og2, "og2")

        def ng(l, a, b):
            return E[f"ng{l}"][:, a * 8:b * 8]

        def ctx(**aps):
            return (S, None, aps)

        def gather(src, dst, Tsrc, Tdst):
            S.begin_phase()
            if _NOCC:
                rows = src.shape[0]
                for k_ in range(nk):
                    for r0 in range(0, rows, 512):
                        r1 = min(rows, r0 + 512)
                        S.dma("sp", dst[k_ * rows + r0:k_ * rows + r1, :], src[r0:r1, :], reads=[Tsrc], writes=[Tdst])
            else:
                S.collective("AllGather", groups, src, dst, reads=[Tsrc], writes=[Tdst])
            S.end_phase()
        build_ffn(TPC_, 256, ctx=ctx(xT=E["xT"], w_in=E["fwi00"], w_out=E["fwo00"], ng=ng(0, 0, 2), yT=xa))
        nsain2_phase(ctx(xT=xa, w_fm=E["nsa_wfm"], ng=ng(0, 2, 3), cs=E["cs"], all1=all1), TPC_, 256)
        gather(all1, gat1, D_ALL1, D_GAT1)
        pa_ = dict(m1)
        pa_.update(gat1=gat1, GAT1=D_GAT1, selq=E["selq"], selk=E["selk"], selg=E["selg"])
        nsaprep_phase(ctx(**pa_), S_len, TPC_)
        aa_ = dict(m1)
        aa_.update({k_: E[k_] for k_ in ("w1s", "w2", "pos2", "ident", "tri_le", "tri_gt", "cb", "erow", "mmat", "fb")})
        aa_["o"] = o1
        build_nsa_attn(S_len, ctx=ctx(**aa_))
        gather(o1, og1, D_O1, D_OG1)
        out2_phase(ctx(xT=xa, w=E["nsa_wo"], ng=ng(0, 3, 4), yT=xb_, ogat=og1, OGAT=D_OG1, wsel=E["wsel"]), S_len, TPC_, 256)
        build_ffn(TPC_, 256, ctx=ctx(xT=xb_, w_in=E["fwi01"], w_out=E["fwo01"], ng=ng(0, 4, 6), yT=xa))
        build_ple(TPC_, 256, ctx=ctx(xT=xa, pT=E["pT0"], w_p=E["wp0"], w_g=E["wg0"], ng=ng(0, 6, 8), yT=xb_))
        build_ffn(TPC_, 256, ctx=ctx(xT=xb_, w_in=E["fwi10"], w_out=E["fwo10"], ng=ng(1, 0, 2), yT=xa))
        hgrnin2_phase(ctx(xT=xa, w=E["hg_w"], ng=ng(1, 2, 3), all3a=a3a, all3b=a3b), TPC_, 256)
        gather(a3a, g3a, D_A3A, D_G3A)
        gather(a3b, g3b, D_A3B, D_G3B)
        pb_ = dict(m2)
        pb_.update(gat3a=g3a, gat3b=g3b, GAT3A=D_G3A, GAT3B=D_G3B, wI=E["wI"], wsc=E["wsc"])
        hgrnprep_phase(ctx(**pb_), S_len, TPC_)
        hb_ = dict(m2)
        hb_.update({k_: E[k_] for k_ in ("lg", "gn", "smask", "cmask", "ident")})
        hb_["o"] = o2
        build_hgrn(S_len, 1024, ctx=ctx(**hb_))
        gather(o2, og2, D_O2, D_OG2)
        out2_phase(ctx(xT=xa, w=E["hg_wo"], ng=ng(1, 3, 4), yT=xb_, ogat=og2, OGAT=D_OG2, wsel=E["wsel"]), S_len, TPC_, 256)
        build_ffn(TPC_, 256, ctx=ctx(xT=xb_, w_in=E["fwi11"], w_out=E["fwo11"], ng=ng(1, 4, 6), yT=xa))
        build_ple(TPC_, 256, ctx=ctx(xT=xa, pT=E["pT1"], w_p=E["wp1"], w_g=E["wg1"], ng=ng(1, 6, 8), yT=yT))
    return nc


def fused_inputs(S_len, TPC_, x, p, norm_gains, ffn_w_in, ffn_w_out, ple_w_in, ple_w_gate, nsa_w_in, nsa_w_out,
                 nsa_cmp_pos, nsa_cmp_w1, nsa_cmp_w2, hgrn_w_in, hgrn_w_out, hgrn_norm, hgrn_lb_logits):
    f32 = np.float32
    c_ = lambda a: np.ascontiguousarray(np.asarray(a, f32))
    nk = S_len // TPC_
    shared = {}
    for l in range(2):
        shared[f"ng{l}"] = ng_layout(np.asarray(norm_gains, f32)[l], range(8))
        for k in range(2):
            shared[f"fwi{l}{k}"] = c_(ffn_w_in[l, k])
            shared[f"fwo{l}{k}"] = c_(ffn_w_out[l, k])
        shared[f"wp{l}"] = c_(ple_w_in[l])
        shared[f"wg{l}"] = c_(ple_w_gate[l])
    shared["nsa_wfm"] = nsa_w_layout2(np.asarray(nsa_w_in, f32)[0])
    shared["nsa_wo"] = c_(np.asarray(nsa_w_out)[0])
    shared["hg_w"] = c_(np.asarray(hgrn_w_in)[0])
    shared["hg_wo"] = c_(np.asarray(hgrn_w_out)[0])
    w1 = np.asarray(nsa_cmp_w1, f32)[0].reshape(2, 2, 16, 64, 64)
    shared["w1s"] = np.ascontiguousarray(w1.transpose(0, 1, 3, 2, 4).reshape(2, 128, 16, 64))
    shared["w2"] = c_(np.asarray(nsa_cmp_w2)[0])
    pp = np.asarray(nsa_cmp_pos, f32)[0].reshape(2, 2, 16, 64)
    shared["pos2"] = np.ascontiguousarray(pp.transpose(0, 1, 3, 2).reshape(2, 128, 16))
    shared.update(nsa_consts(S_len))
    shared.update(hgrn_consts(1024))
    shared["gn"] = np.ascontiguousarray(np.tile(np.asarray(hgrn_norm, f32)[0][None, :], (64, 1)))
    lgn = np.asarray(hgrn_lb_logits, f32)
    x = np.asarray(x, f32)
    p = np.asarray(p, f32)
    in_maps = []
    for core in range(NCORES):
        b, r = core // nk, core % nk
        t0 = r * TPC_
        d = dict(shared)
        d["xT"] = np.ascontiguousarray(x[b, t0:t0 + TPC_].T)
        for l in range(2):
            d[f"pT{l}"] = np.ascontiguousarray(p[l, b, t0:t0 + TPC_].T)
        d["cs"] = rope_tables(np.arange(t0, t0 + TPC_))
        d.update(nsaprep_sel(r))
        d["wsel"] = out2_sel(r)
        d.update(hgrnprep_sel(r))
        d["lg"] = np.ascontiguousarray(lgn[:, 256 * r:256 * r + 256].reshape(2, 2, 128).transpose(2, 0, 1))
        in_maps.append(d)
    return in_maps


def _decl(nc):
    def ein(name, shape, dt):
        return nc.dram_tensor(name, list(shape), dt, kind="ExternalInput").ap()

    def eout(name, shape, dt):
        return nc.dram_tensor(name, list(shape), dt, kind="ExternalOutput").ap()

    def itn(name, shape, dt):
        return nc.dram_tensor(name, list(shape), dt).ap()
    return ein, eout, itn


def build_launch_a(TPC_):
    nc = bass.Bass("TRN2", target_bir_lowering=False)
    ein, eout, itn = _decl(nc)
    xT = ein("xT", [D, TPC_], F32)
    fwi, fwo = ein("fwi", [D, 2 * DFF], F32), ein("fwo", [DFF, D], F32)
    ngf, ngn = ein("ngf", [128, 16], F32), ein("ngn", [128, 8], F32)
    w_fm, w_tm, cs = ein("w_fm", [D, 30 * 128], F32), ein("w_tm", [D, 560], F32), ein("cs", [128, 2, TPC_], F32)
    x1 = eout("x1", [D, TPC_], F32)
    outs = {"qkT": eout("qkT", [NSA_NROPE * 128, TPC_], BF16), "vcT": eout("vcT", [256, TPC_], BF16),
            "vsw": eout("vsw", [TPC_, 512], BF16), "gates": eout("gates", [TPC_, 48], F32)}
    with ExitStack() as es:
        S = Sched(nc, es)
        build_ffn(TPC_, 256, ctx=(S, None, dict(xT=xT, w_in=fwi, w_out=fwo, ng=ngf, yT=x1)))
        a = dict(xT=x1, w_fm=w_fm, w_tm=w_tm, ng=ngn, cs=cs)
        a.update(outs)
        build_nsain(TPC_, 256, ctx=(S, None, a))
    return nc


def build_launch_c(TPC_):
    nc = bass.Bass("TRN2", target_bir_lowering=False)
    ein, eout, itn = _decl(nc)
    xT, oT = ein("xT", [D, TPC_], F32), ein("oT", [D, TPC_], BF16)
    wo, ngo = ein("wo", [D, D], F32), ein("ngo", [128, 8], F32)
    fwi1, fwo1, ngf1 = ein("fwi1", [D, 2 * DFF], F32), ein("fwo1", [DFF, D], F32), ein("ngf1", [128, 16], F32)
    pT, wp, wg, ngp = ein("pT", [256, TPC_], F32), ein("wp", [256, D], F32), ein("wg", [D, D], F32), ein("ngp", [128, 16], F32)
    fwi2, fwo2, ngf2 = ein("fwi2", [D, 2 * DFF], F32), ein("fwo2", [DFF, D], F32), ein("ngf2", [128, 16], F32)
    hw, ngh = ein("hw", [D, 4096], F32), ein("ngh", [128, 8], F32)
    xa, xb_ = itn("xa", [D, TPC_], F32), itn("xb", [D, TPC_], F32)
    x5 = eout("x5", [D, TPC_], F32)
    qfT, iv, gs = eout("qfT", [2048, TPC_], F32), eout("iv", [TPC_, D], BF16), eout("gs", [TPC_, D], F32)
    with ExitStack() as es:
        S = Sched(nc, es)
        build_out(TPC_, 256, ctx=(S, None, dict(xT=xT, oT=oT, w=wo, ng=ngo, yT=xa)))
        build_ffn(TPC_, 256, ctx=(S, None, dict(xT=xa, w_in=fwi1, w_out=fwo1, ng=ngf1, yT=xb_)))
        build_ple(TPC_, 256, ctx=(S, None, dict(xT=xb_, pT=pT, w_p=wp, w_g=wg, ng=ngp, yT=xa)))
        build_ffn(TPC_, 256, ctx=(S, None, dict(xT=xa, w_in=fwi2, w_out=fwo2, ng=ngf2, yT=x5)))
        build_hgrnin(TPC_, 256, ctx=(S, None, dict(xT=x5, w=hw, ng=ngh, qfT=qfT, iv=iv, gs=gs)))
    return nc


def build_launch_e(TPC_):
    nc = bass.Bass("TRN2", target_bir_lowering=False)
    ein, eout, itn = _decl(nc)
    xT, oT = ein("xT", [D, TPC_], F32), ein("oT", [D, TPC_], BF16)
    wo, ngo = ein("wo", [D, D], F32), ein("ngo", [128, 8], F32)
    fwi1, fwo1, ngf1 = ein("fwi1", [D, 2 * DFF], F32), ein("fwo1", [DFF, D], F32), ein("ngf1", [128, 16], F32)
    pT, wp, wg, ngp = ein("pT", [256, TPC_], F32), ein("wp", [256, D], F32), ein("wg", [D, D], F32), ein("ngp", [128, 16], F32)
    xa, xb_ = itn("xa", [D, TPC_], F32), itn("xb", [D, TPC_], F32)
    yT = eout("yT", [D, TPC_], F32)
    with ExitStack() as es:
        S = Sched(nc, es)
        build_out(TPC_, 256, ctx=(S, None, dict(xT=xT, oT=oT, w=wo, ng=ngo, yT=xa)))
        build_ffn(TPC_, 256, ctx=(S, None, dict(xT=xa, w_in=fwi1, w_out=fwo1, ng=ngf1, yT=xb_)))
        build_ple(TPC_, 256, ctx=(S, None, dict(xT=xb_, pT=pT, w_p=wp, w_g=wg, ng=ngp, yT=yT)))
    return nc


def kernel5(x, p, norm_gains, ffn_w_in, ffn_w_out, ple_w_in, ple_w_gate, nsa_w_in, nsa_w_out,
            nsa_cmp_pos, nsa_cmp_w1, nsa_cmp_w2, hgrn_w_in, hgrn_w_out, hgrn_norm, hgrn_lb_logits, S_len=S_, TPC_=TPC):
    f32 = np.float32
    c_ = lambda a: np.ascontiguousarray(np.asarray(a, f32))
    x = np.asarray(x, f32)
    p = np.asarray(p, f32)
    norm_gains = np.asarray(norm_gains, f32)
    nk = S_len // TPC_
    cores = [(r // nk, (r % nk) * TPC_) for r in range(NCORES)]

    def to_rows(o_cores):
        res = []
        for (b, t0) in cores:
            blk = np.concatenate([o_cores[b * 4 + g][t0:t0 + TPC_] for g in range(4)], axis=1)
            res.append(np.ascontiguousarray(blk.T))
        return res
    nc = _prog(("A", TPC_), lambda: build_launch_a(TPC_))
    w_fm, w_tm = nsa_w_layout(np.asarray(nsa_w_in, f32)[0])
    sh = dict(fwi=c_(ffn_w_in[0, 0]), fwo=c_(ffn_w_out[0, 0]), ngf=ng_layout(norm_gains[0], [0, 1]), ngn=ng_layout(norm_gains[0], [2]), w_fm=w_fm, w_tm=w_tm)
    r = _run(nc, [dict(sh, xT=np.ascontiguousarray(x[b, t0:t0 + TPC_].T), cs=rope_tables(np.arange(t0, t0 + TPC_))) for (b, t0) in cores])
    x1 = [q["x1"] for q in r]
    consts = nsa_consts(S_len)
    in_maps = []
    for b in range(B_):
        qk = np.concatenate([r[b * nk + k]["qkT"] for k in range(nk)], axis=1)
        vc = np.concatenate([r[b * nk + k]["vcT"] for k in range(nk)], axis=1)
        vsw = np.concatenate([r[b * nk + k]["vsw"] for k in range(nk)], axis=0)
        gt = np.concatenate([r[b * nk + k]["gates"] for k in range(nk)], axis=0)
        for g in range(4):
            in_maps.append(nsa_attn_inputs(qk, vc, vsw, gt, np.asarray(nsa_cmp_pos, f32)[0], np.asarray(nsa_cmp_w1, f32)[0],
                                           np.asarray(nsa_cmp_w2, f32)[0], g, consts))
    nc = _prog(("attn", S_len), lambda: build_nsa_attn(S_len))
    r = _run(nc, in_maps)
    oT = to_rows([q["o"] for q in r])
    nc = _prog(("C", TPC_), lambda: build_launch_c(TPC_))
    sh = dict(wo=c_(np.asarray(nsa_w_out)[0]), ngo=ng_layout(norm_gains[0], [3]),
              fwi1=c_(ffn_w_in[0, 1]), fwo1=c_(ffn_w_out[0, 1]), ngf1=ng_layout(norm_gains[0], [4, 5]),
              wp=c_(ple_w_in[0]), wg=c_(ple_w_gate[0]), ngp=ng_layout(norm_gains[0], [6, 7]),
              fwi2=c_(ffn_w_in[1, 0]), fwo2=c_(ffn_w_out[1, 0]), ngf2=ng_layout(norm_gains[1], [0, 1]),
              hw=c_(np.asarray(hgrn_w_in)[0]), ngh=ng_layout(norm_gains[1], [2]))
    r = _run(nc, [dict(sh, xT=x1[i], oT=oT[i], pT=np.ascontiguousarray(p[0, b, t0:t0 + TPC_].T)) for i, (b, t0) in enumerate(cores)])
    x5 = [q["x5"] for q in r]
    hc = hgrn_consts(1024)
    lgn = np.asarray(hgrn_lb_logits, f32)
    gn = np.ascontiguousarray(np.tile(np.asarray(hgrn_norm, f32)[0][None, :], (64, 1)))
    in_maps = []
    for b in range(B_):
        qf = np.concatenate([r[b * nk + k]["qfT"] for k in range(nk)], axis=1)
        iv = np.concatenate([r[b * nk + k]["iv"] for k in range(nk)], axis=0)
        gs = np.concatenate([r[b * nk + k]["gs"] for k in range(nk)], axis=0)
        for r2 in range(4):
            hs = slice(256 * r2, 256 * r2 + 256)
            d = {"qT": np.ascontiguousarray(qf[hs]), "fT": np.ascontiguousarray(qf[1024 + 256 * r2:1024 + 256 * r2 + 256]),
                 "v": np.ascontiguousarray(iv[:, hs]), "gs": np.ascontiguousarray(gs[:, hs]),
                 "lg": np.ascontiguousarray(lgn[:, hs].reshape(2, 2, 128).transpose(2, 0, 1)), "gn": gn}
            d.update(hc)
            in_maps.append(d)
    nc = _prog(("hgrn", S_len), lambda: build_hgrn(S_len, 1024))
    r = _run(nc, in_maps)
    oT = to_rows([q["o"] for q in r])
    nc = _prog(("E", TPC_), lambda: build_launch_e(TPC_))
    sh = dict(wo=c_(np.asarray(hgrn_w_out)[0]), ngo=ng_layout(norm_gains[1], [3]),
              fwi1=c_(ffn_w_in[1, 1]), fwo1=c_(ffn_w_out[1, 1]), ngf1=ng_layout(norm_gains[1], [4, 5]),
              wp=c_(ple_w_in[1]), wg=c_(ple_w_gate[1]), ngp=ng_layout(norm_gains[1], [6, 7]))
    r = _run(nc, [dict(sh, xT=x5[i], oT=oT[i], pT=np.ascontiguousarray(p[1, b, t0:t0 + TPC_].T)) for i, (b, t0) in enumerate(cores)])
    out = np.empty((B_, S_len, D), f32)
    for i, (b, t0) in enumerate(cores):
        out[b, t0:t0 + TPC_] = r[i]["yT"].T
    return out
```
